# Optimizing a Trainium2 kernel written in Bass

```python
import math
import jax, jax.numpy as jnp
from jax import lax
import numpy as np

D_MODEL = 1024
BATCH = 8
SEQ = 2048
DEPTH = 1

HEAD_DIM = 64
NSA_HEADS = 8
NSA_KV_HEADS = 2
NSA_WIDTH = NSA_HEADS * HEAD_DIM
KV_WIDTH = NSA_KV_HEADS * HEAD_DIM
CONV_CH = D_MODEL - NSA_WIDTH
CMP_LEN = 32
CMP_STRIDE = 16
CMP_HIDDEN = 4 * HEAD_DIM
SLC_BLOCK = 64
SLC_TOPN = 8
WINDOW = 512
Q_BLOCK = 128
CONV_WIDTH = 31
FFN_HIDDEN = -(-8 * D_MODEL // (3 * 256)) * 256
ROPE_THETA = 10000.0
EPS = 1e-6
NEG = -1e30
FORCE = 1e6
IN_WIDTH = NSA_WIDTH + 6 * KV_WIDTH + 3 * NSA_HEADS + 2 * CONV_CH

kernel_name = "hymba_nsa_conformer_conv_hybrid"


def rms_norm(x, g):
    xf = x.astype(jnp.float32)
    y = xf * lax.rsqrt(jnp.mean(xf * xf, axis=-1, keepdims=True) + EPS)
    return (y * g.astype(jnp.float32)).astype(x.dtype)


def rope(x, pos):
    half = HEAD_DIM // 2
    inv = ROPE_THETA ** (-jnp.arange(half, dtype=jnp.float32) / half)
    ang = pos.astype(jnp.float32)[:, None] * inv[None, :]
    cos, sin = jnp.cos(ang), jnp.sin(ang)
    xf = x.astype(jnp.float32)
    x1, x2 = xf[..., :half], xf[..., half:]
    return jnp.concatenate([x1 * cos - x2 * sin, x2 * cos + x1 * sin], axis=-1).astype(x.dtype)


def masked_softmax(s, m):
    p = jax.nn.softmax(jnp.where(m, s, NEG), axis=-1)
    return jnp.where(m, p, 0.0)


def n_cmp_blocks(seq):
    return (seq - CMP_LEN) // CMP_STRIDE + 1


def cmp_to_slc_overlap(seq):
    cs = np.arange(n_cmp_blocks(seq))[:, None] * CMP_STRIDE
    ss = np.arange(seq // SLC_BLOCK)[None, :] * SLC_BLOCK
    ov = np.clip(np.minimum(cs + CMP_LEN, ss + SLC_BLOCK) - np.maximum(cs, ss), 0, None)
    return (ov / CMP_LEN).astype(np.float32)


def compress(t, pe, w1, w2):
    b, g, s, hd = t.shape
    ncmp = n_cmp_blocks(s)
    idx = np.arange(ncmp)[:, None] * CMP_STRIDE + np.arange(CMP_LEN)[None, :]
    blocks = t[:, :, idx] + pe
    flat = blocks.reshape(b, g, ncmp, CMP_LEN * hd)
    return jax.nn.silu(flat @ w1) @ w2


def nsa_attention(q, kc, vc, ks, vs, kw, vw, gates):
    b, h, s, hd = q.shape
    g = kc.shape[1]
    r = h // g
    nb = s // SLC_BLOCK
    n_sel = min(SLC_TOPN, nb)
    n_qb = s // Q_BLOCK
    ncmp = kc.shape[2]
    overlap = jnp.asarray(cmp_to_slc_overlap(s))
    cmp_end = jnp.arange(ncmp) * CMP_STRIDE + CMP_LEN - 1
    scale = HEAD_DIM ** -0.5
    kcf, vcf = kc.astype(jnp.float32), vc.astype(jnp.float32)
    ks_blk = ks.reshape(b, g, nb, SLC_BLOCK, hd)
    vs_blk = vs.reshape(b, g, nb, SLC_BLOCK, hd)
    kw_pad = jnp.pad(kw, ((0, 0), (0, 0), (WINDOW, 0), (0, 0)))
    vw_pad = jnp.pad(vw, ((0, 0), (0, 0), (WINDOW, 0), (0, 0)))
    qb = q.reshape(b, g, r, n_qb, Q_BLOCK, hd).transpose(3, 0, 1, 2, 4, 5)
    gb = gates.reshape(b, g, r, n_qb, Q_BLOCK, 3).transpose(3, 0, 1, 2, 4, 5)
    bi = jnp.arange(b)[:, None, None, None]
    gi = jnp.arange(g)[None, :, None, None]
    blk = jnp.arange(nb)

    def block_fn(args):
        c, qc, gc = args
        t = c * Q_BLOCK + jnp.arange(Q_BLOCK)
        qf = qc.astype(jnp.float32) * scale
        s_c = jnp.einsum('bgrqd,bgnd->bgrqn', qf, kcf)
        p_c = masked_softmax(s_c, cmp_end[None, :] <= t[:, None])
        o_c = jnp.einsum('bgrqn,bgnd->bgrqd', p_c, vcf)
        imp = jnp.einsum('bgrqn,nj->bgqj', p_c, overlap)
        cur = t // SLC_BLOCK
        valid = blk[None, :] <= cur[:, None]
        forced = (blk[None, :] == 0) | (blk[None, :] == cur[:, None]) | (blk[None, :] == cur[:, None] - 1)
        score = jnp.where(valid, imp + jnp.where(forced, FORCE, 0.0), -FORCE)
        top_val, top_idx = lax.top_k(score, n_sel)
        k_sel = ks_blk[bi, gi, top_idx].astype(jnp.float32)
        v_sel = vs_blk[bi, gi, top_idx].astype(jnp.float32)
        tok = top_idx[..., None] * SLC_BLOCK + jnp.arange(SLC_BLOCK)
        m_s = (tok <= t[None, None, :, None, None]) & (top_val > -1.0)[..., None]
        s_s = jnp.einsum('bgrqd,bgqnld->bgrqnl', qf, k_sel).reshape(b, g, r, Q_BLOCK, n_sel * SLC_BLOCK)
        p_s = masked_softmax(s_s, m_s.reshape(b, g, 1, Q_BLOCK, n_sel * SLC_BLOCK))
        o_s = jnp.einsum('bgrqk,bgqkd->bgrqd', p_s, v_sel.reshape(b, g, Q_BLOCK, n_sel * SLC_BLOCK, hd))
        start = c * Q_BLOCK
        k_win = lax.dynamic_slice_in_dim(kw_pad, start, WINDOW + Q_BLOCK, axis=2).astype(jnp.float32)
        v_win = lax.dynamic_slice_in_dim(vw_pad, start, WINDOW + Q_BLOCK, axis=2).astype(jnp.float32)
        kpos = start - WINDOW + jnp.arange(WINDOW + Q_BLOCK)
        m_w = (kpos[None, :] <= t[:, None]) & (kpos[None, :] > t[:, None] - WINDOW) & (kpos[None, :] >= 0)
        s_w = jnp.einsum('bgrqd,bgkd->bgrqk', qf, k_win)
        o_w = jnp.einsum('bgrqk,bgkd->bgrqd', masked_softmax(s_w, m_w), v_win)
        o = gc[..., 0:1] * o_c + gc[..., 1:2] * o_s + gc[..., 2:3] * o_w
        return o.astype(q.dtype)

    out = lax.map(block_fn, (jnp.arange(n_qb), qb, gb))
    out = out.transpose(1, 2, 3, 0, 4, 5).reshape(b, h, s, hd)
    return out.transpose(0, 2, 1, 3).reshape(b, s, h * hd)


def conformer_conv(u, w_dw, b_dw, ln_g, ln_b):
    a, gate = jnp.split(u, 2, axis=-1)
    hcv = a * jax.nn.sigmoid(gate)
    hcv = lax.conv_general_dilated(hcv, w_dw, window_strides=(1,), padding=[(CONV_WIDTH - 1, 0)],
                                   dimension_numbers=('NWC', 'WIO', 'NWC'), feature_group_count=CONV_CH) + b_dw
    hf = hcv.astype(jnp.float32)
    mu = jnp.mean(hf, axis=-1, keepdims=True)
    var = jnp.mean(jnp.square(hf - mu), axis=-1, keepdims=True)
    hn = (hf - mu) * lax.rsqrt(var + EPS) * ln_g.astype(jnp.float32) + ln_b.astype(jnp.float32)
    return jax.nn.silu(hn).astype(u.dtype)


def setup_inputs(seed: int = 0) -> dict:
    key = jax.random.key(seed)
    ks = jax.random.split(key, 24)
    f32 = jnp.float32
    L = DEPTH

    def nrm(k, shape, scale):
        return jax.random.normal(k, shape, f32) * scale

    def gain(k, n):
        return 1.0 + 0.02 * jax.random.normal(k, (L, n), f32)

    return {
        "x": jax.random.normal(ks[0], (BATCH, SEQ, D_MODEL), f32),
        "attn_norm_g": gain(ks[1], D_MODEL),
        "w_in": nrm(ks[2], (L, D_MODEL, IN_WIDTH), D_MODEL ** -0.5),
        "q_norm_g": gain(ks[3], HEAD_DIM),
        "k_norm_cmp_g": gain(ks[4], HEAD_DIM),
        "k_norm_slc_g": gain(ks[5], HEAD_DIM),
        "k_norm_win_g": gain(ks[6], HEAD_DIM),
        "cmp_pe_k": nrm(ks[7], (L, CMP_LEN, HEAD_DIM), 0.1),
        "cmp_w1_k": nrm(ks[8], (L, CMP_LEN * HEAD_DIM, CMP_HIDDEN), (CMP_LEN * HEAD_DIM) ** -0.5),
        "cmp_w2_k": nrm(ks[9], (L, CMP_HIDDEN, HEAD_DIM), CMP_HIDDEN ** -0.5),
        "cmp_pe_v": nrm(ks[10], (L, CMP_LEN, HEAD_DIM), 0.1),
        "cmp_w1_v": nrm(ks[11], (L, CMP_LEN * HEAD_DIM, CMP_HIDDEN), (CMP_LEN * HEAD_DIM) ** -0.5),
        "cmp_w2_v": nrm(ks[12], (L, CMP_HIDDEN, HEAD_DIM), CMP_HIDDEN ** -0.5),
        "conv_dw_w": nrm(ks[13], (L, CONV_WIDTH, 1, CONV_CH), CONV_WIDTH ** -0.5),
        "conv_dw_b": nrm(ks[14], (L, CONV_CH), 0.02),
        "conv_ln_g": gain(ks[15], CONV_CH),
        "conv_ln_b": nrm(ks[16], (L, CONV_CH), 0.02),
        "out_norm_nsa_g": gain(ks[17], NSA_WIDTH),
        "out_norm_conv_g": gain(ks[18], CONV_CH),
        "w_out": nrm(ks[19], (L, D_MODEL, D_MODEL), D_MODEL ** -0.5),
        "ffn_norm_g": gain(ks[20], D_MODEL),
        "w_gate_up": nrm(ks[21], (L, D_MODEL, 2 * FFN_HIDDEN), D_MODEL ** -0.5),
        "w_down": nrm(ks[22], (L, FFN_HIDDEN, D_MODEL), FFN_HIDDEN ** -0.5),
    }


def reference(x, attn_norm_g, w_in, q_norm_g, k_norm_cmp_g, k_norm_slc_g, k_norm_win_g,
              cmp_pe_k, cmp_w1_k, cmp_w2_k, cmp_pe_v, cmp_w1_v, cmp_w2_v,
              conv_dw_w, conv_dw_b, conv_ln_g, conv_ln_b, out_norm_nsa_g, out_norm_conv_g,
              w_out, ffn_norm_g, w_gate_up, w_down):
    b, s, _ = x.shape
    pos = jnp.arange(s)
    cmp_pos = jnp.arange(n_cmp_blocks(s)) * CMP_STRIDE + CMP_LEN - 1
    offs = list(np.cumsum([NSA_WIDTH] + [KV_WIDTH] * 6 + [3 * NSA_HEADS]))

    def heads(t, n):
        return t.reshape(b, s, n, HEAD_DIM).transpose(0, 2, 1, 3)

    for l in range(DEPTH):
        xn = rms_norm(x, attn_norm_g[l])
        proj = xn @ w_in[l]
        q, kc_raw, vc_raw, ks_, vs_, kw_, vw_, gate_logits, conv_in = jnp.split(proj, offs, axis=-1)
        q = rope(rms_norm(heads(q, NSA_HEADS), q_norm_g[l]), pos)
        k_slc = rope(rms_norm(heads(ks_, NSA_KV_HEADS), k_norm_slc_g[l]), pos)
        k_win = rope(rms_norm(heads(kw_, NSA_KV_HEADS), k_norm_win_g[l]), pos)
        k_cmp = compress(heads(kc_raw, NSA_KV_HEADS), cmp_pe_k[l], cmp_w1_k[l], cmp_w2_k[l])
        k_cmp = rope(rms_norm(k_cmp, k_norm_cmp_g[l]), cmp_pos)
        v_cmp = compress(heads(vc_raw, NSA_KV_HEADS), cmp_pe_v[l], cmp_w1_v[l], cmp_w2_v[l])
        gates = jax.nn.sigmoid(gate_logits.astype(jnp.float32)).reshape(b, s, NSA_HEADS, 3).transpose(0, 2, 1, 3)
        o_nsa = nsa_attention(q, k_cmp, v_cmp, k_slc, heads(vs_, NSA_KV_HEADS), k_win,
                              heads(vw_, NSA_KV_HEADS), gates)
        o_conv = conformer_conv(conv_in, conv_dw_w[l], conv_dw_b[l], conv_ln_g[l], conv_ln_b[l])
        mix = jnp.concatenate([rms_norm(o_nsa, out_norm_nsa_g[l]), rms_norm(o_conv, out_norm_conv_g[l])], axis=-1)
        x = x + mix @ w_out[l]
        hn = rms_norm(x, ffn_norm_g[l])
        g_ff, u_ff = jnp.split(hn @ w_gate_up[l], 2, axis=-1)
        x = x + (jax.nn.silu(g_ff) * u_ff) @ w_down[l]
    return x
```

```python
import contextlib
import os
import numpy as np
import ml_dtypes
import concourse.bass as bass
import concourse.mybir as mybir
from concourse.bass_utils import run_bass_kernel_spmd

F32 = mybir.dt.float32
BF = mybir.dt.bfloat16
U8 = mybir.dt.uint8
AF = mybir.ActivationFunctionType
ALU = mybir.AluOpType
AX = mybir.AxisListType

PE, ACT, DVE, POOL, SP = "pe", "act", "dve", "pool", "sp"
ENGS = [PE, ACT, DVE, POOL, SP]

S_LEN = 2048
D = 1024
NT = 16
EPS = 1e-6
BIG = 29952.0
SCALE = 0.125
FFN = 2816
NJ = 22


class Tok:
    __slots__ = ("name", "w", "r")

    def __init__(self, name):
        self.name = name
        self.w = None
        self.r = []


class Op:
    __slots__ = ("eng", "fn", "deps", "dma", "sem", "val", "signal", "slot_prev")

    def __init__(self, eng, fn, dma):
        self.eng = eng
        self.fn = fn
        self.dma = dma
        self.deps = []
        self.sem = None
        self.val = None
        self.signal = dma
        self.slot_prev = None


class Sched:
    def __init__(self, nc, n_dma_sems=8):
        self.nc = nc
        self.ops = {e: [] for e in ENGS}
        self.n_dma_sems = n_dma_sems
        self.dma_count = {e: 0 for e in ENGS}
        self.dma_last = {}
        self.pending = {e: [] for e in ENGS}

    def add(self, eng, fn, r=(), w=(), dma=False):
        op = Op(eng, fn, dma)
        deps = []
        for t in r:
            if t.w is not None:
                deps.append((t.w, True))
        for t in w:
            if t.w is not None:
                deps.append((t.w, False))
            for rd in t.r:
                deps.append((rd, False))
        for d in self.pending[eng]:
            deps.append((d, True))
        self.pending[eng] = []
        seen = set()
        for d, raw in deps:
            if d is op or id(d) in seen:
                continue
            same = (d.eng == eng) and (not d.dma) and (not dma)
            if same and eng == PE:
                continue
            if same and not raw and int(os.environ.get("TESTC", "0")) == 1:
                continue
            seen.add(id(d))
            op.deps.append(d)
            d.signal = True
        for t in r:
            if not dma:
                t.r = [x for x in t.r if x.dma or x.eng != eng]
            t.r.append(op)
        for t in w:
            t.w = op
            t.r = []
        if dma:
            n = self.dma_count[eng]
            self.dma_count[eng] = n + 1
            key = (eng, n % self.n_dma_sems)
            op.slot_prev = self.dma_last.get(key)
            self.dma_last[key] = op
            op.sem = key
            op.val = 16 * (n // self.n_dma_sems + 1)
        self.ops[eng].append(op)
        return op

    def barrier(self):
        lasts = []
        for e in ENGS:
            for op in reversed(self.ops[e]):
                if not op.dma:
                    lasts.append(op)
                    break
        lasts += list(self.dma_last.values())
        for e in ENGS:
            self.pending[e] = self.pending[e] + lasts

    def emit(self, final_wait_ops=()):
        nc = self.nc
        with contextlib.ExitStack() as st:
            sems = {}
            for e in ENGS:
                sems[e] = st.enter_context(nc.semaphore("s_" + e))
                for k in range(self.n_dma_sems):
                    if self.dma_count[e] > k:
                        sems[(e, k)] = st.enter_context(nc.semaphore("d_%s_%d" % (e, k)))
            for e in ENGS:
                c = 0
                for op in self.ops[e]:
                    if op.dma:
                        continue
                    if op.signal:
                        c += 1
                        op.sem = e
                        op.val = c
            block = st.enter_context(nc.Block())
            engobj = {PE: block.tensor, ACT: block.scalar, DVE: block.vector, POOL: block.gpsimd, SP: block.sync}
            stats = {}
            for e in ENGS:
                ops = self.ops[e]
                if not ops and not (e == SP and final_wait_ops):
                    continue

                def body(eng, ops=ops, e=e):
                    waited = {}
                    nw = 0

                    def wait(semkey, val):
                        nonlocal nw
                        if waited.get(semkey, 0) >= val:
                            return
                        waited[semkey] = val
                        eng.wait_ge(sems[semkey], val)
                        nw += 1

                    for op in ops:
                        for d in op.deps:
                            wait(d.sem, d.val)
                        if op.dma and op.slot_prev is not None:
                            wait(op.slot_prev.sem, op.slot_prev.val)
                        ins = op.fn(eng)
                        if op.dma:
                            ins.then_inc(sems[op.sem], 16)
                        elif op.signal:
                            ins.then_inc(sems[op.sem], 1)
                    if e == SP:
                        for op in final_wait_ops:
                            wait(op.sem, op.val)
                    stats[e] = (len(ops), nw)

                engobj[e](body)
            self.stats = stats


CF_COS, CF_SIN, CF_G12, CF_GNSA, CF_ADDC, CF_CONVC, CF_COSC, CF_SINC, CF_GCMP, CF_END = (
    0, 512, 1024, 1792, 2304, 2816, 2956, 2988, 3020, 3084)
CB_ID, CB_MASKC, CB_DMASK, CB_END = 0, 128, 2176, 2432

C_Q, C_ROPEK, C_KV, C_V, C_GATE, C_CA, C_CG, C_END = 0, 512, 768, 1024, 1280, 1304, 1816, 2328

FFN_GROUPS = [(0, 4), (4, 8), (8, 12), (12, 16), (16, 20), (20, 22)]


def bl(ap, n):
    shp = list(ap.shape)
    return ap.unsqueeze(len(shp)).to_broadcast(shp + [n])


def bm(ap, n):
    shp = list(ap.shape)
    return ap.unsqueeze(1).to_broadcast([shp[0], n] + shp[1:])


def build(dbg=False, phases=3):
    nc = bass.Bass("TRN2", target_bir_lowering=False)

    def din(name, shape, dt=F32):
        return nc.dram_tensor(name, list(shape), dt, kind="ExternalInput").ap()

    x_d = din("x", [S_LEN, D])
    win_d = din("w_in", [D, C_END])
    wout_d = din("w_out", [D, D])
    wgu_d = din("w_gu", [D, 2 * FFN])
    wdn_d = din("w_dn", [FFN, D])
    w1k_d = din("w1k", [2048, 256])
    w1v_d = din("w1v", [2048, 256])
    w2k_d = din("w2k", [256, 64])
    w2v_d = din("w2v", [256, 64])
    cf_d = din("cf", [128, CF_END])
    gba_d = din("gbc_attn", [128, D])
    gbf_d = din("gbc_ffn", [128, D])
    cb_d = din("cb", [128, CB_END], BF)
    etab_d = din("etab", [32, S_LEN], BF)
    ovl_d = din("ovl", [127, 32], BF)
    pet_d = din("pet", [128, 32])
    y_d = nc.dram_tensor("y", [S_LEN, D], F32, kind="ExternalOutput").ap()
    dbg_out = {}

    with contextlib.ExitStack() as st:
        ARENA_BYTES = 206 * 1024
        Aten = st.enter_context(nc.sbuf_tensor("arena", [128, ARENA_BYTES], U8))
        ps = st.enter_context(nc.psum_tensor("ps", [128, 8, 512], F32))

        def psb(b):
            return ps[:, b, :]

        def psh(b):
            return ps[:, b, :].bitcast(BF)

        allocs = []

        class Reg:
            def __init__(self, base, size, phases):
                self.base, self.size, self.phases, self.ptr = base, size, phases, 0

            def alloc(self, nbytes, name=""):
                o = self.base + self.ptr
                self.ptr += (nbytes + 63) // 64 * 64
                assert self.ptr <= self.size, ("arena region overflow", name, self.ptr, self.size)
                allocs.append((name, o, nbytes, self.phases))
                return Aten[:, o:o + nbytes]

        SZ_CONST = 24 * 1024
        SZ_MIXC = 16384
        SZ_A = 37248 + 64
        SZ_B = 61 * 1024
        SZ_C = 67328
        o = 0
        R_const = Reg(o, SZ_CONST, "all"); o += SZ_CONST
        R_mixc = Reg(o, SZ_MIXC, "all"); o += SZ_MIXC
        baseA = o
        R_A1 = Reg(o, SZ_A, "p1a"); R_A1b = Reg(o, SZ_A, "p1b"); R_A2 = Reg(o, SZ_A, "p2"); o += SZ_A
        baseB = o
        SZ_HCVB = (4 * 2078 * 2 + 63) // 64 * 64
        R_Bh = Reg(o, SZ_HCVB, "p1")
        R_B1 = Reg(o + SZ_HCVB, SZ_B - SZ_HCVB, "p1a"); R_B1b = Reg(o + SZ_HCVB, SZ_B - SZ_HCVB, "p1b")
        B2HEAD = 17 * 1024
        R_B2 = Reg(o, B2HEAD, "p2"); o += SZ_B
        baseC = o
        R_C = Reg(o, SZ_C, "p12"); o += SZ_C
        assert o <= ARENA_BYTES, o
        R_3X = Reg(baseA + 16384, (baseB + B2HEAD) - (baseA + 16384), "p3")
        R_3Y = Reg(baseB + B2HEAD, SZ_B - B2HEAD, "p23")
        R_3Z = Reg(baseC, SZ_C, "p3")

        cf = R_const.alloc(CF_END * 4, "cf").bitcast(F32)
        gbc = R_const.alloc(D * 4, "gbc").bitcast(F32)
        cb = R_const.alloc(CB_END * 2, "cb").bitcast(BF)
        ones = R_const.alloc(512, "ones").bitcast(F32)
        peT = R_const.alloc(64, "peT").bitcast(BF)
        convh = R_const.alloc(32, "convh").bitcast(F32).rearrange("p (c k) -> p c k", c=4)
        nh = R_const.alloc(4, "nh").bitcast(F32)
        selpad = R_const.alloc(192, "selpad").bitcast(BF)
        smallf = R_const.alloc(1536, "small").bitcast(F32)

        cos_t = cf[:, CF_COS:CF_COS + 512].rearrange("p (t i) -> p t i", t=NT)
        sin_t = cf[:, CF_SIN:CF_SIN + 512].rearrange("p (t i) -> p t i", t=NT)
        gain12 = cf[:, CF_G12:CF_G12 + 768]
        gnsa = cf[:, CF_GNSA:CF_GNSA + 512]
        addc = cf[:, CF_ADDC:CF_ADDC + 512].rearrange("p (t j) -> p t j", t=NT)
        convc = cf[:, CF_CONVC:CF_CONVC + 140].rearrange("p (c k) -> p c k", c=4)
        cosc = cf[0:127, CF_COSC:CF_COSC + 32]
        sinc = cf[0:127, CF_SINC:CF_SINC + 32]
        gcmp = cf[0:127, CF_GCMP:CF_GCMP + 64]
        ident = cb[:, CB_ID:CB_ID + 128]
        maskc = cb[:, CB_MASKC:CB_MASKC + 2048].rearrange("p (t q) -> p t q", t=NT)
        dmask = cb[:, CB_DMASK:CB_DMASK + 256].rearrange("p (m q) -> p m q", m=2)

        _sp = [0]

        def small(n):
            o_ = _sp[0]
            _sp[0] += n
            assert _sp[0] <= 384
            return smallf[:, o_:o_ + n]

        ssq = small(1); ssq2 = small(1); rstd = small(1)
        ss12 = small(12); ss12b = small(12); r12 = small(12)
        gtmp = small(24)
        den3 = small(12); rd3 = small(12); coef3 = small(12)
        dcl = small(4); rcl = small(4)
        top8 = small(8); thr = small(1)
        imp = small(32); score = small(32); selt = small(32)
        ssqc = small(1); ssqc2 = small(1); rstdc = small(1)
        eps_ap = small(1); eps4_ap = small(1)

        mix_raw = Aten[:, R_mixc.base:R_mixc.base + 32768].bitcast(BF).rearrange("p (k t) -> p k t", k=8)
        R_mixc.alloc(16384, "mixT_conv")
        R_A2.alloc(16384, "mixT_nsa")
        mixT = mix_raw

        QB = [R_C.alloc(16384, "QB%d" % g).bitcast(BF)[0:96].rearrange("p (c r t) -> p c r t", c=NT, r=4) for g in range(2)]
        KE = [R_C.alloc(4096, "KE%d" % g).bitcast(BF)[0:96] for g in range(2)]
        KW = [R_C.alloc(4096, "KW%d" % g).bitcast(BF)[0:64] for g in range(2)]
        VA = R_C.alloc(4 * NT * 65 * 2, "VA").bitcast(BF).rearrange("p (i t d) -> p i t d", i=4, t=NT)
        kvT = [R_C.alloc(4096, "kvT%d" % g).bitcast(BF) for g in range(2)]
        gates = R_C.alloc(NT * 24 * 4, "gates").bitcast(F32).rearrange("p (t c) -> p t c", t=NT)

        w_in = R_A1.alloc(8 * C_END * 2, "w_in").bitcast(BF).rearrange("p (k c) -> p k c", k=8)
        xnT = R_B1.alloc(8192, "xnT").bitcast(BF).rearrange("p (k t) -> p k t", k=8)
        xt = [R_B1.alloc(4096, "xt%d" % i).bitcast(F32) for i in range(2)]
        xn = R_B1.alloc(2048, "xn").bitcast(BF)
        sq = R_B1.alloc(3072, "sq").bitcast(F32)
        qn = R_B1.alloc(3072, "qn").bitcast(F32)
        qr = R_B1.alloc(1536, "qr").bitcast(BF)
        rt1 = R_B1.alloc(1536, "rt1").bitcast(F32).rearrange("p (h i) -> p h i", h=12)
        rt2 = R_B1.alloc(1536, "rt2").bitcast(F32).rearrange("p (h i) -> p h i", h=12)
        kvst = R_B1.alloc(512, "kvst").bitcast(BF)
        hcvb = R_Bh.alloc(4 * 2078 * 2, "hcvb").bitcast(BF).rearrange("p (c t) -> p c t", c=4)
        Fg = [R_B1.alloc(2048, "Fg%d" % i).bitcast(F32) for i in range(2)]
        junk = R_B1.alloc(2048, "junk").bitcast(BF)
        diag = R_A1b.alloc(124 * 128 * 2, "diag").bitcast(BF).rearrange("p (i m) -> p i m", i=124)
        accs = [R_B1b.alloc(8192, "acc%d" % i).bitcast(F32).rearrange("p (c t) -> p c t", c=4) for i in range(2)]
        Ft = [R_B1b.alloc(2048, "F%d" % i).bitcast(F32) for i in range(6)]

        w1 = R_A2.alloc(16384, "w1").bitcast(BF).rearrange("p (l j) -> p l j", l=32)
        w2 = R_A2.alloc(512, "w2").bitcast(BF).rearrange("p (c v d) -> p c v d", c=2, v=2)
        kcTc = [R_A2.alloc(256, "kcTc%d" % g).bitcast(BF)[0:64, 0:127] for g in range(2)]
        VCa = [R_A2.alloc(256, "VCa%d" % g).bitcast(BF)[0:127, 0:97] for g in range(2)]
        PT = [R_B2.alloc(1024, "PT%d" % i).bitcast(BF) for i in range(4)]
        onsa = [R_B2.alloc(2048, "onsa%d" % i).bitcast(F32) for i in range(2)]
        otmp = [R_B2.alloc(1024, "otmp%d" % i).bitcast(F32) for i in range(2)]
        onb = R_B2.alloc(1024, "onb").bitcast(BF)
        cth_off = R_B2.base + R_B2.ptr
        cths = [R_B2.alloc(1024, "cth%d" % i).bitcast(F32)[0:127] for i in range(2)]
        chss = [R_B2.alloc(512, "chs%d" % i).bitcast(BF)[0:127] for i in range(2)]
        chsT2 = R_B2.alloc(1024, "chsT").bitcast(BF).rearrange("p (c n) -> p c n", c=4)
        kcm = R_B2.alloc(256, "kcm").bitcast(F32)[0:127]
        kcn = R_B2.alloc(256, "kcn").bitcast(F32)[0:127]
        kcb = R_B2.alloc(128, "kcb").bitcast(BF)[0:127]
        ct1 = R_B2.alloc(128, "ct1").bitcast(F32)[0:127]
        ct2 = R_B2.alloc(128, "ct2").bitcast(F32)[0:127]
        cjunk = R_B2.alloc(256, "cjunk").bitcast(F32)[0:127]

        hbuf = R_3Z.alloc(65536, "h").bitcast(F32).rearrange("p (t c) -> p t c", t=NT)
        wout = R_3Y.alloc(16384, "wout").bitcast(BF).rearrange("p (k c) -> p k c", k=8)
        hns = [R_3X.alloc(2048, "hn%d" % i).bitcast(BF) for i in range(2)]
        _wgu = [R_3Y.alloc(16384, "wgu0"), R_3X.alloc(16384, "wgu1")]
        wgu = [w_.bitcast(BF).rearrange("p (k u c) -> p k u c", k=8, u=2) for w_ in _wgu]
        _wdn = [R_3Y.alloc(8192, "wdn0"), R_3X.alloc(8192, "wdn1")]
        wdn = [w_.bitcast(BF).rearrange("p (j c) -> p j c", j=4) for w_ in _wdn]
        _act = [R_3Y.alloc(4096, "actT0"), R_3X.alloc(4096, "actT1")]
        actT = [a_.bitcast(BF).rearrange("p (j t) -> p j t", j=4) for a_ in _act]
        fth = [R_3X.alloc(2048, "fth%d" % i).bitcast(F32) for i in range(2)]
        fz = fth
        junk3 = fth[0].bitcast(BF)

        S = Sched(nc)
        toks = {}

        def tk(*key):
            t = toks.get(key)
            if t is None:
                t = toks[key] = Tok(str(key))
            return t

        defer = [None]

        def A(eng, fn, r=(), w=(), dma=False):
            pr = [t for t in r if t.name.startswith("('ps'")]
            if pr:
                r = [t for t in r if not t.name.startswith("('ps'")]
                w = list(w) + [t for t in pr if t not in w]
            if defer[0] is not None:
                defer[0].append((eng, fn, list(r), list(w), dma))
                return None
            return S.add(eng, fn, r=r, w=w, dma=dma)

        conv_q = []

        def drain(n):
            while n > 0 and conv_q:
                eng, fn, r, w, dma = conv_q.pop(0)
                S.add(eng, fn, r=r, w=w, dma=dma)
                n -= 1

        def rsqrt_pool(out, in_, n, scale, eps, tin, tout):
            tmp = in_
            A(POOL, lambda e: e.tensor_scalar(out=out, in0=in_, scalar1=scale, scalar2=eps, op0=ALU.mult, op1=ALU.add),
              r=[tin], w=[tout])
            A(POOL, lambda e: e.tensor_tensor(out=out, in0=out, in1=nh[0:out.shape[0], 0:1].to_broadcast(list(out.shape)), op=ALU.pow),
              r=[tout, tk("nh")], w=[tout])

        A(SP, lambda e: e.dma_start(out=cf, in_=cf_d), w=[tk("cf")], dma=True)
        A(SP, lambda e: e.dma_start(out=cb, in_=cb_d), w=[tk("cb")], dma=True)
        A(SP, lambda e: e.dma_start(out=gbc, in_=gba_d), w=[tk("gbc")], dma=True)
        WGRP = [(0, 512), (512, 1024), (1024, 1304), (1304, 2328)]

        def win_group(gi_):
            c0_, c1_ = WGRP[gi_]
            for k in range(8):
                A(POOL, lambda e, k=k: e.dma_start(out=w_in[:, k, c0_:c1_], in_=win_d[k * 128:(k + 1) * 128, c0_:c1_]),
                  w=[tk("w_in", gi_, k)], dma=True)
        for g in range(2):
            A(SP, lambda e, g=g: e.dma_start(out=KE[g][64:96, :], in_=etab_d), w=[tk("KEe", g)], dma=True)
        A(POOL, lambda e: e.memset(nh, -0.5), w=[tk("nh")])
        A(POOL, lambda e: e.memset(eps_ap, EPS), w=[tk("epsc")])
        A(POOL, lambda e: e.memset(eps4_ap, 4.0 * EPS), w=[tk("epsc")])
        A(POOL, lambda e: e.memset(ones, 1.0), w=[tk("ones")])
        A(POOL, lambda e: e.memset(VA[:, :, :, 64:65], 1.0), w=[tk("VAones")])
        if int(os.environ.get("TESTB", "0")) == 0:
            A(DVE, lambda e: e.memset(hcvb[:, :, 0:30], 0.0), w=[tk("hcvpad")])
        A(POOL, lambda e: e.memset(selpad, 0.0), w=[tk("selpad")])
        A(DVE, lambda e: e.tensor_scalar(out=convh, in0=convc[:, :, 32:34], scalar1=0.5, scalar2=None, op0=ALU.mult),
          r=[tk("cf")], w=[tk("convh")])
        win_group(0)
        w_in_toks = None

        ssq_p = [small(1), small(1)]
        rstd_p = [small(1), small(1)]
        def pbank(t):
            return 0 if t % 2 == 0 else 5

        def P01f(t):
            b0 = pbank(t)
            return ps[:, b0:b0 + 2, :].rearrange("p a b -> p (a b)")

        def p01f(t):
            b0 = pbank(t)
            return [tk("ps", b0), tk("ps", b0 + 1)]

        def stageA1(t):
            xti, txt = xt[t % 2], tk("xt", t % 2)
            sq_, rs_ = ssq_p[t % 2], rstd_p[t % 2]
            A(SP, lambda e: e.dma_start(out=xti, in_=x_d[t * 128:(t + 1) * 128, :]), w=[txt], dma=True)
            A(ACT, lambda e: e.activation(out=junk, in_=xti, func=AF.Square, scale=1.0 / 32.0, accum_out=sq_),
              r=[txt], w=[tk("junk"), tk("ssqp", t % 2)])
            rsqrt_pool(rs_, sq_, 1, 1.0, EPS, tk("ssqp", t % 2), tk("rstdp", t % 2))

        def stageA2a(t):
            xti, txt = xt[t % 2], tk("xt", t % 2)
            rs_ = rstd_p[t % 2]
            A(DVE, lambda e: e.scalar_tensor_tensor(out=xn, in0=xti, scalar=rs_, in1=gbc, op0=ALU.mult, op1=ALU.mult),
              r=[txt, tk("rstdp", t % 2), tk("gbc")], w=[tk("xn")])

        def stageA2b(t):
            tl = t % 4
            for k in range(8):
                A(PE, lambda e, k=k: e.transpose(out=psh(3)[:, k * 128:(k + 1) * 128], in_=xn[:, k * 128:(k + 1) * 128], identity=ident),
                  r=[tk("xn"), tk("cb")], w=[tk("ps", 3)])
            A(ACT, lambda e: e.copy(out=xnT[:, :, tl * 128:(tl + 1) * 128], in_=psh(3).rearrange("p (k t) -> p k t", k=8)),
              r=[tk("ps", 3)], w=[tk("xnT", tl)])

        def stageB(t):
            tl = t % 4
            b0 = pbank(t)
            for gi_, c0, c1 in [(0, 0, 512), (1, 512, 1024), (2, 1024, 1304)]:
                bank = b0 + gi_
                for k in range(8):
                    A(PE, lambda e, k=k, bank=bank, c0=c0, c1=c1: e.matmul(
                        out=ps[:, bank, 0:c1 - c0], lhsT=xnT[:, k, tl * 128:(tl + 1) * 128], rhs=w_in[:, k, c0:c1],
                        start=(k == 0), stop=(k == 7)),
                      r=[tk("xnT", tl), tk("w_in", gi_, k)], w=[tk("ps", bank)])

        def stageC1(t):
            P01, p01 = P01f(t), p01f(t)
            A(ACT, lambda e: e.activation(out=sq, in_=P01[:, 0:768], func=AF.Square), r=p01, w=[tk("sq")])
            A(DVE, lambda e: e.reduce_sum(out=ss12, in_=sq.rearrange("p (h d) -> p h d", d=64), axis=AX.X),
              r=[tk("sq")], w=[tk("ss12")])
            rsqrt_pool(r12, ss12, 12, 1.0 / 64.0, EPS, tk("ss12"), tk("r12"))

        def stageC2a(t):
            P01, p01 = P01f(t), p01f(t)
            b2 = pbank(t) + 2
            qn3 = qn.rearrange("p (h d) -> p h d", d=64)
            A(DVE, lambda e: e.tensor_tensor(out=qn3, in0=P01[:, 0:768].rearrange("p (h d) -> p h d", d=64), in1=bl(r12, 64), op=ALU.mult),
              r=p01 + [tk("r12")], w=[tk("qn")])
            A(ACT, lambda e: e.copy(out=kvst, in_=P01[:, 768:1024]), r=[p01[1]], w=[tk("kvst")])
            A(ACT, lambda e: e.copy(out=VA[:, :, t, 0:64], in_=ps[:, b2, 0:256].rearrange("p (i d) -> p i d", i=4)),
              r=[tk("ps", b2)], w=[tk("VA", t)])
            A(ACT, lambda e: e.activation(out=gtmp, in_=ps[:, b2, 256:280], func=AF.Tanh, scale=0.5), r=[tk("ps", b2)], w=[tk("gtmp")])
            A(DVE, lambda e: e.tensor_scalar(out=gates[:, t, :], in0=gtmp, scalar1=0.5, scalar2=0.5, op0=ALU.mult, op1=ALU.add),
              r=[tk("gtmp")], w=[tk("gates", t)])

        def stageC2b(t):
            qn3 = qn.rearrange("p (h d) -> p h d", d=64)
            qr3 = qr.rearrange("p (h d) -> p h d", d=64)
            A(DVE, lambda e: e.tensor_tensor(out=qn, in0=qn, in1=gain12, op=ALU.mult), r=[tk("qn"), tk("cf")], w=[tk("qn")])
            cb_ = bm(cos_t[:, t, :], 12)
            sb_ = bm(sin_t[:, t, :], 12)
            x1 = qn3[:, :, 0:32]
            x2 = qn3[:, :, 32:64]
            A(POOL, lambda e: e.tensor_tensor(out=rt2, in0=x2, in1=sb_, op=ALU.mult), r=[tk("qn"), tk("cf")], w=[tk("rt2")])
            A(DVE, lambda e: e.tensor_tensor(out=rt1, in0=x1, in1=cb_, op=ALU.mult), r=[tk("qn"), tk("cf")], w=[tk("rt1")])
            A(DVE, lambda e: e.tensor_tensor(out=qr3[:, :, 0:32], in0=rt1, in1=rt2, op=ALU.subtract),
              r=[tk("rt1"), tk("rt2")], w=[tk("qr")])
            A(POOL, lambda e: e.tensor_tensor(out=rt2, in0=x1, in1=sb_, op=ALU.mult), r=[tk("qn"), tk("cf")], w=[tk("rt2")])
            A(DVE, lambda e: e.tensor_tensor(out=rt1, in0=x2, in1=cb_, op=ALU.mult), r=[tk("qn"), tk("cf")], w=[tk("rt1")])
            A(DVE, lambda e: e.tensor_tensor(out=qr3[:, :, 32:64], in0=rt1, in1=rt2, op=ALU.add),
              r=[tk("rt1"), tk("rt2")], w=[tk("qr")])

        def stageC3(t):
            for h in range(8):
                A(PE, lambda e, h=h: e.transpose(out=psh(4)[0:64, h * 128:(h + 1) * 128], in_=qr[:, h * 64:(h + 1) * 64], identity=ident),
                  r=[tk("qr"), tk("cb")], w=[tk("ps", 4)])
            for g in range(2):
                A(ACT, lambda e, g=g: e.copy(out=QB[g][0:64, t, :, :], in_=psh(4)[0:64, g * 512:(g + 1) * 512].rearrange("p (r q) -> p r q", r=4)),
                  r=[tk("ps", 4)], w=[tk("QBq", g, t)])
            for i in range(4):
                A(PE, lambda e, i=i: e.transpose(out=psh(3)[0:64, i * 128:(i + 1) * 128], in_=qr[:, (8 + i) * 64:(9 + i) * 64], identity=ident),
                  r=[tk("qr"), tk("cb")], w=[tk("ps", 3)])
            for g in range(2):
                A(PE, lambda e, g=g: e.transpose(out=psh(3)[:, 512 + g * 128:512 + (g + 1) * 128], in_=kvst[:, g * 128:(g + 1) * 128], identity=ident),
                  r=[tk("kvst"), tk("cb")], w=[tk("ps", 3)])
            for g in range(2):
                A(ACT, lambda e, g=g: e.copy(out=KE[g][0:64, t * 128:(t + 1) * 128], in_=psh(3)[0:64, g * 128:(g + 1) * 128]),
                  r=[tk("ps", 3)], w=[tk("KE", g, t)])
                A(ACT, lambda e, g=g: e.copy(out=KW[g][0:64, t * 128:(t + 1) * 128], in_=psh(3)[0:64, (2 + g) * 128:(3 + g) * 128]),
                  r=[tk("ps", 3)], w=[tk("KW", g, t)])
            for g in range(2):
                A(DVE, lambda e, g=g: e.tensor_copy(out=kvT[g][:, t * 128:(t + 1) * 128], in_=psh(3)[:, 512 + g * 128:512 + (g + 1) * 128]),
                  r=[tk("ps", 3)], w=[tk("kvT", g)])

        def phase1_glu(tb):
            xr = [tk("xnT", i) for i in range(4)]
            for cc in range(4):
                ba, bg = (0, 1) if cc % 2 == 0 else (2, 4)
                for k in range(8):
                    A(PE, lambda e, k=k, cc=cc, ba=ba: e.matmul(out=psb(ba), lhsT=w_in[:, k, C_CA + cc * 128:C_CA + (cc + 1) * 128], rhs=xnT[:, k, :],
                                                                start=(k == 0), stop=(k == 7)), r=xr + [tk("w_in", 3, k)], w=[tk("ps", ba)])
                for k in range(8):
                    A(PE, lambda e, k=k, cc=cc, bg=bg: e.matmul(out=psb(bg), lhsT=w_in[:, k, C_CG + cc * 128:C_CG + (cc + 1) * 128], rhs=xnT[:, k, :],
                                                                start=(k == 0), stop=(k == 7)), r=xr + [tk("w_in", 3, k)], w=[tk("ps", bg)])
                Fi = Fg[cc % 2]
                tFi = tk("Fg", cc % 2)
                A(ACT, lambda e, Fi=Fi, bg=bg: e.activation(out=Fi, in_=psb(bg), func=AF.Tanh, scale=0.5), r=[tk("ps", bg)], w=[tFi])
                A(DVE, lambda e, Fi=Fi: e.tensor_scalar(out=Fi, in0=Fi, scalar1=0.5, scalar2=0.5, op0=ALU.mult, op1=ALU.add), r=[tFi], w=[tFi])
                _o = hcvb[:, cc, 30 + tb * 512:30 + (tb + 1) * 512]
                A(DVE, lambda e, Fi=Fi, cc=cc, _o=_o, ba=ba: e.tensor_tensor(out=_o, in0=psb(ba), in1=Fi, op=ALU.mult),
                  r=[tk("ps", ba), tFi], w=[tk("hcvb", tb)])

        def phase1b_setup():
            for i in range(124):
                cc, k = divmod(i, 31)
                eng = (ACT, DVE)[i % 2]
                if eng == ACT:
                    A(ACT, lambda e, i=i, cc=cc, k=k: e.activation(out=diag[:, i, :], in_=ident, func=AF.Copy, scale=convc[:, cc, k:k + 1]),
                      r=[tk("cb"), tk("cf")], w=[tk("diag", i)])
                else:
                    A(eng, lambda e, i=i, cc=cc, k=k: e.tensor_scalar(out=diag[:, i, :], in0=ident, scalar1=convc[:, cc, k:k + 1], scalar2=None, op0=ALU.mult),
                      r=[tk("cb"), tk("cf")], w=[tk("diag", i)])

        def conv_cc(tb, cc):
            acc = accs[tb % 2]
            hr = [tk("hcvb", tb), tk("hcvpad")] + ([tk("hcvb", tb - 1)] if tb > 0 else [])
            if True:
                bank = cc
                for k in range(31):
                    A(PE, lambda e, cc=cc, k=k, bank=bank: e.matmul(out=psb(bank), lhsT=diag[:, cc * 31 + k, :], rhs=hcvb[:, cc, tb * 512 + k:tb * 512 + k + 512],
                                                                    start=(k == 0), stop=(k == 30)),
                      r=hr + [tk("diag", cc * 31 + k)], w=[tk("ps", bank)])
                A(ACT, lambda e, cc=cc, bank=bank: e.activation(out=acc[:, cc, :], in_=psb(bank), func=AF.Identity, bias=convc[:, cc, 31:32], scale=1.0),
                  r=[tk("ps", bank), tk("cf")], w=[tk("acc", tb % 2, cc, 0), tk("acc", tb % 2, cc, 1)])

        def ln_half(tb, h):
            acc = accs[tb % 2]
            cs = slice(h * 256, (h + 1) * 256)
            tacc = [tk("acc", tb % 2, c, h) for c in range(4)]
            b1, b2 = (6, 7) if h == 0 else (4, 5)
            p1 = ps[:, b1, 0:256]
            p2 = ps[:, b2, 0:256]
            F = lambda i: Ft[i][:, cs]
            tF = lambda i: tk("F", i, h)
            mean, var, r2 = F(2), F(3), F(4)
            for cc in range(4):
                Fi, tFi = F(cc % 2), tF(cc % 2)
                A(ACT, lambda e, Fi=Fi, cc=cc: e.activation(out=Fi, in_=acc[:, cc, cs], func=AF.Square), r=[tacc[cc]], w=[tFi])
                A(PE, lambda e, cc=cc: e.matmul(out=p1, lhsT=ones, rhs=acc[:, cc, cs], start=(cc == 0), stop=(cc == 3)),
                  r=[tacc[cc], tk("ones")], w=[tk("ps", b1)])
                A(PE, lambda e, Fi=Fi, cc=cc: e.matmul(out=p2, lhsT=ones, rhs=Fi, start=(cc == 0), stop=(cc == 3)),
                  r=[tFi, tk("ones")], w=[tk("ps", b2)])
                if cc % 2 == 1:
                    yield
            A(DVE, lambda e: e.tensor_scalar(out=mean, in0=p1, scalar1=1.0 / 512.0, scalar2=None, op0=ALU.mult), r=[tk("ps", b1)], w=[tF(2)])
            A(DVE, lambda e: e.tensor_tensor(out=var, in0=mean, in1=mean, op=ALU.mult), r=[tF(2)], w=[tF(3)])
            A(DVE, lambda e: e.scalar_tensor_tensor(out=var, in0=p2, scalar=1.0 / 512.0, in1=var, op0=ALU.mult, op1=ALU.subtract),
              r=[tk("ps", b2), tF(3)], w=[tF(3)])
            yield
            A(ACT, lambda e: e.activation(out=var, in_=var, func=AF.Sqrt, bias=eps_ap, scale=1.0), r=[tF(3), tk("epsc")], w=[tF(3)])
            yield
            A(DVE, lambda e: e.reciprocal(out=var, in_=var), r=[tF(3)], w=[tF(3)])
            yield
            for cc in range(4):
                th, z = F(5), F(cc % 2)
                tth, tz = tF(5), tF(cc % 2)
                A(DVE, lambda e, cc=cc: e.tensor_tensor(out=acc[:, cc, cs], in0=acc[:, cc, cs], in1=mean, op=ALU.subtract),
                  r=[tacc[cc], tF(2)], w=[tacc[cc]])
                A(DVE, lambda e, cc=cc: e.tensor_tensor(out=acc[:, cc, cs], in0=acc[:, cc, cs], in1=var, op=ALU.mult),
                  r=[tacc[cc], tF(3)], w=[tacc[cc]])
                yield
                A(ACT, lambda e, cc=cc, th=th: e.activation(out=th, in_=acc[:, cc, cs], func=AF.Tanh, scale=convh[:, cc, 0:1], bias=convh[:, cc, 1:2]),
                  r=[tacc[cc], tk("convh")], w=[tth])
                A(DVE, lambda e, cc=cc, z=z: e.tensor_scalar(out=z, in0=acc[:, cc, cs], scalar1=convc[:, cc, 32:33], scalar2=convc[:, cc, 33:34],
                                                             op0=ALU.mult, op1=ALU.add), r=[tacc[cc], tk("cf")], w=[tz])
                yield
                A(DVE, lambda e, cc=cc, z=z, th=th: e.scalar_tensor_tensor(out=acc[:, cc, cs], in0=th, scalar=1.0, in1=z, op0=ALU.add, op1=ALU.mult),
                  r=[tth, tz], w=[tacc[cc]])
                yield
                A(ACT, lambda e, cc=cc, th=th: e.activation(out=th, in_=acc[:, cc, cs], func=AF.Square), r=[tacc[cc]], w=[tth])
                A(PE, lambda e, cc=cc, th=th: e.matmul(out=p1, lhsT=ones, rhs=th, start=(cc == 0), stop=(cc == 3)),
                  r=[tth, tk("ones")], w=[tk("ps", b1)])
                yield
            A(DVE, lambda e: e.tensor_copy(out=r2, in_=p1), r=[tk("ps", b1)], w=[tF(4)])
            yield
            A(ACT, lambda e: e.activation(out=r2, in_=r2, func=AF.Sqrt, bias=eps4_ap, scale=1.0 / 512.0), r=[tF(4), tk("epsc")], w=[tF(4)])
            yield
            A(DVE, lambda e: e.reciprocal(out=r2, in_=r2), r=[tF(4)], w=[tF(4)])
            yield
            t0 = tb * 512 + h * 256
            for cc in range(4):
                A(DVE, lambda e, cc=cc: e.scalar_tensor_tensor(out=mixT[:, cc, t0:t0 + 256], in0=acc[:, cc, cs], scalar=convc[:, cc, 34:35],
                                                               in1=r2, op0=ALU.mult, op1=ALU.mult),
                  r=[tacc[cc], tF(4), tk("cf")], w=[tk("mixT", tb * 4 + h * 2 + i) for i in range(2)])

        def ln_block(tb, fillers):
            gens = [ln_half(tb, 0), ln_half(tb, 1)]
            alive = [True, True]
            step = 0
            while any(alive):
                for i in range(2):
                    if alive[i]:
                        try:
                            next(gens[i])
                        except StopIteration:
                            alive[i] = False
                step += 1
                if fillers and step in (2, 5, 9, 13):
                    fillers.pop(0)()
            while fillers:
                fillers.pop(0)()


        stageA1(0)
        stageA1(1)
        win_group(1)
        win_group(2)
        A(POOL, lambda e: e.dma_start(out=peT, in_=pet_d), w=[tk("peT")], dma=True)
        stageA2a(0)
        stageA2b(0)
        stageB(0)
        for t in range(NT):
            if t % 4 == 3:
                phase1_glu(t // 4)
            if t + 2 < NT:
                stageA1(t + 2)
            if t == 0:
                win_group(3)
            if t + 1 < NT:
                stageA2a(t + 1)
            stageC1(t)
            if t + 1 < NT:
                stageA2b(t + 1)
            stageC2a(t)
            if t + 1 < NT:
                stageB(t + 1)
            stageC2b(t)
            stageC3(t)

        _skip = int(os.environ.get("SKIP1B", "0"))
        S.barrier()
        if _skip != 1:
            phase1b_setup()
        if _skip == 0:
            for cc in range(4):
                conv_cc(0, cc)
            for tb in range(4):
                fillers = [(lambda tb=tb, cc=cc: conv_cc(tb + 1, cc)) for cc in range(4)] if tb + 1 < 4 else []
                ln_block(tb, fillers)
        out_ops = []

        def dump(name, ap, shape, dt, rtoks):
            d = nc.dram_tensor("dbg_" + name, list(shape), dt, kind="ExternalOutput").ap()
            dbg_out[name] = d
            out_ops.append(A(SP, lambda e: e.dma_start(out=d, in_=ap), r=rtoks, dma=True))

        if dbg:
            S.barrier()
            alltoks = list(toks.values())
            for g in range(2):
                dump("QB%d" % g, QB[g][0:64].rearrange("p c r t -> p (c r t)"), [64, 8192], BF, alltoks)
                dump("KE%d" % g, KE[g], [96, 2048], BF, alltoks)
                dump("KW%d" % g, KW[g], [64, 2048], BF, alltoks)
                dump("kvT%d" % g, kvT[g], [128, 2048], BF, alltoks)
            dump("VA", VA.rearrange("p i t d -> p (i t d)"), [128, 4 * NT * 65], BF, alltoks)
            dump("gates", gates.rearrange("p t c -> p (t c)"), [128, NT * 24], F32, alltoks)
            dump("mixc", mixT[:, 0:4, :].rearrange("p k t -> p (k t)"), [128, 4 * 2048], BF, alltoks)

        _st = [0]
        _pt = [0]

        def next_st():
            _st[0] = (_st[0] + 1) % 3
            return _st[0]

        def next_pt():
            _pt[0] = (_pt[0] + 1) % 4
            return _pt[0]

        def phase2_setup():
            A(POOL, lambda e: e.dma_start(out=w1[0:64, :, :], in_=w1k_d.rearrange("(l d) j -> d l j", d=64)), w=[tk("w1", 0)], dma=True)
            A(POOL, lambda e: e.dma_start(out=w1[64:128, :, :], in_=w1v_d.rearrange("(l d) j -> d l j", d=64)), w=[tk("w1", 1)], dma=True)
            A(POOL, lambda e: e.dma_start(out=w2[:, :, 0, :], in_=w2k_d.rearrange("(c p) d -> p c d", p=128)), w=[tk("w2", 0)], dma=True)
            A(POOL, lambda e: e.dma_start(out=w2[:, :, 1, :], in_=w2v_d.rearrange("(c p) d -> p c d", p=128)), w=[tk("w2", 1)], dma=True)
            for g in range(2):
                A(SP, lambda e, g=g: e.dma_start(out=VCa[g][:, 65:97], in_=ovl_d), w=[tk("VCa", g)], dma=True)
                A(POOL, lambda e, g=g: e.memset(VCa[g][:, 64:65], 1.0), w=[tk("VCa", g)])

        def compress(g):
            banks = [7, 5]
            Hs = [ps[0:127, bk, 0:256] for bk in banks]
            KOs = [ps[0:127, bk, 256:320] for bk in banks]
            pts = [tk("ps", bk) for bk in banks]
            rows = [slice(0, 64), slice(64, 128)]
            for l in range(32):
                for kv in range(2):
                    A(PE, lambda e, l=l, kv=kv: e.matmul(out=Hs[kv], lhsT=kvT[g][rows[kv], l:l + 16 * 126 + 1:16], rhs=w1[rows[kv], l, :], start=(l == 0), stop=False),
                      r=[tk("kvT", g), tk("w1", kv)], w=[pts[kv]])
            for l in range(32):
                for kv in range(2):
                    A(PE, lambda e, l=l, kv=kv: e.matmul(out=Hs[kv], lhsT=peT[rows[kv], l:l + 1].to_broadcast([64, 127]), rhs=w1[rows[kv], l, :], start=False, stop=(l == 31)),
                      r=[tk("peT"), tk("w1", kv)], w=[pts[kv]])
            for kv in range(2):
                A(ACT, lambda e, kv=kv: e.activation(out=cths[kv], in_=Hs[kv], func=AF.Tanh, scale=0.5), r=[pts[kv]], w=[tk("cth", kv)])
                A(DVE, lambda e, kv=kv: e.scalar_tensor_tensor(out=chss[kv], in0=cths[kv], scalar=1.0, in1=Hs[kv], op0=ALU.add, op1=ALU.mult),
                  r=[tk("cth", kv), pts[kv]], w=[tk("chs", kv)])
            for kv in range(2):
                for jc in range(2):
                    A(PE, lambda e, jc=jc, kv=kv: e.transpose(out=psh(6)[:, kv * 256 + jc * 128:kv * 256 + jc * 128 + 127], in_=chss[kv][:, jc * 128:(jc + 1) * 128], identity=ident[0:127, 0:127]),
                      r=[tk("chs", kv), tk("cb")], w=[tk("ps", 6)])
            A(ACT, lambda e: e.copy(out=chsT2[:, :, 0:127], in_=psh(6)[:, 0:512].rearrange("p (c n) -> p c n", c=4)[:, :, 0:127]),
              r=[tk("ps", 6)], w=[tk("chsT")])
            for kv in range(2):
                for jc in range(2):
                    A(PE, lambda e, jc=jc, kv=kv: e.matmul(out=KOs[kv], lhsT=chsT2[:, kv * 2 + jc, 0:127], rhs=w2[:, jc, kv, :], start=(jc == 0), stop=(jc == 1)),
                      r=[tk("chsT"), tk("w2", kv)], w=[pts[kv]])
            KO = KOs[0]
            p7 = pts[0]
            A(ACT, lambda e: e.mul(out=VCa[g][:, 0:64], in_=KOs[1], mul=0.5), r=[pts[1]], w=[tk("VCa", g)])
            sc, rc_ = ssqc[0:127], rstdc[0:127]
            A(ACT, lambda e: e.mul(out=kcm, in_=KO, mul=0.5), r=[p7], w=[tk("kcm")])
            A(ACT, lambda e: e.activation(out=cjunk, in_=kcm, func=AF.Square, scale=0.125, accum_out=sc), r=[tk("kcm")], w=[tk("cjunk"), tk("ssqc")])
            rsqrt_pool(rc_, sc, 1, 1.0, EPS, tk("ssqc"), tk("rstdc"))
            A(DVE, lambda e: e.scalar_tensor_tensor(out=kcn, in0=kcm, scalar=rc_, in1=gcmp, op0=ALU.mult, op1=ALU.mult),
              r=[tk("kcm"), tk("rstdc"), tk("cf")], w=[tk("kcn")])
            x1, x2 = kcn[:, 0:32], kcn[:, 32:64]
            A(DVE, lambda e: e.tensor_tensor(out=ct1, in0=x1, in1=cosc, op=ALU.mult), r=[tk("kcn"), tk("cf")], w=[tk("ct1")])
            A(DVE, lambda e: e.tensor_tensor(out=ct2, in0=x2, in1=sinc, op=ALU.mult), r=[tk("kcn"), tk("cf")], w=[tk("ct2")])
            A(DVE, lambda e: e.tensor_tensor(out=kcb[:, 0:32], in0=ct1, in1=ct2, op=ALU.subtract), r=[tk("ct1"), tk("ct2")], w=[tk("kcb")])
            A(DVE, lambda e: e.tensor_tensor(out=ct1, in0=x2, in1=cosc, op=ALU.mult), r=[tk("kcn"), tk("cf")], w=[tk("ct1")])
            A(DVE, lambda e: e.tensor_tensor(out=ct2, in0=x1, in1=sinc, op=ALU.mult), r=[tk("kcn"), tk("cf")], w=[tk("ct2")])
            A(DVE, lambda e: e.tensor_tensor(out=kcb[:, 32:64], in0=ct1, in1=ct2, op=ALU.add), r=[tk("ct1"), tk("ct2")], w=[tk("kcb")])
            A(PE, lambda e: e.transpose(out=psh(6)[0:64, 512:639], in_=kcb, identity=ident[0:127, 0:127]), r=[tk("kcb"), tk("cb")], w=[tk("ps", 6)])
            A(ACT, lambda e: e.copy(out=kcTc[g], in_=psh(6)[0:64, 512:639]), r=[tk("ps", 6)], w=[tk("kcTc", g)])

        PIPE_D = 3
        ST_BANKS = [0, 1, 2, 7]
        pend = []
        _u = [0]
        Oc = ps[:, 3, 0:388].rearrange("p (r d) -> p r d", r=4)
        Os = ps[:, 4, 0:260].rearrange("p (r d) -> p r d", r=4)
        Ow = ps[:, 5, 0:260].rearrange("p (r d) -> p r d", r=4)
        p3, p4, p5 = tk("ps", 3), tk("ps", 4), tk("ps", 5)

        def emit_pv(u):
            pi = u["pi"]
            np_ = u["np"]
            for r_ in range(4):
                A(PE, lambda e, r_=r_, u=u, pi=pi, np_=np_: e.matmul(out=u["out"](r_), lhsT=PT[pi][0:np_, r_ * 128:(r_ + 1) * 128], rhs=u["v"],
                                                                   start=(u["first"] and r_ == 0), stop=u["last"], skip_group_check=True),
                  r=[tk("PT", pi)] + u["vtoks"], w=[u["otok"]])
            if u.get("after"):
                u["after"]()

        def pop_one():
            emit_pv(pend.pop(0))

        delayed = []

        def tick():
            for d in delayed:
                d[0] -= 1
            while delayed and delayed[0][0] <= 0:
                delayed.pop(0)[1]()

        def emit_unit(u):
            tick()
            i = _u[0]
            _u[0] += 1
            sb = ST_BANKS[i % 4]
            pi = i % 4
            u["pi"] = pi
            np_ = u["np"]
            A(PE, lambda e: e.matmul(out=ps[0:np_, sb, :], lhsT=u["k"], rhs=u["q"], start=True, stop=True), r=u["ktoks"], w=[tk("ps", sb)])
            A(ACT, lambda e: e.activation(out=PT[pi][0:np_, :], in_=ps[0:np_, sb, :], func=AF.Exp, scale=SCALE), r=[tk("ps", sb)], w=[tk("PT", pi)])
            if u.get("mask") is not None:
                pv = PT[pi][0:np_, :].rearrange("p (r q) -> p r q", r=4)
                A(POOL, lambda e: e.tensor_tensor(out=pv, in0=pv, in1=bm(u["mask"], 4), op=ALU.mult), r=[tk("PT", pi), tk("cb")], w=[tk("PT", pi)])
            pend.append(u)
            while len(pend) > PIPE_D:
                pop_one()

        def q64(c, g):
            return QB[g][0:64, c, :, :].rearrange("p r q -> p (r q)")

        def q96(c, g):
            return QB[g][0:96, c, :, :].rearrange("p r q -> p (r q)")

        dclA = small(4); rclA = small(4); coefA = small(4)
        den2 = small(8); rd2 = small(8); coef2 = small(8)

        def selection(c, g):
            A(DVE, lambda e: e.tensor_scalar(out=dclA, in0=Oc[:, :, 64], scalar1=1e-30, scalar2=None, op0=ALU.max), r=[p3], w=[tk("dclA")])
            A(DVE, lambda e: e.reciprocal(out=rclA, in_=dclA), r=[tk("dclA")], w=[tk("rclA")])
            A(DVE, lambda e: e.scalar_tensor_tensor(out=imp, in0=Oc[:, 0, 65:97], scalar=rclA[:, 0:1], in1=addc[:, c, :], op0=ALU.mult, op1=ALU.add),
              r=[p3, tk("rclA"), tk("cf")], w=[tk("imp")])
            for r_ in range(1, 4):
                A(DVE, lambda e, r_=r_: e.scalar_tensor_tensor(out=imp, in0=Oc[:, r_, 65:97], scalar=rclA[:, r_:r_ + 1], in1=imp, op0=ALU.mult, op1=ALU.add),
                  r=[p3, tk("rclA"), tk("imp")], w=[tk("imp")])
            A(DVE, lambda e: e.max(out=top8, in_=imp), r=[tk("imp")], w=[tk("top8")])
            A(DVE, lambda e: e.tensor_scalar(out=thr, in0=top8[:, 7:8], scalar1=-1.0, scalar2=None, op0=ALU.max), r=[tk("top8")], w=[tk("thr")])
            A(DVE, lambda e: e.tensor_scalar(out=selt, in0=imp, scalar1=thr, scalar2=None, op0=ALU.is_ge), r=[tk("imp"), tk("thr")], w=[tk("selt")])
            A(DVE, lambda e: e.tensor_scalar(out=selpad[:, 64:96], in0=selt, scalar1=-1.0, scalar2=BIG, op0=ALU.add, op1=ALU.mult),
              r=[tk("selt")], w=[tk("selpad")])
            gv = gates[:, c, g * 12:(g + 1) * 12].rearrange("p (r b) -> p b r", b=3)
            A(DVE, lambda e: e.tensor_tensor(out=coefA, in0=rclA, in1=gv[:, 0, :], op=ALU.mult), r=[tk("rclA"), tk("gates", c)], w=[tk("coefA")])
            on = onsa[c % 2][:, g * 256:(g + 1) * 256].rearrange("p (r d) -> p r d", r=4)
            A(DVE, lambda e: e.tensor_tensor(out=on, in0=Oc[:, :, 0:64], in1=bl(coefA, 64), op=ALU.mult), r=[p3, tk("coefA")], w=[tk("onsa", c % 2, g)])

        def bias_T(c, g):
            A(PE, lambda e: e.transpose(out=psh(6)[0:96, 0:128], in_=selpad, identity=ident), r=[tk("selpad"), tk("cb")], w=[tk("ps", 6)])
            A(DVE, lambda e: e.tensor_copy(out=QB[g][64:96, c, :, :], in_=bm(psh(6)[64:96, 0:128], 4)), r=[tk("ps", 6)], w=[tk("QBb", g, c)])

        def combineB(c, g):
            d2 = den2.rearrange("p (b r) -> p b r", b=2)
            r2_ = rd2.rearrange("p (b r) -> p b r", b=2)
            c2_ = coef2.rearrange("p (b r) -> p b r", b=2)
            A(DVE, lambda e: e.tensor_copy(out=obuf, in_=ps[:, 4:6, 0:260]), r=[p4, p5], w=[tk("obuf")])
            Os_ = obuf[:, 0, :].rearrange("p (r d) -> p r d", r=4)
            Ow_ = obuf[:, 1, :].rearrange("p (r d) -> p r d", r=4)
            A(DVE, lambda e: e.tensor_scalar(out=d2, in0=obuf.rearrange("p b (r d) -> p b r d", r=4)[:, :, :, 64], scalar1=1e-30, scalar2=None, op0=ALU.max),
              r=[tk("obuf")], w=[tk("den2")])
            A(DVE, lambda e: e.reciprocal(out=rd2, in_=den2), r=[tk("den2")], w=[tk("rd2")])
            gv = gates[:, c, g * 12:(g + 1) * 12].rearrange("p (r b) -> p b r", b=3)
            A(DVE, lambda e: e.tensor_tensor(out=c2_, in0=r2_, in1=gv[:, 1:3, :], op=ALU.mult), r=[tk("rd2"), tk("gates", c)], w=[tk("coef2")])
            on = onsa[c % 2][:, g * 256:(g + 1) * 256].rearrange("p (r d) -> p r d", r=4)
            ton = tk("onsa", c % 2, g)
            for bi, O_ in enumerate([Os_, Ow_]):
                ot = otmp[bi].rearrange("p (r d) -> p r d", r=4)
                A(DVE, lambda e, bi=bi, O_=O_, ot=ot: e.tensor_tensor(out=ot, in0=O_[:, :, 0:64], in1=bl(c2_[:, bi, :], 64), op=ALU.mult),
                  r=[tk("obuf"), tk("coef2")], w=[tk("otmp", bi)])
                A(DVE, lambda e, ot=ot: e.tensor_tensor(out=on, in0=on, in1=ot, op=ALU.add), r=[ton, tk("otmp", bi)], w=[ton])

        def cmp_unit(c, g, after):
            vca = [tk("VCa", g), tk("VCa", g), tk("VCa", g)]
            return dict(np=127, k=kcTc[g], q=q64(c, g), ktoks=[tk("kcTc", g), tk("QBq", g, c)], mask=maskc[0:127, c, :],
                        out=lambda r_: ps[:, 3, r_ * 97:(r_ + 1) * 97], v=VCa[g], vtoks=vca, otok=p3, first=True, last=True, after=after)

        def main_units(c, g, after_last):
            us = []
            k0 = max(0, c - 4)
            for kt in range(k0, c + 1):
                mi = 0 if kt == c else (1 if kt == c - 4 else None)
                us.append(dict(np=128, k=KW[g][0:64, kt * 128:(kt + 1) * 128], q=q64(c, g), ktoks=[tk("KW", g, kt), tk("QBq", g, c)],
                               mask=(dmask[:, mi, :] if mi is not None else None),
                               out=lambda r_: ps[:, 5, r_ * 65:(r_ + 1) * 65], v=VA[:, 2 + g, kt, :], vtoks=[tk("VA", kt), tk("VAones")], otok=p5,
                               first=(kt == k0), last=(kt == c)))
            for kt in range(c + 1):
                us.append(dict(np=128, k=KE[g][0:96, kt * 128:(kt + 1) * 128], q=q96(c, g),
                               ktoks=[tk("KE", g, kt), tk("KEe", g), tk("QBq", g, c), tk("QBb", g, c)],
                               mask=(dmask[:, 0, :] if kt == c else None),
                               out=lambda r_: ps[:, 4, r_ * 65:(r_ + 1) * 65], v=VA[:, g, kt, :], vtoks=[tk("VA", kt), tk("VAones")], otok=p4,
                               first=(kt == 0), last=(kt == c)))
            us[-1]["after"] = after_last
            return us

        def attention_all():
            order = [(c, g) for c in range(NT) for g in range(2)]
            state = {}

            def start_cmp(j):
                cj, gj = order[j]
                state[j] = False

                def after():
                    selection(cj, gj)
                    state[j] = True
                emit_unit(cmp_unit(cj, gj, after))

            start_cmp(0)
            while not state[0]:
                pop_one()
            bias_T(*order[0])
            for i, (c, g) in enumerate(order):
                if i + 1 < len(order):
                    start_cmp(i + 1)

                def after_last(c=c, g=g):
                    combineB(c, g)
                    if g == 1:
                        delayed.append([3, lambda c=c: attn_finish_a(c)])
                        delayed.append([8, lambda c=c: attn_finish_b(c)])
                for u in main_units(c, g, after_last):
                    emit_unit(u)
                if i + 1 < len(order):
                    while not state[i + 1]:
                        pop_one()
                    bias_T(*order[i + 1])
            while pend:
                pop_one()
            while delayed:
                delayed.pop(0)[1]()

        def attn_finish_a(c):
            o_ = onsa[c % 2]
            tt_ = [tk("onsa", c % 2, 0), tk("onsa", c % 2, 1)]
            A(ACT, lambda e: e.activation(out=junk2, in_=o_, func=AF.Square, scale=float(1.0 / np.sqrt(512.0)), accum_out=ssq), r=tt_, w=[tk("junk2"), tk("ssq")])
            rsqrt_pool(rstd, ssq, 1, 1.0, EPS, tk("ssq"), tk("rstd"))
            A(DVE, lambda e: e.scalar_tensor_tensor(out=onb, in0=o_, scalar=rstd, in1=gnsa, op0=ALU.mult, op1=ALU.mult), r=tt_ + [tk("rstd"), tk("cf")], w=[tk("onb")])

        def attn_finish_b(c):
            for k in range(4):
                A(PE, lambda e, k=k: e.transpose(out=psh(6)[:, 256 + k * 128:256 + (k + 1) * 128], in_=onb[:, k * 128:(k + 1) * 128], identity=ident),
                  r=[tk("onb"), tk("cb")], w=[tk("ps", 6)])
            A(DVE, lambda e: e.tensor_copy(out=mixT[:, 4:8, c * 128:(c + 1) * 128], in_=psh(6)[:, 256:768].rearrange("p (k t) -> p k t", k=4)),
              r=[tk("ps", 6)], w=[tk("mixT", c)])

        def ffn_weight_dmas(gi):
            j0, j1 = FFN_GROUPS[gi]
            nj = j1 - j0
            b = gi % 2
            for u in range(2):
                A(POOL, lambda e, u=u: e.dma_start(
                    out=wgu[b][:, :, u, 0:nj * 128], in_=wgu_d[:, u * FFN + j0 * 128:u * FFN + j1 * 128].rearrange("(k p) c -> p k c", p=128)),
                  w=[tk("wgu", b, u)], dma=True)
            A(POOL, lambda e: e.dma_start(out=wdn[b][:, 0:nj, :], in_=wdn_d[j0 * 128:j1 * 128, :].rearrange("(j p) c -> p j c", p=128)),
              w=[tk("wdn", b)], dma=True)

        def prefetch3():
            A(SP, lambda e: e.dma_start(out=gbc, in_=gbf_d), w=[tk("gbc")], dma=True)
            for k in range(8):
                A(POOL, lambda e, k=k: e.dma_start(out=wout[:, k, :], in_=wout_d[k * 128:(k + 1) * 128, :]), w=[tk("wout", k)], dma=True)
            ffn_weight_dmas(0)

        def phase3():
            for t in range(NT):
                A(SP, lambda e, t=t: e.dma_start(out=hbuf[:, t, :], in_=x_d[t * 128:(t + 1) * 128, :]), w=[tk("h", t)], dma=True)

            def s1(t):
                b0 = 0 if t % 2 == 0 else 4
                Pv = ps[:, b0:b0 + 2, :].rearrange("p a b -> p (a b)")
                sq_, rs_ = ssq_p[t % 2], rstd_p[t % 2]
                for cc in range(2):
                    for k in range(8):
                        A(PE, lambda e, cc=cc, k=k: e.matmul(out=psb(b0 + cc), lhsT=mixT[:, k, t * 128:(t + 1) * 128], rhs=wout[:, k, cc * 512:(cc + 1) * 512],
                                                             start=(k == 0), stop=(k == 7)), r=[tk("mixT", t), tk("wout", k)], w=[tk("ps", b0 + cc)])
                A(DVE, lambda e: e.tensor_tensor(out=hbuf[:, t, :], in0=hbuf[:, t, :], in1=Pv, op=ALU.add), r=[tk("ps", b0), tk("ps", b0 + 1)], w=[tk("h", t)])
                A(ACT, lambda e: e.activation(out=junk3, in_=hbuf[:, t, :], func=AF.Square, scale=1.0 / 32.0, accum_out=sq_), r=[tk("h", t)], w=[tk("fth", 0), tk("ssqp", t % 2)])
                rsqrt_pool(rs_, sq_, 1, 1.0, EPS, tk("ssqp", t % 2), tk("rstdp", t % 2))

            def s2(t):
                hn = hns[t % 2]
                tb_ = 2 if t % 2 == 0 else 6
                A(DVE, lambda e: e.scalar_tensor_tensor(out=hn, in0=hbuf[:, t, :], scalar=rstd_p[t % 2], in1=gbc, op0=ALU.mult, op1=ALU.mult),
                  r=[tk("h", t), tk("rstdp", t % 2), tk("gbc")], w=[tk("hn", t % 2)])
                for k in range(8):
                    A(PE, lambda e, k=k: e.transpose(out=psh(tb_)[:, k * 128:(k + 1) * 128], in_=hn[:, k * 128:(k + 1) * 128], identity=ident),
                      r=[tk("hn", t % 2), tk("cb")], w=[tk("ps", tb_)])
                A(ACT, lambda e: e.copy(out=mixT[:, :, t * 128:(t + 1) * 128], in_=psh(tb_).rearrange("p (k t) -> p k t", k=8)),
                  r=[tk("ps", tb_)], w=[tk("mixT", t)])

            s1(0)
            for t in range(NT):
                if t + 1 < NT:
                    s1(t + 1)
                s2(t)
            if dbg:
                for t in range(NT):
                    pass
            for gi, (j0, j1) in enumerate(FFN_GROUPS):
                nj = j1 - j0
                b = gi % 2
                if gi > 0:
                    ffn_weight_dmas(gi)
                last = gi == len(FFN_GROUPS) - 1
                for tb in range(4):
                    ab, tab = actT[tb % 2], tk("actT", tb % 2)
                    hT = [tk("mixT", tb * 4 + i) for i in range(4)]
                    for jj in range(nj):
                        gb, ub = (0, 1) if jj % 2 == 0 else (2, 3)
                        f = jj % 2
                        for u, bank in ((0, gb), (1, ub)):
                            for k in range(8):
                                A(PE, lambda e, u=u, bank=bank, k=k, jj=jj, tb=tb, b=b: e.matmul(
                                    out=psb(bank), lhsT=wgu[b][:, k, u, jj * 128:(jj + 1) * 128], rhs=mixT[:, k, tb * 512:(tb + 1) * 512],
                                    start=(k == 0), stop=(k == 7)), r=hT + [tk("wgu", b, u)], w=[tk("ps", bank)])
                        tf = tk("fth", f)
                        A(ACT, lambda e, f=f, gb=gb: e.activation(out=fth[f], in_=psb(gb), func=AF.Tanh, scale=0.5), r=[tk("ps", gb)], w=[tf])
                        A(DVE, lambda e, f=f, gb=gb: e.scalar_tensor_tensor(out=fz[f], in0=fth[f], scalar=1.0, in1=psb(gb), op0=ALU.add, op1=ALU.mult),
                          r=[tf, tk("ps", gb)], w=[tf])
                        A(DVE, lambda e, f=f, ub=ub, jj=jj, ab=ab: e.scalar_tensor_tensor(out=ab[:, jj, :], in0=fz[f], scalar=0.5, in1=psb(ub), op0=ALU.mult, op1=ALU.mult),
                          r=[tf, tk("ps", ub)], w=[tab])
                    for tl in range(4):
                        t = tb * 4 + tl
                        for cc in range(2):
                            yb = 4 + (tl * 2 + cc) % 4
                            for jj in range(nj):
                                A(PE, lambda e, jj=jj, tl=tl, cc=cc, yb=yb, ab=ab, b=b, nj=nj: e.matmul(
                                    out=psb(yb), lhsT=ab[:, jj, tl * 128:(tl + 1) * 128], rhs=wdn[b][:, jj, cc * 512:(cc + 1) * 512],
                                    start=(jj == 0), stop=(jj == nj - 1)), r=[tab, tk("wdn", b)], w=[tk("ps", yb)])
                            hs = hbuf[:, t, cc * 512:(cc + 1) * 512]
                            A(DVE, lambda e, hs=hs, yb=yb: e.tensor_tensor(out=hs, in0=hs, in1=psb(yb), op=ALU.add), r=[tk("h", t), tk("ps", yb)], w=[tk("h", t)])
                        if last:
                            out_ops.append(A(SP, lambda e, t=t: e.dma_start(out=y_d[t * 128:(t + 1) * 128, :], in_=hbuf[:, t, :]), r=[tk("h", t)], dma=True))

        if phases >= 2:
            S.barrier()
            junk2 = Aten[:, cth_off:cth_off + 1024].bitcast(BF)
            obuf = Aten[:, cth_off + 1024:cth_off + 1024 + 2080].bitcast(F32).rearrange("p (b n) -> p b n", b=2)
            phase2_setup()
            if phases >= 3:
                prefetch3()
            for g in range(2):
                compress(g)
            if dbg:
                for g in range(2):
                    dump("kcTc%d" % g, kcTc[g], [64, 127], BF, [tk("kcTc", g)])
                    dump("VCa%d" % g, VCa[g], [127, 97], BF, [tk("VCa", g), tk("VCa", g), tk("VCa", g)])
            attention_all()
            if dbg:
                dump("mixn", mixT[:, 4:8, :].rearrange("p k t -> p (k t)"), [128, 4 * 2048], BF, [tk("mixT", t) for t in range(NT)])
        if phases >= 3:
            S.barrier()
            phase3()

        def live(pa, pb):
            sets = {"all": {0, 1, 2, 3}, "p1a": {0}, "p1b": {1}, "p1": {0, 1}, "p2": {2}, "p12": {0, 1, 2}, "p3": {3}, "p23": {2, 3}}
            return bool(sets[pa] & sets[pb])
        for i in range(len(allocs)):
            for j in range(i + 1, len(allocs)):
                n1, o1, s1, p1 = allocs[i]
                n2, o2, s2, p2 = allocs[j]
                if live(p1, p2) and o1 < o2 + s2 and o2 < o1 + s1:
                    raise AssertionError("arena overlap %s %s" % (allocs[i], allocs[j]))

        S.emit(final_wait_ops=out_ops)
        build.stats = S.stats
    return nc, dbg_out


def _consts():
    half = 32
    inv = 10000.0 ** (-np.arange(half, dtype=np.float64) / half)
    pos = np.arange(S_LEN, dtype=np.float64)
    ang = pos[:, None] * inv[None, :]
    cos = np.cos(ang).astype(np.float32).reshape(NT, 128, 32).transpose(1, 0, 2).reshape(128, 512)
    sin = np.sin(ang).astype(np.float32).reshape(NT, 128, 32).transpose(1, 0, 2).reshape(128, 512)
    cpos = np.arange(127, dtype=np.float64) * 16 + 31
    angc = cpos[:, None] * inv[None, :]
    cosc = np.zeros((128, 32), np.float32); cosc[:127] = np.cos(angc)
    sinc = np.zeros((128, 32), np.float32); sinc[:127] = np.sin(angc)
    ql = np.arange(128)[:, None, None]
    c = np.arange(NT)[None, :, None]
    j = np.arange(32)[None, None, :]
    cur = 2 * c + (ql >= 64)
    valid = j <= cur
    forced = (j == 0) | (j == cur) | (j == cur - 1)
    addc = np.where(valid, np.where(forced, 1e6, 0.0), -2e6).astype(np.float32).reshape(128, 512)
    n = np.arange(128)[:, None, None]
    c2 = np.arange(NT)[None, :, None]
    q2 = np.arange(128)[None, None, :]
    maskc = ((16 * n + 31) <= (128 * c2 + q2)).astype(np.float32)
    maskc[127] = 0
    kl = np.arange(128)[:, None]
    qq = np.arange(128)[None, :]
    dmask = np.stack([(kl <= qq), (kl > qq)], axis=1).astype(np.float32)
    cbb = np.concatenate([np.eye(128, dtype=np.float32), maskc.reshape(128, 2048), dmask.reshape(128, 256)], axis=1).astype(ml_dtypes.bfloat16)
    etab = (np.arange(S_LEN)[None, :] // 64 == np.arange(32)[:, None]).astype(np.float32).astype(ml_dtypes.bfloat16)
    cs = np.arange(127)[:, None] * 16
    ss = np.arange(32)[None, :] * 64
    ov = np.clip(np.minimum(cs + 32, ss + 64) - np.maximum(cs, ss), 0, None) / 32.0
    ovl = ov.astype(np.float32).astype(ml_dtypes.bfloat16)
    return cos, sin, cosc, sinc, addc, cbb, etab, ovl


def _prep(inputs):
    f = lambda a: np.ascontiguousarray(np.asarray(a, dtype=np.float32))
    cos, sin, cosc, sinc, addc, cbb, etab, ovl = _consts()
    w_in = f(inputs["w_in"])[0]
    cols = np.concatenate([
        np.arange(0, 512),
        np.arange(768, 896), np.arange(1024, 1152),
        np.arange(512, 576), np.arange(640, 704), np.arange(576, 640), np.arange(704, 768),
        np.arange(896, 1024), np.arange(1152, 1280),
        np.arange(1280, 1304),
        np.arange(1304, 2328)])
    w_in_p = np.ascontiguousarray(w_in[:, cols])
    w_out = f(inputs["w_out"])[0]
    w_out_p = np.ascontiguousarray(np.concatenate([w_out[512:], w_out[:512]], axis=0))
    rep = lambda v, nrep: np.tile(f(v).reshape(-1), nrep)
    g12 = np.concatenate([rep(inputs["q_norm_g"], 8), rep(inputs["k_norm_slc_g"], 2), rep(inputs["k_norm_win_g"], 2)])
    bc = lambda v: np.ascontiguousarray(np.broadcast_to(np.asarray(v, np.float32).reshape(1, -1), (128, np.asarray(v).size)))
    convc = np.zeros((128, 4, 35), np.float32)
    dw = f(inputs["conv_dw_w"])[0, :, 0, :]
    convc[:, :, 0:31] = dw.T.reshape(4, 128, 31).transpose(1, 0, 2)
    for idx, nm in [(31, "conv_dw_b"), (32, "conv_ln_g"), (33, "conv_ln_b"), (34, "out_norm_conv_g")]:
        convc[:, :, idx] = f(inputs[nm])[0].reshape(4, 128).T
    cfb = np.concatenate([cos, sin, bc(g12), bc(f(inputs["out_norm_nsa_g"])[0]), addc, convc.reshape(128, 140), cosc, sinc,
                          bc(f(inputs["k_norm_cmp_g"])[0])], axis=1)
    assert cfb.shape == (128, CF_END), cfb.shape
    pet = np.concatenate([f(inputs["cmp_pe_k"])[0].T, f(inputs["cmp_pe_v"])[0].T], axis=0)
    shared = {
        "w_in": w_in_p, "w_out": w_out_p, "w_gu": f(inputs["w_gate_up"])[0], "w_dn": f(inputs["w_down"])[0],
        "w1k": f(inputs["cmp_w1_k"])[0], "w1v": f(inputs["cmp_w1_v"])[0], "w2k": f(inputs["cmp_w2_k"])[0], "w2v": f(inputs["cmp_w2_v"])[0],
        "cf": np.ascontiguousarray(cfb), "gbc_attn": bc(f(inputs["attn_norm_g"])[0]), "gbc_ffn": bc(f(inputs["ffn_norm_g"])[0]),
        "cb": np.ascontiguousarray(cbb), "etab": np.ascontiguousarray(etab), "ovl": np.ascontiguousarray(ovl), "pet": np.ascontiguousarray(pet),
    }
    x = f(inputs["x"])
    return [dict(shared, x=np.ascontiguousarray(x[b])) for b in range(8)]


def kernel(**inputs):
    nc, _ = build()
    in_maps = _prep(inputs)
    res = run_bass_kernel_spmd(nc, in_maps, core_ids=list(range(8)))
    return np.stack([np.asarray(r["y"], dtype=np.float32) for r in res.results], axis=0)
```

```python
import contextlib
import os
import numpy as np
import ml_dtypes
import concourse.bass as bass
import concourse.mybir as mybir
from concourse.bass_utils import run_bass_kernel_spmd

F32 = mybir.dt.float32
BF = mybir.dt.bfloat16
U8 = mybir.dt.uint8
AF = mybir.ActivationFunctionType
ALU = mybir.AluOpType
AX = mybir.AxisListType

PE, ACT, DVE, POOL, SP = "pe", "act", "dve", "pool", "sp"
ENGS = [PE, ACT, DVE, POOL, SP]

S_LEN = 2048
D = 1024
NT = 16
EPS = 1e-6
BIG = 29952.0
SCALE = 0.125
FFN = 2816
NJ = 22


class Tok:
    __slots__ = ("name", "w", "r")

    def __init__(self, name):
        self.name = name
        self.w = None
        self.r = []


class Op:
    __slots__ = ("eng", "fn", "deps", "dma", "sem", "val", "signal", "slot_prev")

    def __init__(self, eng, fn, dma):
        self.eng = eng
        self.fn = fn
        self.dma = dma
        self.deps = []
        self.sem = None
        self.val = None
        self.signal = dma
        self.slot_prev = None


class Sched:
    def __init__(self, nc, n_dma_sems=8):
        self.nc = nc
        self.ops = {e: [] for e in ENGS}
        self.n_dma_sems = n_dma_sems
        self.dma_count = {e: 0 for e in ENGS}
        self.dma_last = {}
        self.pending = {e: [] for e in ENGS}

    def add(self, eng, fn, r=(), w=(), dma=False):
        op = Op(eng, fn, dma)
        deps = []
        for t in r:
            if t.w is not None:
                deps.append((t.w, True))
        for t in w:
            if t.w is not None:
                deps.append((t.w, False))
            for rd in t.r:
                deps.append((rd, False))
        for d in self.pending[eng]:
            deps.append((d, True))
        self.pending[eng] = []
        seen = set()
        for d, raw in deps:
            if d is op or id(d) in seen:
                continue
            same = (d.eng == eng) and (not d.dma) and (not dma)
            if same and eng == PE:
                continue
            if same and not raw and int(os.environ.get("TESTC", "0")) == 1:
                continue
            seen.add(id(d))
            op.deps.append(d)
            d.signal = True
        for t in r:
            if not dma:
                t.r = [x for x in t.r if x.dma or x.eng != eng]
            t.r.append(op)
        for t in w:
            t.w = op
            t.r = []
        if dma:
            n = self.dma_count[eng]
            self.dma_count[eng] = n + 1
            key = (eng, n % self.n_dma_sems)
            op.slot_prev = self.dma_last.get(key)
            self.dma_last[key] = op
            op.sem = key
            op.val = 16 * (n // self.n_dma_sems + 1)
        self.ops[eng].append(op)
        return op

    def barrier(self):
        lasts = []
        for e in ENGS:
            for op in reversed(self.ops[e]):
                if not op.dma:
                    lasts.append(op)
                    break
        lasts += list(self.dma_last.values())
        for e in ENGS:
            self.pending[e] = self.pending[e] + lasts

    def emit(self, final_wait_ops=()):
        nc = self.nc
        with contextlib.ExitStack() as st:
            sems = {}
            for e in ENGS:
                sems[e] = st.enter_context(nc.semaphore("s_" + e))
                for k in range(self.n_dma_sems):
                    if self.dma_count[e] > k:
                        sems[(e, k)] = st.enter_context(nc.semaphore("d_%s_%d" % (e, k)))
            for e in ENGS:
                c = 0
                for op in self.ops[e]:
                    if op.dma:
                        continue
                    if op.signal:
                        c += 1
                        op.sem = e
                        op.val = c
            block = st.enter_context(nc.Block())
            engobj = {PE: block.tensor, ACT: block.scalar, DVE: block.vector, POOL: block.gpsimd, SP: block.sync}
            stats = {}
            for e in ENGS:
                ops = self.ops[e]
                if not ops and not (e == SP and final_wait_ops):
                    continue

                def body(eng, ops=ops, e=e):
                    waited = {}
                    nw = 0

                    def wait(semkey, val):
                        nonlocal nw
                        if waited.get(semkey, 0) >= val:
                            return
                        waited[semkey] = val
                        eng.wait_ge(sems[semkey], val)
                        nw += 1

                    for op in ops:
                        for d in op.deps:
                            wait(d.sem, d.val)
                        if op.dma and op.slot_prev is not None:
                            wait(op.slot_prev.sem, op.slot_prev.val)
                        ins = op.fn(eng)
                        if op.dma:
                            ins.then_inc(sems[op.sem], 16)
                        elif op.signal:
                            ins.then_inc(sems[op.sem], 1)
                    if e == SP:
                        for op in final_wait_ops:
                            wait(op.sem, op.val)
                    stats[e] = (len(ops), nw)

                engobj[e](body)
            self.stats = stats


CF_COS, CF_SIN, CF_G12, CF_GNSA, CF_ADDC, CF_CONVC, CF_COSC, CF_SINC, CF_GCMP, CF_END = (
    0, 512, 1024, 1792, 2304, 2816, 2956, 2988, 3020, 3084)
CB_ID, CB_MASKC, CB_DMASK, CB_END = 0, 128, 2176, 2432

C_Q, C_ROPEK, C_KV, C_V, C_GATE, C_CA, C_CG, C_END = 0, 512, 768, 1024, 1280, 1304, 1816, 2328

FFN_GROUPS = [(0, 4), (4, 8), (8, 12), (12, 16), (16, 20), (20, 22)]


def bl(ap, n):
    shp = list(ap.shape)
    return ap.unsqueeze(len(shp)).to_broadcast(shp + [n])


def bm(ap, n):
    shp = list(ap.shape)
    return ap.unsqueeze(1).to_broadcast([shp[0], n] + shp[1:])


def build(dbg=False, phases=3):
    nc = bass.Bass("TRN2", target_bir_lowering=False)

    def din(name, shape, dt=F32):
        return nc.dram_tensor(name, list(shape), dt, kind="ExternalInput").ap()

    x_d = din("x", [S_LEN, D])
    win_d = din("w_in", [D, C_END])
    wout_d = din("w_out", [D, D])
    wgu_d = din("w_gu", [D, 2 * FFN])
    wdn_d = din("w_dn", [FFN, D])
    w1k_d = din("w1k", [2048, 256])
    w1v_d = din("w1v", [2048, 256])
    w2k_d = din("w2k", [256, 64])
    w2v_d = din("w2v", [256, 64])
    cf_d = din("cf", [128, CF_END])
    gba_d = din("gbc_attn", [128, D])
    gbf_d = din("gbc_ffn", [128, D])
    cb_d = din("cb", [128, CB_END], BF)
    etab_d = din("etab", [32, S_LEN], BF)
    ovl_d = din("ovl", [127, 32], BF)
    pet_d = din("pet", [128, 32])
    y_d = nc.dram_tensor("y", [S_LEN, D], F32, kind="ExternalOutput").ap()
    dbg_out = {}

    with contextlib.ExitStack() as st:
        ARENA_BYTES = 206 * 1024
        Aten = st.enter_context(nc.sbuf_tensor("arena", [128, ARENA_BYTES], U8))
        ps = st.enter_context(nc.psum_tensor("ps", [128, 8, 512], F32))

        def psb(b):
            return ps[:, b, :]

        def psh(b):
            return ps[:, b, :].bitcast(BF)

        allocs = []

        class Reg:
            def __init__(self, base, size, phases):
                self.base, self.size, self.phases, self.ptr = base, size, phases, 0

            def alloc(self, nbytes, name=""):
                o = self.base + self.ptr
                self.ptr += (nbytes + 63) // 64 * 64
                assert self.ptr <= self.size, ("arena region overflow", name, self.ptr, self.size)
                allocs.append((name, o, nbytes, self.phases))
                return Aten[:, o:o + nbytes]

        SZ_CONST = 24 * 1024
        SZ_MIXC = 16384
        SZ_A = 37248 + 64
        SZ_B = 61 * 1024
        SZ_C = 67328
        o = 0
        R_const = Reg(o, SZ_CONST, "all"); o += SZ_CONST
        R_mixc = Reg(o, SZ_MIXC, "all"); o += SZ_MIXC
        baseA = o
        R_A1 = Reg(o, SZ_A, "p1a"); R_A1b = Reg(o, SZ_A, "p1b"); R_A2 = Reg(o, SZ_A, "p2"); o += SZ_A
        baseB = o
        SZ_HCVB = (4 * 2078 * 2 + 63) // 64 * 64
        R_Bh = Reg(o, SZ_HCVB, "p1")
        R_B1 = Reg(o + SZ_HCVB, SZ_B - SZ_HCVB, "p1a"); R_B1b = Reg(o + SZ_HCVB, SZ_B - SZ_HCVB, "p1b")
        B2HEAD = 17 * 1024
        R_B2 = Reg(o, B2HEAD, "p2"); o += SZ_B
        baseC = o
        R_C = Reg(o, SZ_C, "p12"); o += SZ_C
        assert o <= ARENA_BYTES, o
        R_3X = Reg(baseA + 16384, (baseB + B2HEAD) - (baseA + 16384), "p3")
        R_3Y = Reg(baseB + B2HEAD, SZ_B - B2HEAD, "p23")
        R_3Z = Reg(baseC, SZ_C, "p3")

        cf = R_const.alloc(CF_END * 4, "cf").bitcast(F32)
        gbc = R_const.alloc(D * 4, "gbc").bitcast(F32)
        cb = R_const.alloc(CB_END * 2, "cb").bitcast(BF)
        ones = R_const.alloc(512, "ones").bitcast(F32)
        peT = R_const.alloc(64, "peT").bitcast(BF)
        convh = R_const.alloc(32, "convh").bitcast(F32).rearrange("p (c k) -> p c k", c=4)
        nh = R_const.alloc(4, "nh").bitcast(F32)
        selpad = R_const.alloc(192, "selpad").bitcast(BF)
        smallf = R_const.alloc(1536, "small").bitcast(F32)

        cos_t = cf[:, CF_COS:CF_COS + 512].rearrange("p (t i) -> p t i", t=NT)
        sin_t = cf[:, CF_SIN:CF_SIN + 512].rearrange("p (t i) -> p t i", t=NT)
        gain12 = cf[:, CF_G12:CF_G12 + 768]
        gnsa = cf[:, CF_GNSA:CF_GNSA + 512]
        addc = cf[:, CF_ADDC:CF_ADDC + 512].rearrange("p (t j) -> p t j", t=NT)
        convc = cf[:, CF_CONVC:CF_CONVC + 140].rearrange("p (c k) -> p c k", c=4)
        cosc = cf[0:127, CF_COSC:CF_COSC + 32]
        sinc = cf[0:127, CF_SINC:CF_SINC + 32]
        gcmp = cf[0:127, CF_GCMP:CF_GCMP + 64]
        ident = cb[:, CB_ID:CB_ID + 128]
        maskc = cb[:, CB_MASKC:CB_MASKC + 2048].rearrange("p (t q) -> p t q", t=NT)
        dmask = cb[:, CB_DMASK:CB_DMASK + 256].rearrange("p (m q) -> p m q", m=2)

        _sp = [0]

        def small(n):
            o_ = _sp[0]
            _sp[0] += n
            assert _sp[0] <= 384
            return smallf[:, o_:o_ + n]

        ssq = small(1); ssq2 = small(1); rstd = small(1)
        ss12 = small(12); ss12b = small(12); r12 = small(12)
        gtmp = small(24)
        den3 = small(12); rd3 = small(12); coef3 = small(12)
        dcl = small(4); rcl = small(4)
        top8 = small(8); thr = small(1)
        imp = small(32); score = small(32); selt = small(32)
        ssqc = small(1); ssqc2 = small(1); rstdc = small(1)
        eps_ap = small(1); eps4_ap = small(1)

        mix_raw = Aten[:, R_mixc.base:R_mixc.base + 32768].bitcast(BF).rearrange("p (k t) -> p k t", k=8)
        R_mixc.alloc(16384, "mixT_conv")
        R_A2.alloc(16384, "mixT_nsa")
        mixT = mix_raw

        QB = [R_C.alloc(16384, "QB%d" % g).bitcast(BF)[0:96].rearrange("p (c r t) -> p c r t", c=NT, r=4) for g in range(2)]
        KE = [R_C.alloc(4096, "KE%d" % g).bitcast(BF)[0:96] for g in range(2)]
        KW = [R_C.alloc(4096, "KW%d" % g).bitcast(BF)[0:64] for g in range(2)]
        VA = R_C.alloc(4 * NT * 65 * 2, "VA").bitcast(BF).rearrange("p (i t d) -> p i t d", i=4, t=NT)
        kvT = [R_C.alloc(4096, "kvT%d" % g).bitcast(BF) for g in range(2)]
        gates = R_C.alloc(NT * 24 * 4, "gates").bitcast(F32).rearrange("p (t c) -> p t c", t=NT)

        w_in = R_A1.alloc(8 * C_END * 2, "w_in").bitcast(BF).rearrange("p (k c) -> p k c", k=8)
        xnT = R_B1.alloc(8192, "xnT").bitcast(BF).rearrange("p (k t) -> p k t", k=8)
        xt = [R_B1.alloc(4096, "xt%d" % i).bitcast(F32) for i in range(2)]
        xn = R_B1.alloc(2048, "xn").bitcast(BF)
        sq = R_B1.alloc(3072, "sq").bitcast(F32)
        qn = R_B1.alloc(3072, "qn").bitcast(F32)
        qr = R_B1.alloc(1536, "qr").bitcast(BF)
        rt1 = R_B1.alloc(1536, "rt1").bitcast(F32).rearrange("p (h i) -> p h i", h=12)
        rt2 = R_B1.alloc(1536, "rt2").bitcast(F32).rearrange("p (h i) -> p h i", h=12)
        kvst = R_B1.alloc(512, "kvst").bitcast(BF)
        hcvb = R_Bh.alloc(4 * 2078 * 2, "hcvb").bitcast(BF).rearrange("p (c t) -> p c t", c=4)
        Fg = [R_B1.alloc(2048, "Fg%d" % i).bitcast(F32) for i in range(2)]
        junk = R_B1.alloc(2048, "junk").bitcast(BF)
        diag = R_A1b.alloc(124 * 128 * 2, "diag").bitcast(BF).rearrange("p (i m) -> p i m", i=124)
        accs = [R_B1b.alloc(8192, "acc%d" % i).bitcast(F32).rearrange("p (c t) -> p c t", c=4) for i in range(2)]
        Ft = [R_B1b.alloc(2048, "F%d" % i).bitcast(F32) for i in range(6)]

        w1 = R_A2.alloc(16384, "w1").bitcast(BF).rearrange("p (l j) -> p l j", l=32)
        w2 = R_A2.alloc(512, "w2").bitcast(BF).rearrange("p (c v d) -> p c v d", c=2, v=2)
        kcTc = [R_A2.alloc(256, "kcTc%d" % g).bitcast(BF)[0:64, 0:127] for g in range(2)]
        VCa = [R_A2.alloc(256, "VCa%d" % g).bitcast(BF)[0:127, 0:97] for g in range(2)]
        PT = [R_B2.alloc(1024, "PT%d" % i).bitcast(BF) for i in range(4)]
        onsa = [R_B2.alloc(2048, "onsa%d" % i).bitcast(F32) for i in range(2)]
        otmp = [R_B2.alloc(1024, "otmp%d" % i).bitcast(F32) for i in range(2)]
        onb = R_B2.alloc(1024, "onb").bitcast(BF)
        cth_off = R_B2.base + R_B2.ptr
        cths = [R_B2.alloc(1024, "cth%d" % i).bitcast(F32)[0:127] for i in range(2)]
        chss = [R_B2.alloc(512, "chs%d" % i).bitcast(BF)[0:127] for i in range(2)]
        chsT2 = R_B2.alloc(1024, "chsT").bitcast(BF).rearrange("p (c n) -> p c n", c=4)
        kcm = R_B2.alloc(256, "kcm").bitcast(F32)[0:127]
        kcn = R_B2.alloc(256, "kcn").bitcast(F32)[0:127]
        kcb = R_B2.alloc(128, "kcb").bitcast(BF)[0:127]
        ct1 = R_B2.alloc(128, "ct1").bitcast(F32)[0:127]
        ct2 = R_B2.alloc(128, "ct2").bitcast(F32)[0:127]
        cjunk = R_B2.alloc(256, "cjunk").bitcast(F32)[0:127]

        hbuf = R_3Z.alloc(65536, "h").bitcast(F32).rearrange("p (t c) -> p t c", t=NT)
        wout = R_3Y.alloc(16384, "wout").bitcast(BF).rearrange("p (k c) -> p k c", k=8)
        hns = [R_3X.alloc(2048, "hn%d" % i).bitcast(BF) for i in range(2)]
        _wgu = [R_3Y.alloc(16384, "wgu0"), R_3X.alloc(16384, "wgu1")]
        wgu = [w_.bitcast(BF).rearrange("p (k u c) -> p k u c", k=8, u=2) for w_ in _wgu]
        _wdn = [R_3Y.alloc(8192, "wdn0"), R_3X.alloc(8192, "wdn1")]
        wdn = [w_.bitcast(BF).rearrange("p (j c) -> p j c", j=4) for w_ in _wdn]
        _act = [R_3Y.alloc(4096, "actT0"), R_3X.alloc(4096, "actT1")]
        actT = [a_.bitcast(BF).rearrange("p (j t) -> p j t", j=4) for a_ in _act]
        fth = [R_3X.alloc(2048, "fth%d" % i).bitcast(F32) for i in range(2)]
        fz = fth
        junk3 = fth[0].bitcast(BF)

        S = Sched(nc)
        toks = {}

        def tk(*key):
            t = toks.get(key)
            if t is None:
                t = toks[key] = Tok(str(key))
            return t

        defer = [None]

        def A(eng, fn, r=(), w=(), dma=False):
            pr = [t for t in r if t.name.startswith("('ps'")]
            if pr:
                r = [t for t in r if not t.name.startswith("('ps'")]
                w = list(w) + [t for t in pr if t not in w]
            if defer[0] is not None:
                defer[0].append((eng, fn, list(r), list(w), dma))
                return None
            return S.add(eng, fn, r=r, w=w, dma=dma)

        conv_q = []

        def drain(n):
            while n > 0 and conv_q:
                eng, fn, r, w, dma = conv_q.pop(0)
                S.add(eng, fn, r=r, w=w, dma=dma)
                n -= 1

        def rsqrt_pool(out, in_, n, scale, eps, tin, tout):
            tmp = in_
            A(POOL, lambda e: e.tensor_scalar(out=out, in0=in_, scalar1=scale, scalar2=eps, op0=ALU.mult, op1=ALU.add),
              r=[tin], w=[tout])
            A(POOL, lambda e: e.tensor_tensor(out=out, in0=out, in1=nh[0:out.shape[0], 0:1].to_broadcast(list(out.shape)), op=ALU.pow),
              r=[tout, tk("nh")], w=[tout])

        A(SP, lambda e: e.dma_start(out=cf, in_=cf_d), w=[tk("cf")], dma=True)
        A(SP, lambda e: e.dma_start(out=cb, in_=cb_d), w=[tk("cb")], dma=True)
        A(SP, lambda e: e.dma_start(out=gbc, in_=gba_d), w=[tk("gbc")], dma=True)
        WGRP = [(0, 512), (512, 1024), (1024, 1304), (1304, 2328)]

        def win_group(gi_):
            c0_, c1_ = WGRP[gi_]
            for k in range(8):
                A(POOL, lambda e, k=k: e.dma_start(out=w_in[:, k, c0_:c1_], in_=win_d[k * 128:(k + 1) * 128, c0_:c1_]),
                  w=[tk("w_in", gi_, k)], dma=True)
        for g in range(2):
            A(SP, lambda e, g=g: e.dma_start(out=KE[g][64:96, :], in_=etab_d), w=[tk("KEe", g)], dma=True)
        A(POOL, lambda e: e.memset(nh, -0.5), w=[tk("nh")])
        A(POOL, lambda e: e.memset(eps_ap, EPS), w=[tk("epsc")])
        A(POOL, lambda e: e.memset(eps4_ap, 4.0 * EPS), w=[tk("epsc")])
        A(POOL, lambda e: e.memset(ones, 1.0), w=[tk("ones")])
        A(POOL, lambda e: e.memset(VA[:, :, :, 64:65], 1.0), w=[tk("VAones")])
        if int(os.environ.get("TESTB", "0")) == 0:
            A(DVE, lambda e: e.memset(hcvb[:, :, 0:30], 0.0), w=[tk("hcvpad")])
        A(POOL, lambda e: e.memset(selpad, 0.0), w=[tk("selpad")])
        A(DVE, lambda e: e.tensor_scalar(out=convh, in0=convc[:, :, 32:34], scalar1=0.5, scalar2=None, op0=ALU.mult),
          r=[tk("cf")], w=[tk("convh")])
        win_group(0)
        w_in_toks = None

        ssq_p = [small(1), small(1)]
        rstd_p = [small(1), small(1)]
        def pbank(t):
            return 0 if t % 2 == 0 else 5

        def P01f(t):
            b0 = pbank(t)
            return ps[:, b0:b0 + 2, :].rearrange("p a b -> p (a b)")

        def p01f(t):
            b0 = pbank(t)
            return [tk("ps", b0), tk("ps", b0 + 1)]

        def stageA1(t):
            xti, txt = xt[t % 2], tk("xt", t % 2)
            sq_, rs_ = ssq_p[t % 2], rstd_p[t % 2]
            A(SP, lambda e: e.dma_start(out=xti, in_=x_d[t * 128:(t + 1) * 128, :]), w=[txt], dma=True)
            A(ACT, lambda e: e.activation(out=junk, in_=xti, func=AF.Square, scale=1.0 / 32.0, accum_out=sq_),
              r=[txt], w=[tk("junk"), tk("ssqp", t % 2)])
            rsqrt_pool(rs_, sq_, 1, 1.0, EPS, tk("ssqp", t % 2), tk("rstdp", t % 2))

        def stageA2a(t):
            xti, txt = xt[t % 2], tk("xt", t % 2)
            rs_ = rstd_p[t % 2]
            A(DVE, lambda e: e.scalar_tensor_tensor(out=xn, in0=xti, scalar=rs_, in1=gbc, op0=ALU.mult, op1=ALU.mult),
              r=[txt, tk("rstdp", t % 2), tk("gbc")], w=[tk("xn")])

        def stageA2b(t):
            tl = t % 4
            for k in range(8):
                A(PE, lambda e, k=k: e.transpose(out=psh(3)[:, k * 128:(k + 1) * 128], in_=xn[:, k * 128:(k + 1) * 128], identity=ident),
                  r=[tk("xn"), tk("cb")], w=[tk("ps", 3)])
            A(ACT, lambda e: e.copy(out=xnT[:, :, tl * 128:(tl + 1) * 128], in_=psh(3).rearrange("p (k t) -> p k t", k=8)),
              r=[tk("ps", 3)], w=[tk("xnT", tl)])

        def stageB(t):
            tl = t % 4
            b0 = pbank(t)
            for gi_, c0, c1 in [(0, 0, 512), (1, 512, 1024), (2, 1024, 1304)]:
                bank = b0 + gi_
                for k in range(8):
                    A(PE, lambda e, k=k, bank=bank, c0=c0, c1=c1: e.matmul(
                        out=ps[:, bank, 0:c1 - c0], lhsT=xnT[:, k, tl * 128:(tl + 1) * 128], rhs=w_in[:, k, c0:c1],
                        start=(k == 0), stop=(k == 7)),
                      r=[tk("xnT", tl), tk("w_in", gi_, k)], w=[tk("ps", bank)])

        def stageC1(t):
            P01, p01 = P01f(t), p01f(t)
            A(ACT, lambda e: e.activation(out=sq, in_=P01[:, 0:768], func=AF.Square), r=p01, w=[tk("sq")])
            A(DVE, lambda e: e.reduce_sum(out=ss12, in_=sq.rearrange("p (h d) -> p h d", d=64), axis=AX.X),
              r=[tk("sq")], w=[tk("ss12")])
            rsqrt_pool(r12, ss12, 12, 1.0 / 64.0, EPS, tk("ss12"), tk("r12"))

        def stageC2a(t):
            P01, p01 = P01f(t), p01f(t)
            b2 = pbank(t) + 2
            qn3 = qn.rearrange("p (h d) -> p h d", d=64)
            A(DVE, lambda e: e.tensor_tensor(out=qn3, in0=P01[:, 0:768].rearrange("p (h d) -> p h d", d=64), in1=bl(r12, 64), op=ALU.mult),
              r=p01 + [tk("r12")], w=[tk("qn")])
            A(ACT, lambda e: e.copy(out=kvst, in_=P01[:, 768:1024]), r=[p01[1]], w=[tk("kvst")])
            A(ACT, lambda e: e.copy(out=VA[:, :, t, 0:64], in_=ps[:, b2, 0:256].rearrange("p (i d) -> p i d", i=4)),
              r=[tk("ps", b2)], w=[tk("VA", t)])
            A(ACT, lambda e: e.activation(out=gtmp, in_=ps[:, b2, 256:280], func=AF.Tanh, scale=0.5), r=[tk("ps", b2)], w=[tk("gtmp")])
            A(DVE, lambda e: e.tensor_scalar(out=gates[:, t, :], in0=gtmp, scalar1=0.5, scalar2=0.5, op0=ALU.mult, op1=ALU.add),
              r=[tk("gtmp")], w=[tk("gates", t)])

        def stageC2b(t):
            qn3 = qn.rearrange("p (h d) -> p h d", d=64)
            qr3 = qr.rearrange("p (h d) -> p h d", d=64)
            A(DVE, lambda e: e.tensor_tensor(out=qn, in0=qn, in1=gain12, op=ALU.mult), r=[tk("qn"), tk("cf")], w=[tk("qn")])
            cb_ = bm(cos_t[:, t, :], 12)
            sb_ = bm(sin_t[:, t, :], 12)
            x1 = qn3[:, :, 0:32]
            x2 = qn3[:, :, 32:64]
            A(POOL, lambda e: e.tensor_tensor(out=rt2, in0=x2, in1=sb_, op=ALU.mult), r=[tk("qn"), tk("cf")], w=[tk("rt2")])
            A(DVE, lambda e: e.tensor_tensor(out=rt1, in0=x1, in1=cb_, op=ALU.mult), r=[tk("qn"), tk("cf")], w=[tk("rt1")])
            A(DVE, lambda e: e.tensor_tensor(out=qr3[:, :, 0:32], in0=rt1, in1=rt2, op=ALU.subtract),
              r=[tk("rt1"), tk("rt2")], w=[tk("qr")])
            A(POOL, lambda e: e.tensor_tensor(out=rt2, in0=x1, in1=sb_, op=ALU.mult), r=[tk("qn"), tk("cf")], w=[tk("rt2")])
            A(DVE, lambda e: e.tensor_tensor(out=rt1, in0=x2, in1=cb_, op=ALU.mult), r=[tk("qn"), tk("cf")], w=[tk("rt1")])
            A(DVE, lambda e: e.tensor_tensor(out=qr3[:, :, 32:64], in0=rt1, in1=rt2, op=ALU.add),
              r=[tk("rt1"), tk("rt2")], w=[tk("qr")])

        def stageC3(t):
            for h in range(8):
                A(PE, lambda e, h=h: e.transpose(out=psh(4)[0:64, h * 128:(h + 1) * 128], in_=qr[:, h * 64:(h + 1) * 64], identity=ident),
                  r=[tk("qr"), tk("cb")], w=[tk("ps", 4)])
            for g in range(2):
                A(ACT, lambda e, g=g: e.copy(out=QB[g][0:64, t, :, :], in_=psh(4)[0:64, g * 512:(g + 1) * 512].rearrange("p (r q) -> p r q", r=4)),
                  r=[tk("ps", 4)], w=[tk("QBq", g, t)])
            for i in range(4):
                A(PE, lambda e, i=i: e.transpose(out=psh(3)[0:64, i * 128:(i + 1) * 128], in_=qr[:, (8 + i) * 64:(9 + i) * 64], identity=ident),
                  r=[tk("qr"), tk("cb")], w=[tk("ps", 3)])
            for g in range(2):
                A(PE, lambda e, g=g: e.transpose(out=psh(3)[:, 512 + g * 128:512 + (g + 1) * 128], in_=kvst[:, g * 128:(g + 1) * 128], identity=ident),
                  r=[tk("kvst"), tk("cb")], w=[tk("ps", 3)])
            for g in range(2):
                A(ACT, lambda e, g=g: e.copy(out=KE[g][0:64, t * 128:(t + 1) * 128], in_=psh(3)[0:64, g * 128:(g + 1) * 128]),
                  r=[tk("ps", 3)], w=[tk("KE", g, t)])
                A(ACT, lambda e, g=g: e.copy(out=KW[g][0:64, t * 128:(t + 1) * 128], in_=psh(3)[0:64, (2 + g) * 128:(3 + g) * 128]),
                  r=[tk("ps", 3)], w=[tk("KW", g, t)])
            for g in range(2):
                A(ACT, lambda e, g=g: e.copy(out=kvT[g][:, t * 128:(t + 1) * 128], in_=psh(3)[:, 512 + g * 128:512 + (g + 1) * 128]),
                  r=[tk("ps", 3)], w=[tk("kvT", g)])

        def phase1_glu(tb):
            xr = [tk("xnT", i) for i in range(4)]
            for cc in range(4):
                ba, bg = (0, 1) if cc % 2 == 0 else (2, 4)
                for k in range(8):
                    A(PE, lambda e, k=k, cc=cc, ba=ba: e.matmul(out=psb(ba), lhsT=w_in[:, k, C_CA + cc * 128:C_CA + (cc + 1) * 128], rhs=xnT[:, k, :],
                                                                start=(k == 0), stop=(k == 7)), r=xr + [tk("w_in", 3, k)], w=[tk("ps", ba)])
                for k in range(8):
                    A(PE, lambda e, k=k, cc=cc, bg=bg: e.matmul(out=psb(bg), lhsT=w_in[:, k, C_CG + cc * 128:C_CG + (cc + 1) * 128], rhs=xnT[:, k, :],
                                                                start=(k == 0), stop=(k == 7)), r=xr + [tk("w_in", 3, k)], w=[tk("ps", bg)])
                Fi = Fg[cc % 2]
                tFi = tk("Fg", cc % 2)
                A(ACT, lambda e, Fi=Fi, bg=bg: e.activation(out=Fi, in_=psb(bg), func=AF.Tanh, scale=0.5), r=[tk("ps", bg)], w=[tFi])
                A(DVE, lambda e, Fi=Fi: e.tensor_scalar(out=Fi, in0=Fi, scalar1=0.5, scalar2=0.5, op0=ALU.mult, op1=ALU.add), r=[tFi], w=[tFi])
                _o = hcvb[:, cc, 30 + tb * 512:30 + (tb + 1) * 512]
                A(DVE, lambda e, Fi=Fi, cc=cc, _o=_o, ba=ba: e.tensor_tensor(out=_o, in0=psb(ba), in1=Fi, op=ALU.mult),
                  r=[tk("ps", ba), tFi], w=[tk("hcvb", tb)])

        def phase1b_setup():
            for i in range(124):
                cc, k = divmod(i, 31)
                eng = (ACT, DVE)[i % 2]
                if eng == ACT:
                    A(ACT, lambda e, i=i, cc=cc, k=k: e.activation(out=diag[:, i, :], in_=ident, func=AF.Copy, scale=convc[:, cc, k:k + 1]),
                      r=[tk("cb"), tk("cf")], w=[tk("diag", i)])
                else:
                    A(eng, lambda e, i=i, cc=cc, k=k: e.tensor_scalar(out=diag[:, i, :], in0=ident, scalar1=convc[:, cc, k:k + 1], scalar2=None, op0=ALU.mult),
                      r=[tk("cb"), tk("cf")], w=[tk("diag", i)])

        def conv_cc(tb, cc):
            acc = accs[tb % 2]
            hr = [tk("hcvb", tb), tk("hcvpad")] + ([tk("hcvb", tb - 1)] if tb > 0 else [])
            if True:
                bank = cc
                for k in range(31):
                    A(PE, lambda e, cc=cc, k=k, bank=bank: e.matmul(out=psb(bank), lhsT=diag[:, cc * 31 + k, :], rhs=hcvb[:, cc, tb * 512 + k:tb * 512 + k + 512],
                                                                    start=(k == 0), stop=(k == 30)),
                      r=hr + [tk("diag", cc * 31 + k)], w=[tk("ps", bank)])
                A(ACT, lambda e, cc=cc, bank=bank: e.activation(out=acc[:, cc, :], in_=psb(bank), func=AF.Identity, bias=convc[:, cc, 31:32], scale=1.0),
                  r=[tk("ps", bank), tk("cf")], w=[tk("acc", tb % 2, cc, 0), tk("acc", tb % 2, cc, 1)])

        def ln_half(tb, h):
            acc = accs[tb % 2]
            cs = slice(h * 256, (h + 1) * 256)
            tacc = [tk("acc", tb % 2, c, h) for c in range(4)]
            b1, b2 = (6, 7) if h == 0 else (4, 5)
            p1 = ps[:, b1, 0:256]
            p2 = ps[:, b2, 0:256]
            F = lambda i: Ft[i][:, cs]
            tF = lambda i: tk("F", i, h)
            mean, var, r2 = F(2), F(3), F(4)
            for cc in range(4):
                Fi, tFi = F(cc % 2), tF(cc % 2)
                A(ACT, lambda e, Fi=Fi, cc=cc: e.activation(out=Fi, in_=acc[:, cc, cs], func=AF.Square), r=[tacc[cc]], w=[tFi])
                A(PE, lambda e, cc=cc: e.matmul(out=p1, lhsT=ones, rhs=acc[:, cc, cs], start=(cc == 0), stop=(cc == 3)),
                  r=[tacc[cc], tk("ones")], w=[tk("ps", b1)])
                A(PE, lambda e, Fi=Fi, cc=cc: e.matmul(out=p2, lhsT=ones, rhs=Fi, start=(cc == 0), stop=(cc == 3)),
                  r=[tFi, tk("ones")], w=[tk("ps", b2)])
                if cc % 2 == 1:
                    yield
            A(DVE, lambda e: e.tensor_scalar(out=mean, in0=p1, scalar1=1.0 / 512.0, scalar2=None, op0=ALU.mult), r=[tk("ps", b1)], w=[tF(2)])
            A(DVE, lambda e: e.tensor_tensor(out=var, in0=mean, in1=mean, op=ALU.mult), r=[tF(2)], w=[tF(3)])
            A(DVE, lambda e: e.scalar_tensor_tensor(out=var, in0=p2, scalar=1.0 / 512.0, in1=var, op0=ALU.mult, op1=ALU.subtract),
              r=[tk("ps", b2), tF(3)], w=[tF(3)])
            yield
            A(ACT, lambda e: e.activation(out=var, in_=var, func=AF.Sqrt, bias=eps_ap, scale=1.0), r=[tF(3), tk("epsc")], w=[tF(3)])
            yield
            A(DVE, lambda e: e.reciprocal(out=var, in_=var), r=[tF(3)], w=[tF(3)])
            yield
            for cc in range(4):
                th, z = F(5), F(cc % 2)
                tth, tz = tF(5), tF(cc % 2)
                A(DVE, lambda e, cc=cc: e.tensor_tensor(out=acc[:, cc, cs], in0=acc[:, cc, cs], in1=mean, op=ALU.subtract),
                  r=[tacc[cc], tF(2)], w=[tacc[cc]])
                A(DVE, lambda e, cc=cc: e.tensor_tensor(out=acc[:, cc, cs], in0=acc[:, cc, cs], in1=var, op=ALU.mult),
                  r=[tacc[cc], tF(3)], w=[tacc[cc]])
                yield
                A(ACT, lambda e, cc=cc, th=th: e.activation(out=th, in_=acc[:, cc, cs], func=AF.Tanh, scale=convh[:, cc, 0:1], bias=convh[:, cc, 1:2]),
                  r=[tacc[cc], tk("convh")], w=[tth])
                A(DVE, lambda e, cc=cc, z=z: e.tensor_scalar(out=z, in0=acc[:, cc, cs], scalar1=convc[:, cc, 32:33], scalar2=convc[:, cc, 33:34],
                                                             op0=ALU.mult, op1=ALU.add), r=[tacc[cc], tk("cf")], w=[tz])
                yield
                A(DVE, lambda e, cc=cc, z=z, th=th: e.scalar_tensor_tensor(out=acc[:, cc, cs], in0=th, scalar=1.0, in1=z, op0=ALU.add, op1=ALU.mult),
                  r=[tth, tz], w=[tacc[cc]])
                yield
                A(ACT, lambda e, cc=cc, th=th: e.activation(out=th, in_=acc[:, cc, cs], func=AF.Square), r=[tacc[cc]], w=[tth])
                A(PE, lambda e, cc=cc, th=th: e.matmul(out=p1, lhsT=ones, rhs=th, start=(cc == 0), stop=(cc == 3)),
                  r=[tth, tk("ones")], w=[tk("ps", b1)])
                yield
            A(DVE, lambda e: e.tensor_copy(out=r2, in_=p1), r=[tk("ps", b1)], w=[tF(4)])
            yield
            A(ACT, lambda e: e.activation(out=r2, in_=r2, func=AF.Sqrt, bias=eps4_ap, scale=1.0 / 512.0), r=[tF(4), tk("epsc")], w=[tF(4)])
            yield
            A(DVE, lambda e: e.reciprocal(out=r2, in_=r2), r=[tF(4)], w=[tF(4)])
            yield
            t0 = tb * 512 + h * 256
            for cc in range(4):
                A(DVE, lambda e, cc=cc: e.scalar_tensor_tensor(out=mixT[:, cc, t0:t0 + 256], in0=acc[:, cc, cs], scalar=convc[:, cc, 34:35],
                                                               in1=r2, op0=ALU.mult, op1=ALU.mult),
                  r=[tacc[cc], tF(4), tk("cf")], w=[tk("mixT", tb * 4 + h * 2 + i) for i in range(2)])

        def ln_block(tb, fillers):
            gens = [ln_half(tb, 0), ln_half(tb, 1)]
            alive = [True, True]
            step = 0
            while any(alive):
                for i in range(2):
                    if alive[i]:
                        try:
                            next(gens[i])
                        except StopIteration:
                            alive[i] = False
                step += 1
                if fillers and step in (2, 5, 9, 13):
                    fillers.pop(0)()
            while fillers:
                fillers.pop(0)()


        stageA1(0)
        stageA1(1)
        win_group(1)
        win_group(2)
        A(POOL, lambda e: e.dma_start(out=peT, in_=pet_d), w=[tk("peT")], dma=True)
        stageA2a(0)
        stageA2b(0)
        stageB(0)
        for t in range(NT):
            if t % 4 == 3:
                phase1_glu(t // 4)
            if t + 2 < NT:
                stageA1(t + 2)
            if t == 0:
                win_group(3)
            if t + 1 < NT:
                stageA2a(t + 1)
            stageC1(t)
            if t + 1 < NT:
                stageA2b(t + 1)
            stageC2a(t)
            if t + 1 < NT:
                stageB(t + 1)
            stageC2b(t)
            stageC3(t)

        _skip = int(os.environ.get("SKIP1B", "0"))
        S.barrier()
        if _skip != 1:
            phase1b_setup()
        if _skip == 0:
            for cc in range(4):
                conv_cc(0, cc)
            for tb in range(4):
                fillers = [(lambda tb=tb, cc=cc: conv_cc(tb + 1, cc)) for cc in range(4)] if tb + 1 < 4 else []
                ln_block(tb, fillers)
        out_ops = []

        def dump(name, ap, shape, dt, rtoks):
            d = nc.dram_tensor("dbg_" + name, list(shape), dt, kind="ExternalOutput").ap()
            dbg_out[name] = d
            out_ops.append(A(SP, lambda e: e.dma_start(out=d, in_=ap), r=rtoks, dma=True))

        if dbg:
            S.barrier()
            alltoks = list(toks.values())
            for g in range(2):
                dump("QB%d" % g, QB[g][0:64].rearrange("p c r t -> p (c r t)"), [64, 8192], BF, alltoks)
                dump("KE%d" % g, KE[g], [96, 2048], BF, alltoks)
                dump("KW%d" % g, KW[g], [64, 2048], BF, alltoks)
                dump("kvT%d" % g, kvT[g], [128, 2048], BF, alltoks)
            dump("VA", VA.rearrange("p i t d -> p (i t d)"), [128, 4 * NT * 65], BF, alltoks)
            dump("gates", gates.rearrange("p t c -> p (t c)"), [128, NT * 24], F32, alltoks)
            dump("mixc", mixT[:, 0:4, :].rearrange("p k t -> p (k t)"), [128, 4 * 2048], BF, alltoks)

        _st = [0]
        _pt = [0]

        def next_st():
            _st[0] = (_st[0] + 1) % 3
            return _st[0]

        def next_pt():
            _pt[0] = (_pt[0] + 1) % 4
            return _pt[0]

        def phase2_setup():
            A(POOL, lambda e: e.dma_start(out=w1[0:64, :, :], in_=w1k_d.rearrange("(l d) j -> d l j", d=64)), w=[tk("w1", 0)], dma=True)
            A(POOL, lambda e: e.dma_start(out=w1[64:128, :, :], in_=w1v_d.rearrange("(l d) j -> d l j", d=64)), w=[tk("w1", 1)], dma=True)
            A(POOL, lambda e: e.dma_start(out=w2[:, :, 0, :], in_=w2k_d.rearrange("(c p) d -> p c d", p=128)), w=[tk("w2", 0)], dma=True)
            A(POOL, lambda e: e.dma_start(out=w2[:, :, 1, :], in_=w2v_d.rearrange("(c p) d -> p c d", p=128)), w=[tk("w2", 1)], dma=True)
            for g in range(2):
                A(SP, lambda e, g=g: e.dma_start(out=VCa[g][:, 65:97], in_=ovl_d), w=[tk("VCa", g)], dma=True)
                A(POOL, lambda e, g=g: e.memset(VCa[g][:, 64:65], 1.0), w=[tk("VCa", g)])

        def compress(g):
            banks = [7, 5]
            Hs = [ps[0:127, bk, 0:256] for bk in banks]
            KOs = [ps[0:127, bk, 256:320] for bk in banks]
            pts = [tk("ps", bk) for bk in banks]
            rows = [slice(0, 64), slice(64, 128)]
            for l in range(32):
                for kv in range(2):
                    A(PE, lambda e, l=l, kv=kv: e.matmul(out=Hs[kv], lhsT=kvT[g][rows[kv], l:l + 16 * 126 + 1:16], rhs=w1[rows[kv], l, :], start=(l == 0), stop=False),
                      r=[tk("kvT", g), tk("w1", kv)], w=[pts[kv]])
            for l in range(32):
                for kv in range(2):
                    A(PE, lambda e, l=l, kv=kv: e.matmul(out=Hs[kv], lhsT=peT[rows[kv], l:l + 1].to_broadcast([64, 127]), rhs=w1[rows[kv], l, :], start=False, stop=(l == 31)),
                      r=[tk("peT"), tk("w1", kv)], w=[pts[kv]])
            for kv in range(2):
                A(ACT, lambda e, kv=kv: e.activation(out=cths[kv], in_=Hs[kv], func=AF.Tanh, scale=0.5), r=[pts[kv]], w=[tk("cth", kv)])
                A(DVE, lambda e, kv=kv: e.scalar_tensor_tensor(out=chss[kv], in0=cths[kv], scalar=1.0, in1=Hs[kv], op0=ALU.add, op1=ALU.mult),
                  r=[tk("cth", kv), pts[kv]], w=[tk("chs", kv)])
            for kv in range(2):
                for jc in range(2):
                    A(PE, lambda e, jc=jc, kv=kv: e.transpose(out=psh(6)[:, kv * 256 + jc * 128:kv * 256 + jc * 128 + 127], in_=chss[kv][:, jc * 128:(jc + 1) * 128], identity=ident[0:127, 0:127]),
                      r=[tk("chs", kv), tk("cb")], w=[tk("ps", 6)])
            A(ACT, lambda e: e.copy(out=chsT2[:, :, 0:127], in_=psh(6)[:, 0:512].rearrange("p (c n) -> p c n", c=4)[:, :, 0:127]),
              r=[tk("ps", 6)], w=[tk("chsT")])
            for kv in range(2):
                for jc in range(2):
                    A(PE, lambda e, jc=jc, kv=kv: e.matmul(out=KOs[kv], lhsT=chsT2[:, kv * 2 + jc, 0:127], rhs=w2[:, jc, kv, :], start=(jc == 0), stop=(jc == 1)),
                      r=[tk("chsT"), tk("w2", kv)], w=[pts[kv]])
            KO = KOs[0]
            p7 = pts[0]
            A(ACT, lambda e: e.mul(out=VCa[g][:, 0:64], in_=KOs[1], mul=0.5), r=[pts[1]], w=[tk("VCa", g)])
            sc, rc_ = ssqc[0:127], rstdc[0:127]
            A(ACT, lambda e: e.mul(out=kcm, in_=KO, mul=0.5), r=[p7], w=[tk("kcm")])
            A(ACT, lambda e: e.activation(out=cjunk, in_=kcm, func=AF.Square, scale=0.125, accum_out=sc), r=[tk("kcm")], w=[tk("cjunk"), tk("ssqc")])
            rsqrt_pool(rc_, sc, 1, 1.0, EPS, tk("ssqc"), tk("rstdc"))
            A(DVE, lambda e: e.scalar_tensor_tensor(out=kcn, in0=kcm, scalar=rc_, in1=gcmp, op0=ALU.mult, op1=ALU.mult),
              r=[tk("kcm"), tk("rstdc"), tk("cf")], w=[tk("kcn")])
            x1, x2 = kcn[:, 0:32], kcn[:, 32:64]
            A(DVE, lambda e: e.tensor_tensor(out=ct1, in0=x1, in1=cosc, op=ALU.mult), r=[tk("kcn"), tk("cf")], w=[tk("ct1")])
            A(DVE, lambda e: e.tensor_tensor(out=ct2, in0=x2, in1=sinc, op=ALU.mult), r=[tk("kcn"), tk("cf")], w=[tk("ct2")])
            A(DVE, lambda e: e.tensor_tensor(out=kcb[:, 0:32], in0=ct1, in1=ct2, op=ALU.subtract), r=[tk("ct1"), tk("ct2")], w=[tk("kcb")])
            A(DVE, lambda e: e.tensor_tensor(out=ct1, in0=x2, in1=cosc, op=ALU.mult), r=[tk("kcn"), tk("cf")], w=[tk("ct1")])
            A(DVE, lambda e: e.tensor_tensor(out=ct2, in0=x1, in1=sinc, op=ALU.mult), r=[tk("kcn"), tk("cf")], w=[tk("ct2")])
            A(DVE, lambda e: e.tensor_tensor(out=kcb[:, 32:64], in0=ct1, in1=ct2, op=ALU.add), r=[tk("ct1"), tk("ct2")], w=[tk("kcb")])
            A(PE, lambda e: e.transpose(out=psh(6)[0:64, 512:639], in_=kcb, identity=ident[0:127, 0:127]), r=[tk("kcb"), tk("cb")], w=[tk("ps", 6)])
            A(ACT, lambda e: e.copy(out=kcTc[g], in_=psh(6)[0:64, 512:639]), r=[tk("ps", 6)], w=[tk("kcTc", g)])

        PIPE_D = 3
        ST_BANKS = [0, 1, 2, 7]
        pend = []
        _u = [0]
        Oc = ps[:, 3, 0:388].rearrange("p (r d) -> p r d", r=4)
        Os = ps[:, 4, 0:260].rearrange("p (r d) -> p r d", r=4)
        Ow = ps[:, 5, 0:260].rearrange("p (r d) -> p r d", r=4)
        p3, p4, p5 = tk("ps", 3), tk("ps", 4), tk("ps", 5)

        def emit_pv(u):
            pi = u["pi"]
            np_ = u["np"]
            for r_ in range(4):
                A(PE, lambda e, r_=r_, u=u, pi=pi, np_=np_: e.matmul(out=u["out"](r_), lhsT=PT[pi][0:np_, r_ * 128:(r_ + 1) * 128], rhs=u["v"],
                                                                   start=(u["first"] and r_ == 0), stop=u["last"], skip_group_check=True),
                  r=[tk("PT", pi)] + u["vtoks"], w=[u["otok"]])
            if u.get("after"):
                u["after"]()

        def pop_one():
            emit_pv(pend.pop(0))

        delayed = []

        def tick():
            for d in delayed:
                d[0] -= 1
            while delayed and delayed[0][0] <= 0:
                delayed.pop(0)[1]()

        def emit_unit(u):
            tick()
            i = _u[0]
            _u[0] += 1
            sb = ST_BANKS[i % 4]
            pi = i % 4
            u["pi"] = pi
            np_ = u["np"]
            A(PE, lambda e: e.matmul(out=ps[0:np_, sb, :], lhsT=u["k"], rhs=u["q"], start=True, stop=True), r=u["ktoks"], w=[tk("ps", sb)])
            A(ACT, lambda e: e.activation(out=PT[pi][0:np_, :], in_=ps[0:np_, sb, :], func=AF.Exp, scale=SCALE), r=[tk("ps", sb)], w=[tk("PT", pi)])
            if u.get("mask") is not None:
                pv = PT[pi][0:np_, :].rearrange("p (r q) -> p r q", r=4)
                A(POOL, lambda e: e.tensor_tensor(out=pv, in0=pv, in1=bm(u["mask"], 4), op=ALU.mult), r=[tk("PT", pi), tk("cb")], w=[tk("PT", pi)])
            pend.append(u)
            while len(pend) > PIPE_D:
                pop_one()

        def q64(c, g):
            return QB[g][0:64, c, :, :].rearrange("p r q -> p (r q)")

        def q96(c, g):
            return QB[g][0:96, c, :, :].rearrange("p r q -> p (r q)")

        dclA = small(4); rclA = small(4); coefA = small(4)
        den2 = small(8); rd2 = small(8); coef2 = small(8)

        def selection(c, g):
            A(DVE, lambda e: e.tensor_scalar(out=dclA, in0=Oc[:, :, 64], scalar1=1e-30, scalar2=None, op0=ALU.max), r=[p3], w=[tk("dclA")])
            A(DVE, lambda e: e.reciprocal(out=rclA, in_=dclA), r=[tk("dclA")], w=[tk("rclA")])
            A(DVE, lambda e: e.scalar_tensor_tensor(out=imp, in0=Oc[:, 0, 65:97], scalar=rclA[:, 0:1], in1=addc[:, c, :], op0=ALU.mult, op1=ALU.add),
              r=[p3, tk("rclA"), tk("cf")], w=[tk("imp")])
            for r_ in range(1, 4):
                A(DVE, lambda e, r_=r_: e.scalar_tensor_tensor(out=imp, in0=Oc[:, r_, 65:97], scalar=rclA[:, r_:r_ + 1], in1=imp, op0=ALU.mult, op1=ALU.add),
                  r=[p3, tk("rclA"), tk("imp")], w=[tk("imp")])
            A(DVE, lambda e: e.max(out=top8, in_=imp), r=[tk("imp")], w=[tk("top8")])
            A(DVE, lambda e: e.tensor_scalar(out=thr, in0=top8[:, 7:8], scalar1=-1.0, scalar2=None, op0=ALU.max), r=[tk("top8")], w=[tk("thr")])
            A(DVE, lambda e: e.tensor_scalar(out=selt, in0=imp, scalar1=thr, scalar2=None, op0=ALU.is_ge), r=[tk("imp"), tk("thr")], w=[tk("selt")])
            A(DVE, lambda e: e.tensor_scalar(out=selpad[:, 64:96], in0=selt, scalar1=-1.0, scalar2=BIG, op0=ALU.add, op1=ALU.mult),
              r=[tk("selt")], w=[tk("selpad")])
            gv = gates[:, c, g * 12:(g + 1) * 12].rearrange("p (r b) -> p b r", b=3)
            A(DVE, lambda e: e.tensor_tensor(out=coefA, in0=rclA, in1=gv[:, 0, :], op=ALU.mult), r=[tk("rclA"), tk("gates", c)], w=[tk("coefA")])
            on = onsa[c % 2][:, g * 256:(g + 1) * 256].rearrange("p (r d) -> p r d", r=4)
            A(DVE, lambda e: e.tensor_tensor(out=on, in0=Oc[:, :, 0:64], in1=bl(coefA, 64), op=ALU.mult), r=[p3, tk("coefA")], w=[tk("onsa", c % 2, g)])

        def bias_T(c, g):
            A(PE, lambda e: e.transpose(out=psh(6)[0:96, 0:128], in_=selpad, identity=ident), r=[tk("selpad"), tk("cb")], w=[tk("ps", 6)])
            A(DVE, lambda e: e.tensor_copy(out=QB[g][64:96, c, :, :], in_=bm(psh(6)[64:96, 0:128], 4)), r=[tk("ps", 6)], w=[tk("QBb", g, c)])

        def combineB(c, g):
            d2 = den2.rearrange("p (b r) -> p b r", b=2)
            r2_ = rd2.rearrange("p (b r) -> p b r", b=2)
            c2_ = coef2.rearrange("p (b r) -> p b r", b=2)
            A(DVE, lambda e: e.tensor_copy(out=obuf, in_=ps[:, 4:6, 0:260]), r=[p4, p5], w=[tk("obuf")])
            Os_ = obuf[:, 0, :].rearrange("p (r d) -> p r d", r=4)
            Ow_ = obuf[:, 1, :].rearrange("p (r d) -> p r d", r=4)
            A(DVE, lambda e: e.tensor_scalar(out=d2, in0=obuf.rearrange("p b (r d) -> p b r d", r=4)[:, :, :, 64], scalar1=1e-30, scalar2=None, op0=ALU.max),
              r=[tk("obuf")], w=[tk("den2")])
            A(DVE, lambda e: e.reciprocal(out=rd2, in_=den2), r=[tk("den2")], w=[tk("rd2")])
            gv = gates[:, c, g * 12:(g + 1) * 12].rearrange("p (r b) -> p b r", b=3)
            A(DVE, lambda e: e.tensor_tensor(out=c2_, in0=r2_, in1=gv[:, 1:3, :], op=ALU.mult), r=[tk("rd2"), tk("gates", c)], w=[tk("coef2")])
            on = onsa[c % 2][:, g * 256:(g + 1) * 256].rearrange("p (r d) -> p r d", r=4)
            ton = tk("onsa", c % 2, g)
            for bi, O_ in enumerate([Os_, Ow_]):
                ot = otmp[bi].rearrange("p (r d) -> p r d", r=4)
                A(DVE, lambda e, bi=bi, O_=O_, ot=ot: e.tensor_tensor(out=ot, in0=O_[:, :, 0:64], in1=bl(c2_[:, bi, :], 64), op=ALU.mult),
                  r=[tk("obuf"), tk("coef2")], w=[tk("otmp", bi)])
                A(DVE, lambda e, ot=ot: e.tensor_tensor(out=on, in0=on, in1=ot, op=ALU.add), r=[ton, tk("otmp", bi)], w=[ton])

        def cmp_unit(c, g, after):
            vca = [tk("VCa", g), tk("VCa", g), tk("VCa", g)]
            return dict(np=127, k=kcTc[g], q=q64(c, g), ktoks=[tk("kcTc", g), tk("QBq", g, c)], mask=maskc[0:127, c, :],
                        out=lambda r_: ps[:, 3, r_ * 97:(r_ + 1) * 97], v=VCa[g], vtoks=vca, otok=p3, first=True, last=True, after=after)

        def main_units(c, g, after_last):
            us = []
            k0 = max(0, c - 4)
            for kt in range(k0, c + 1):
                mi = 0 if kt == c else (1 if kt == c - 4 else None)
                us.append(dict(np=128, k=KW[g][0:64, kt * 128:(kt + 1) * 128], q=q64(c, g), ktoks=[tk("KW", g, kt), tk("QBq", g, c)],
                               mask=(dmask[:, mi, :] if mi is not None else None),
                               out=lambda r_: ps[:, 5, r_ * 65:(r_ + 1) * 65], v=VA[:, 2 + g, kt, :], vtoks=[tk("VA", kt), tk("VAones")], otok=p5,
                               first=(kt == k0), last=(kt == c)))
            for kt in range(c + 1):
                us.append(dict(np=128, k=KE[g][0:96, kt * 128:(kt + 1) * 128], q=q96(c, g),
                               ktoks=[tk("KE", g, kt), tk("KEe", g), tk("QBq", g, c), tk("QBb", g, c)],
                               mask=(dmask[:, 0, :] if kt == c else None),
                               out=lambda r_: ps[:, 4, r_ * 65:(r_ + 1) * 65], v=VA[:, g, kt, :], vtoks=[tk("VA", kt), tk("VAones")], otok=p4,
                               first=(kt == 0), last=(kt == c)))
            us[-1]["after"] = after_last
            return us

        def attention_all():
            order = [(c, g) for c in range(NT) for g in range(2)]
            state = {}

            def start_cmp(j):
                cj, gj = order[j]
                state[j] = False

                def after():
                    selection(cj, gj)
                    state[j] = True
                emit_unit(cmp_unit(cj, gj, after))

            start_cmp(0)
            while not state[0]:
                pop_one()
            bias_T(*order[0])
            for i, (c, g) in enumerate(order):
                if i + 1 < len(order):
                    start_cmp(i + 1)

                def after_last(c=c, g=g):
                    combineB(c, g)
                    if g == 1:
                        delayed.append([3, lambda c=c: attn_finish_a(c)])
                        delayed.append([8, lambda c=c: attn_finish_b(c)])
                for u in main_units(c, g, after_last):
                    emit_unit(u)
                if i + 1 < len(order):
                    while not state[i + 1]:
                        pop_one()
                    bias_T(*order[i + 1])
            while pend:
                pop_one()
            while delayed:
                delayed.pop(0)[1]()

        def attn_finish_a(c):
            o_ = onsa[c % 2]
            tt_ = [tk("onsa", c % 2, 0), tk("onsa", c % 2, 1)]
            A(ACT, lambda e: e.activation(out=junk2, in_=o_, func=AF.Square, scale=float(1.0 / np.sqrt(512.0)), accum_out=ssq), r=tt_, w=[tk("junk2"), tk("ssq")])
            rsqrt_pool(rstd, ssq, 1, 1.0, EPS, tk("ssq"), tk("rstd"))
            A(DVE, lambda e: e.scalar_tensor_tensor(out=onb, in0=o_, scalar=rstd, in1=gnsa, op0=ALU.mult, op1=ALU.mult), r=tt_ + [tk("rstd"), tk("cf")], w=[tk("onb")])

        def attn_finish_b(c):
            for k in range(4):
                A(PE, lambda e, k=k: e.transpose(out=psh(6)[:, 256 + k * 128:256 + (k + 1) * 128], in_=onb[:, k * 128:(k + 1) * 128], identity=ident),
                  r=[tk("onb"), tk("cb")], w=[tk("ps", 6)])
            A(DVE, lambda e: e.tensor_copy(out=mixT[:, 4:8, c * 128:(c + 1) * 128], in_=psh(6)[:, 256:768].rearrange("p (k t) -> p k t", k=4)),
              r=[tk("ps", 6)], w=[tk("mixT", c)])

        def ffn_weight_dmas(gi):
            j0, j1 = FFN_GROUPS[gi]
            nj = j1 - j0
            b = gi % 2
            for u in range(2):
                A(POOL, lambda e, u=u: e.dma_start(
                    out=wgu[b][:, :, u, 0:nj * 128], in_=wgu_d[:, u * FFN + j0 * 128:u * FFN + j1 * 128].rearrange("(k p) c -> p k c", p=128)),
                  w=[tk("wgu", b, u)], dma=True)
            A(POOL, lambda e: e.dma_start(out=wdn[b][:, 0:nj, :], in_=wdn_d[j0 * 128:j1 * 128, :].rearrange("(j p) c -> p j c", p=128)),
              w=[tk("wdn", b)], dma=True)

        def prefetch3():
            A(SP, lambda e: e.dma_start(out=gbc, in_=gbf_d), w=[tk("gbc")], dma=True)
            for k in range(8):
                A(POOL, lambda e, k=k: e.dma_start(out=wout[:, k, :], in_=wout_d[k * 128:(k + 1) * 128, :]), w=[tk("wout", k)], dma=True)
            ffn_weight_dmas(0)

        def phase3():
            for t in range(NT):
                A(SP, lambda e, t=t: e.dma_start(out=hbuf[:, t, :], in_=x_d[t * 128:(t + 1) * 128, :]), w=[tk("h", t)], dma=True)

            def s1(t):
                b0 = 0 if t % 2 == 0 else 4
                Pv = ps[:, b0:b0 + 2, :].rearrange("p a b -> p (a b)")
                sq_, rs_ = ssq_p[t % 2], rstd_p[t % 2]
                for cc in range(2):
                    for k in range(8):
                        A(PE, lambda e, cc=cc, k=k: e.matmul(out=psb(b0 + cc), lhsT=mixT[:, k, t * 128:(t + 1) * 128], rhs=wout[:, k, cc * 512:(cc + 1) * 512],
                                                             start=(k == 0), stop=(k == 7)), r=[tk("mixT", t), tk("wout", k)], w=[tk("ps", b0 + cc)])
                A(DVE, lambda e: e.tensor_tensor(out=hbuf[:, t, :], in0=hbuf[:, t, :], in1=Pv, op=ALU.add), r=[tk("ps", b0), tk("ps", b0 + 1)], w=[tk("h", t)])
                A(ACT, lambda e: e.activation(out=junk3, in_=hbuf[:, t, :], func=AF.Square, scale=1.0 / 32.0, accum_out=sq_), r=[tk("h", t)], w=[tk("fth", 0), tk("ssqp", t % 2)])
                rsqrt_pool(rs_, sq_, 1, 1.0, EPS, tk("ssqp", t % 2), tk("rstdp", t % 2))

            def s2(t):
                hn = hns[t % 2]
                tb_ = 2 if t % 2 == 0 else 6
                A(DVE, lambda e: e.scalar_tensor_tensor(out=hn, in0=hbuf[:, t, :], scalar=rstd_p[t % 2], in1=gbc, op0=ALU.mult, op1=ALU.mult),
                  r=[tk("h", t), tk("rstdp", t % 2), tk("gbc")], w=[tk("hn", t % 2)])
                for k in range(8):
                    A(PE, lambda e, k=k: e.transpose(out=psh(tb_)[:, k * 128:(k + 1) * 128], in_=hn[:, k * 128:(k + 1) * 128], identity=ident),
                      r=[tk("hn", t % 2), tk("cb")], w=[tk("ps", tb_)])
                A(ACT, lambda e: e.copy(out=mixT[:, :, t * 128:(t + 1) * 128], in_=psh(tb_).rearrange("p (k t) -> p k t", k=8)),
                  r=[tk("ps", tb_)], w=[tk("mixT", t)])

            s1(0)
            for t in range(NT):
                if t + 1 < NT:
                    s1(t + 1)
                s2(t)
            if dbg:
                for t in range(NT):
                    pass
            for gi, (j0, j1) in enumerate(FFN_GROUPS):
                nj = j1 - j0
                b = gi % 2
                if gi > 0:
                    ffn_weight_dmas(gi)
                last = gi == len(FFN_GROUPS) - 1
                for tb in range(4):
                    ab, tab = actT[tb % 2], tk("actT", tb % 2)
                    hT = [tk("mixT", tb * 4 + i) for i in range(4)]
                    for jj in range(nj):
                        gb, ub = (0, 1) if jj % 2 == 0 else (2, 3)
                        f = jj % 2
                        for u, bank in ((0, gb), (1, ub)):
                            for k in range(8):
                                A(PE, lambda e, u=u, bank=bank, k=k, jj=jj, tb=tb, b=b: e.matmul(
                                    out=psb(bank), lhsT=wgu[b][:, k, u, jj * 128:(jj + 1) * 128], rhs=mixT[:, k, tb * 512:(tb + 1) * 512],
                                    start=(k == 0), stop=(k == 7)), r=hT + [tk("wgu", b, u)], w=[tk("ps", bank)])
                        tf = tk("fth", f)
                        A(ACT, lambda e, f=f, gb=gb: e.activation(out=fth[f], in_=psb(gb), func=AF.Tanh, scale=0.5), r=[tk("ps", gb)], w=[tf])
                        A(DVE, lambda e, f=f, gb=gb: e.scalar_tensor_tensor(out=fz[f], in0=fth[f], scalar=1.0, in1=psb(gb), op0=ALU.add, op1=ALU.mult),
                          r=[tf, tk("ps", gb)], w=[tf])
                        A(DVE, lambda e, f=f, ub=ub, jj=jj, ab=ab: e.scalar_tensor_tensor(out=ab[:, jj, :], in0=fz[f], scalar=0.5, in1=psb(ub), op0=ALU.mult, op1=ALU.mult),
                          r=[tf, tk("ps", ub)], w=[tab])
                    for tl in range(4):
                        t = tb * 4 + tl
                        for cc in range(2):
                            yb = 4 + (tl * 2 + cc) % 4
                            for jj in range(nj):
                                A(PE, lambda e, jj=jj, tl=tl, cc=cc, yb=yb, ab=ab, b=b, nj=nj: e.matmul(
                                    out=psb(yb), lhsT=ab[:, jj, tl * 128:(tl + 1) * 128], rhs=wdn[b][:, jj, cc * 512:(cc + 1) * 512],
                                    start=(jj == 0), stop=(jj == nj - 1)), r=[tab, tk("wdn", b)], w=[tk("ps", yb)])
                            hs = hbuf[:, t, cc * 512:(cc + 1) * 512]
                            A(DVE, lambda e, hs=hs, yb=yb: e.tensor_tensor(out=hs, in0=hs, in1=psb(yb), op=ALU.add), r=[tk("h", t), tk("ps", yb)], w=[tk("h", t)])
                        if last:
                            out_ops.append(A(SP, lambda e, t=t: e.dma_start(out=y_d[t * 128:(t + 1) * 128, :], in_=hbuf[:, t, :]), r=[tk("h", t)], dma=True))

        if phases >= 2:
            S.barrier()
            junk2 = Aten[:, cth_off:cth_off + 1024].bitcast(BF)
            obuf = Aten[:, cth_off + 1024:cth_off + 1024 + 2080].bitcast(F32).rearrange("p (b n) -> p b n", b=2)
            phase2_setup()
            if phases >= 3:
                prefetch3()
            for g in range(2):
                compress(g)
            if dbg:
                for g in range(2):
                    dump("kcTc%d" % g, kcTc[g], [64, 127], BF, [tk("kcTc", g)])
                    dump("VCa%d" % g, VCa[g], [127, 97], BF, [tk("VCa", g), tk("VCa", g), tk("VCa", g)])
            attention_all()
            if dbg:
                dump("mixn", mixT[:, 4:8, :].rearrange("p k t -> p (k t)"), [128, 4 * 2048], BF, [tk("mixT", t) for t in range(NT)])
        if phases >= 3:
            S.barrier()
            phase3()

        def live(pa, pb):
            sets = {"all": {0, 1, 2, 3}, "p1a": {0}, "p1b": {1}, "p1": {0, 1}, "p2": {2}, "p12": {0, 1, 2}, "p3": {3}, "p23": {2, 3}}
            return bool(sets[pa] & sets[pb])
        for i in range(len(allocs)):
            for j in range(i + 1, len(allocs)):
                n1, o1, s1, p1 = allocs[i]
                n2, o2, s2, p2 = allocs[j]
                if live(p1, p2) and o1 < o2 + s2 and o2 < o1 + s1:
                    raise AssertionError("arena overlap %s %s" % (allocs[i], allocs[j]))

        S.emit(final_wait_ops=out_ops)
        build.stats = S.stats
    return nc, dbg_out


def _consts():
    half = 32
    inv = 10000.0 ** (-np.arange(half, dtype=np.float64) / half)
    pos = np.arange(S_LEN, dtype=np.float64)
    ang = pos[:, None] * inv[None, :]
    cos = np.cos(ang).astype(np.float32).reshape(NT, 128, 32).transpose(1, 0, 2).reshape(128, 512)
    sin = np.sin(ang).astype(np.float32).reshape(NT, 128, 32).transpose(1, 0, 2).reshape(128, 512)
    cpos = np.arange(127, dtype=np.float64) * 16 + 31
    angc = cpos[:, None] * inv[None, :]
    cosc = np.zeros((128, 32), np.float32); cosc[:127] = np.cos(angc)
    sinc = np.zeros((128, 32), np.float32); sinc[:127] = np.sin(angc)
    ql = np.arange(128)[:, None, None]
    c = np.arange(NT)[None, :, None]
    j = np.arange(32)[None, None, :]
    cur = 2 * c + (ql >= 64)
    valid = j <= cur
    forced = (j == 0) | (j == cur) | (j == cur - 1)
    addc = np.where(valid, np.where(forced, 1e6, 0.0), -2e6).astype(np.float32).reshape(128, 512)
    n = np.arange(128)[:, None, None]
    c2 = np.arange(NT)[None, :, None]
    q2 = np.arange(128)[None, None, :]
    maskc = ((16 * n + 31) <= (128 * c2 + q2)).astype(np.float32)
    maskc[127] = 0
    kl = np.arange(128)[:, None]
    qq = np.arange(128)[None, :]
    dmask = np.stack([(kl <= qq), (kl > qq)], axis=1).astype(np.float32)
    cbb = np.concatenate([np.eye(128, dtype=np.float32), maskc.reshape(128, 2048), dmask.reshape(128, 256)], axis=1).astype(ml_dtypes.bfloat16)
    etab = (np.arange(S_LEN)[None, :] // 64 == np.arange(32)[:, None]).astype(np.float32).astype(ml_dtypes.bfloat16)
    cs = np.arange(127)[:, None] * 16
    ss = np.arange(32)[None, :] * 64
    ov = np.clip(np.minimum(cs + 32, ss + 64) - np.maximum(cs, ss), 0, None) / 32.0
    ovl = ov.astype(np.float32).astype(ml_dtypes.bfloat16)
    return cos, sin, cosc, sinc, addc, cbb, etab, ovl


def _prep(inputs):
    f = lambda a: np.ascontiguousarray(np.asarray(a, dtype=np.float32))
    cos, sin, cosc, sinc, addc, cbb, etab, ovl = _consts()
    w_in = f(inputs["w_in"])[0]
    cols = np.concatenate([
        np.arange(0, 512),
        np.arange(768, 896), np.arange(1024, 1152),
        np.arange(512, 576), np.arange(640, 704), np.arange(576, 640), np.arange(704, 768),
        np.arange(896, 1024), np.arange(1152, 1280),
        np.arange(1280, 1304),
        np.arange(1304, 2328)])
    w_in_p = np.ascontiguousarray(w_in[:, cols])
    w_out = f(inputs["w_out"])[0]
    w_out_p = np.ascontiguousarray(np.concatenate([w_out[512:], w_out[:512]], axis=0))
    rep = lambda v, nrep: np.tile(f(v).reshape(-1), nrep)
    g12 = np.concatenate([rep(inputs["q_norm_g"], 8), rep(inputs["k_norm_slc_g"], 2), rep(inputs["k_norm_win_g"], 2)])
    bc = lambda v: np.ascontiguousarray(np.broadcast_to(np.asarray(v, np.float32).reshape(1, -1), (128, np.asarray(v).size)))
    convc = np.zeros((128, 4, 35), np.float32)
    dw = f(inputs["conv_dw_w"])[0, :, 0, :]
    convc[:, :, 0:31] = dw.T.reshape(4, 128, 31).transpose(1, 0, 2)
    for idx, nm in [(31, "conv_dw_b"), (32, "conv_ln_g"), (33, "conv_ln_b"), (34, "out_norm_conv_g")]:
        convc[:, :, idx] = f(inputs[nm])[0].reshape(4, 128).T
    cfb = np.concatenate([cos, sin, bc(g12), bc(f(inputs["out_norm_nsa_g"])[0]), addc, convc.reshape(128, 140), cosc, sinc,
                          bc(f(inputs["k_norm_cmp_g"])[0])], axis=1)
    assert cfb.shape == (128, CF_END), cfb.shape
    pet = np.concatenate([f(inputs["cmp_pe_k"])[0].T, f(inputs["cmp_pe_v"])[0].T], axis=0)
    shared = {
        "w_in": w_in_p, "w_out": w_out_p, "w_gu": f(inputs["w_gate_up"])[0], "w_dn": f(inputs["w_down"])[0],
        "w1k": f(inputs["cmp_w1_k"])[0], "w1v": f(inputs["cmp_w1_v"])[0], "w2k": f(inputs["cmp_w2_k"])[0], "w2v": f(inputs["cmp_w2_v"])[0],
        "cf": np.ascontiguousarray(cfb), "gbc_attn": bc(f(inputs["attn_norm_g"])[0]), "gbc_ffn": bc(f(inputs["ffn_norm_g"])[0]),
        "cb": np.ascontiguousarray(cbb), "etab": np.ascontiguousarray(etab), "ovl": np.ascontiguousarray(ovl), "pet": np.ascontiguousarray(pet),
    }
    x = f(inputs["x"])
    return [dict(shared, x=np.ascontiguousarray(x[b])) for b in range(8)]


def kernel(**inputs):
    nc, _ = build()
    in_maps = _prep(inputs)
    res = run_bass_kernel_spmd(nc, in_maps, core_ids=list(range(8)))
    return np.stack([np.asarray(r["y"], dtype=np.float32) for r in res.results], axis=0)
```

```python
import contextlib
import os
import numpy as np
import ml_dtypes
import concourse.bass as bass
import concourse.mybir as mybir
from concourse.bass_utils import run_bass_kernel_spmd

F32 = mybir.dt.float32
BF = mybir.dt.bfloat16
U8 = mybir.dt.uint8
AF = mybir.ActivationFunctionType
ALU = mybir.AluOpType
AX = mybir.AxisListType

PE, ACT, DVE, POOL, SP = "pe", "act", "dve", "pool", "sp"
ENGS = [PE, ACT, DVE, POOL, SP]

S_LEN = 2048
D = 1024
NT = 16
EPS = 1e-6
BIG = 29952.0
SCALE = 0.125
FFN = 2816
NJ = 22


class Tok:
    __slots__ = ("name", "w", "r")

    def __init__(self, name):
        self.name = name
        self.w = None
        self.r = []


class Op:
    __slots__ = ("eng", "fn", "deps", "dma", "sem", "val", "signal", "slot_prev")

    def __init__(self, eng, fn, dma):
        self.eng = eng
        self.fn = fn
        self.dma = dma
        self.deps = []
        self.sem = None
        self.val = None
        self.signal = dma
        self.slot_prev = None


class Sched:
    def __init__(self, nc, n_dma_sems=8):
        self.nc = nc
        self.ops = {e: [] for e in ENGS}
        self.n_dma_sems = n_dma_sems
        self.dma_count = {e: 0 for e in ENGS}
        self.dma_last = {}
        self.pending = {e: [] for e in ENGS}

    def add(self, eng, fn, r=(), w=(), dma=False):
        op = Op(eng, fn, dma)
        deps = []
        for t in r:
            if t.w is not None:
                deps.append((t.w, True))
        for t in w:
            if t.w is not None:
                deps.append((t.w, False))
            for rd in t.r:
                deps.append((rd, False))
        for d in self.pending[eng]:
            deps.append((d, True))
        self.pending[eng] = []
        seen = set()
        for d, raw in deps:
            if d is op or id(d) in seen:
                continue
            same = (d.eng == eng) and (not d.dma) and (not dma)
            if same and eng == PE:
                continue
            if same and not raw and int(os.environ.get("TESTC", "0")) == 1:
                continue
            seen.add(id(d))
            op.deps.append(d)
            d.signal = True
        for t in r:
            if not dma:
                t.r = [x for x in t.r if x.dma or x.eng != eng]
            t.r.append(op)
        for t in w:
            t.w = op
            t.r = []
        if dma:
            n = self.dma_count[eng]
            self.dma_count[eng] = n + 1
            key = (eng, n % self.n_dma_sems)
            op.slot_prev = self.dma_last.get(key)
            self.dma_last[key] = op
            op.sem = key
            op.val = 16 * (n // self.n_dma_sems + 1)
        self.ops[eng].append(op)
        return op

    def barrier(self):
        lasts = []
        for e in ENGS:
            for op in reversed(self.ops[e]):
                if not op.dma:
                    lasts.append(op)
                    break
        lasts += list(self.dma_last.values())
        for e in ENGS:
            self.pending[e] = self.pending[e] + lasts

    def emit(self, final_wait_ops=()):
        nc = self.nc
        with contextlib.ExitStack() as st:
            sems = {}
            for e in ENGS:
                sems[e] = st.enter_context(nc.semaphore("s_" + e))
                for k in range(self.n_dma_sems):
                    if self.dma_count[e] > k:
                        sems[(e, k)] = st.enter_context(nc.semaphore("d_%s_%d" % (e, k)))
            for e in ENGS:
                c = 0
                for op in self.ops[e]:
                    if op.dma:
                        continue
                    if op.signal:
                        c += 1
                        op.sem = e
                        op.val = c
            block = st.enter_context(nc.Block())
            engobj = {PE: block.tensor, ACT: block.scalar, DVE: block.vector, POOL: block.gpsimd, SP: block.sync}
            stats = {}
            for e in ENGS:
                ops = self.ops[e]
                if not ops and not (e == SP and final_wait_ops):
                    continue

                def body(eng, ops=ops, e=e):
                    waited = {}
                    nw = 0

                    def wait(semkey, val):
                        nonlocal nw
                        if waited.get(semkey, 0) >= val:
                            return
                        waited[semkey] = val
                        eng.wait_ge(sems[semkey], val)
                        nw += 1

                    for op in ops:
                        for d in op.deps:
                            wait(d.sem, d.val)
                        if op.dma and op.slot_prev is not None:
                            wait(op.slot_prev.sem, op.slot_prev.val)
                        ins = op.fn(eng)
                        if op.dma:
                            ins.then_inc(sems[op.sem], 16)
                        elif op.signal:
                            ins.then_inc(sems[op.sem], 1)
                    if e == SP:
                        for op in final_wait_ops:
                            wait(op.sem, op.val)
                    stats[e] = (len(ops), nw)

                engobj[e](body)
            self.stats = stats


CF_COS, CF_SIN, CF_G12, CF_GNSA, CF_ADDC, CF_CONVC, CF_COSC, CF_SINC, CF_GCMP, CF_END = (
    0, 512, 1024, 1792, 2304, 2816, 2956, 2988, 3020, 3084)
CB_ID, CB_MASKC, CB_DMASK, CB_END = 0, 128, 2176, 2432

C_Q, C_ROPEK, C_KV, C_V, C_GATE, C_CA, C_CG, C_END = 0, 512, 768, 1024, 1280, 1304, 1816, 2328

FFN_GROUPS = [(0, 4), (4, 8), (8, 12), (12, 16), (16, 20), (20, 22)]


def bl(ap, n):
    shp = list(ap.shape)
    return ap.unsqueeze(len(shp)).to_broadcast(shp + [n])


def bm(ap, n):
    shp = list(ap.shape)
    return ap.unsqueeze(1).to_broadcast([shp[0], n] + shp[1:])


def build(dbg=False, phases=3):
    nc = bass.Bass("TRN2", target_bir_lowering=False)

    def din(name, shape, dt=F32):
        return nc.dram_tensor(name, list(shape), dt, kind="ExternalInput").ap()

    x_d = din("x", [S_LEN, D])
    win_d = din("w_in", [D, C_END])
    wout_d = din("w_out", [D, D])
    wgu_d = din("w_gu", [D, 2 * FFN])
    wdn_d = din("w_dn", [FFN, D])
    w1k_d = din("w1k", [2048, 256])
    w1v_d = din("w1v", [2048, 256])
    w2k_d = din("w2k", [256, 64])
    w2v_d = din("w2v", [256, 64])
    cf_d = din("cf", [128, CF_END])
    gba_d = din("gbc_attn", [128, D])
    gbf_d = din("gbc_ffn", [128, D])
    cb_d = din("cb", [128, CB_END], BF)
    etab_d = din("etab", [32, S_LEN], BF)
    ovl_d = din("ovl", [127, 32], BF)
    pet_d = din("pet", [128, 32])
    y_d = nc.dram_tensor("y", [S_LEN, D], F32, kind="ExternalOutput").ap()
    dbg_out = {}

    with contextlib.ExitStack() as st:
        ARENA_BYTES = 206 * 1024
        Aten = st.enter_context(nc.sbuf_tensor("arena", [128, ARENA_BYTES], U8))
        ps = st.enter_context(nc.psum_tensor("ps", [128, 8, 512], F32))

        def psb(b):
            return ps[:, b, :]

        def psh(b):
            return ps[:, b, :].bitcast(BF)

        allocs = []

        class Reg:
            def __init__(self, base, size, phases):
                self.base, self.size, self.phases, self.ptr = base, size, phases, 0

            def alloc(self, nbytes, name=""):
                o = self.base + self.ptr
                self.ptr += (nbytes + 63) // 64 * 64
                assert self.ptr <= self.size, ("arena region overflow", name, self.ptr, self.size)
                allocs.append((name, o, nbytes, self.phases))
                return Aten[:, o:o + nbytes]

        SZ_CONST = 24 * 1024
        SZ_MIXC = 16384
        SZ_A = 37248 + 64
        SZ_B = 61 * 1024
        SZ_C = 67328
        o = 0
        R_const = Reg(o, SZ_CONST, "all"); o += SZ_CONST
        R_mixc = Reg(o, SZ_MIXC, "all"); o += SZ_MIXC
        baseA = o
        R_A1 = Reg(o, SZ_A, "p1a"); R_A1b = Reg(o, SZ_A, "p1b"); R_A2 = Reg(o, SZ_A, "p2"); o += SZ_A
        baseB = o
        SZ_HCVB = (4 * 2078 * 2 + 63) // 64 * 64
        R_Bh = Reg(o, SZ_HCVB, "p1")
        R_B1 = Reg(o + SZ_HCVB, SZ_B - SZ_HCVB, "p1a"); R_B1b = Reg(o + SZ_HCVB, SZ_B - SZ_HCVB, "p1b")
        B2HEAD = 17 * 1024
        R_B2 = Reg(o, B2HEAD, "p2"); o += SZ_B
        baseC = o
        R_C = Reg(o, SZ_C, "p12"); o += SZ_C
        assert o <= ARENA_BYTES, o
        R_3X = Reg(baseA + 16384, (baseB + B2HEAD) - (baseA + 16384), "p3")
        R_3Y = Reg(baseB + B2HEAD, SZ_B - B2HEAD, "p23")
        R_3Z = Reg(baseC, SZ_C, "p3")

        cf = R_const.alloc(CF_END * 4, "cf").bitcast(F32)
        gbc = R_const.alloc(D * 4, "gbc").bitcast(F32)
        cb = R_const.alloc(CB_END * 2, "cb").bitcast(BF)
        ones = R_const.alloc(512, "ones").bitcast(F32)
        peT = R_const.alloc(64, "peT").bitcast(BF)
        convh = R_const.alloc(32, "convh").bitcast(F32).rearrange("p (c k) -> p c k", c=4)
        nh = R_const.alloc(4, "nh").bitcast(F32)
        selpad = R_const.alloc(192, "selpad").bitcast(BF)
        smallf = R_const.alloc(1536, "small").bitcast(F32)

        cos_t = cf[:, CF_COS:CF_COS + 512].rearrange("p (t i) -> p t i", t=NT)
        sin_t = cf[:, CF_SIN:CF_SIN + 512].rearrange("p (t i) -> p t i", t=NT)
        gain12 = cf[:, CF_G12:CF_G12 + 768]
        gnsa = cf[:, CF_GNSA:CF_GNSA + 512]
        addc = cf[:, CF_ADDC:CF_ADDC + 512].rearrange("p (t j) -> p t j", t=NT)
        convc = cf[:, CF_CONVC:CF_CONVC + 140].rearrange("p (c k) -> p c k", c=4)
        cosc = cf[0:127, CF_COSC:CF_COSC + 32]
        sinc = cf[0:127, CF_SINC:CF_SINC + 32]
        gcmp = cf[0:127, CF_GCMP:CF_GCMP + 64]
        ident = cb[:, CB_ID:CB_ID + 128]
        maskc = cb[:, CB_MASKC:CB_MASKC + 2048].rearrange("p (t q) -> p t q", t=NT)
        dmask = cb[:, CB_DMASK:CB_DMASK + 256].rearrange("p (m q) -> p m q", m=2)

        _sp = [0]

        def small(n):
            o_ = _sp[0]
            _sp[0] += n
            assert _sp[0] <= 384
            return smallf[:, o_:o_ + n]

        ssq = small(1); ssq2 = small(1); rstd = small(1)
        ss12 = small(12); ss12b = small(12); r12 = small(12)
        gtmp = small(24)
        den3 = small(12); rd3 = small(12); coef3 = small(12)
        dcl = small(4); rcl = small(4)
        top8 = small(8); thr = small(1)
        imp = small(32); score = small(32); selt = small(32)
        ssqc = small(1); ssqc2 = small(1); rstdc = small(1)
        eps_ap = small(1); eps4_ap = small(1)

        mix_raw = Aten[:, R_mixc.base:R_mixc.base + 32768].bitcast(BF).rearrange("p (k t) -> p k t", k=8)
        R_mixc.alloc(16384, "mixT_conv")
        R_A2.alloc(16384, "mixT_nsa")
        mixT = mix_raw

        QB = [R_C.alloc(16384, "QB%d" % g).bitcast(BF)[0:96].rearrange("p (c r t) -> p c r t", c=NT, r=4) for g in range(2)]
        KE = [R_C.alloc(4096, "KE%d" % g).bitcast(BF)[0:96] for g in range(2)]
        KW = [R_C.alloc(4096, "KW%d" % g).bitcast(BF)[0:64] for g in range(2)]
        VA = R_C.alloc(4 * NT * 65 * 2, "VA").bitcast(BF).rearrange("p (i t d) -> p i t d", i=4, t=NT)
        kvT = [R_C.alloc(4096, "kvT%d" % g).bitcast(BF) for g in range(2)]
        gates = R_C.alloc(NT * 24 * 4, "gates").bitcast(F32).rearrange("p (t c) -> p t c", t=NT)

        w_in = R_A1.alloc(8 * C_END * 2, "w_in").bitcast(BF).rearrange("p (k c) -> p k c", k=8)
        xnT = R_B1.alloc(8192, "xnT").bitcast(BF).rearrange("p (k t) -> p k t", k=8)
        xt = [R_B1.alloc(4096, "xt%d" % i).bitcast(F32) for i in range(2)]
        xn = R_B1.alloc(2048, "xn").bitcast(BF)
        sq = R_B1.alloc(3072, "sq").bitcast(F32)
        qn = R_B1.alloc(3072, "qn").bitcast(F32)
        qr = R_B1.alloc(1536, "qr").bitcast(BF)
        rt1 = R_B1.alloc(1536, "rt1").bitcast(F32).rearrange("p (h i) -> p h i", h=12)
        rt2 = R_B1.alloc(1536, "rt2").bitcast(F32).rearrange("p (h i) -> p h i", h=12)
        kvst = R_B1.alloc(512, "kvst").bitcast(BF)
        hcvb = R_Bh.alloc(4 * 2078 * 2, "hcvb").bitcast(BF).rearrange("p (c t) -> p c t", c=4)
        Fg = [R_B1.alloc(2048, "Fg%d" % i).bitcast(F32) for i in range(2)]
        junk = R_B1.alloc(2048, "junk").bitcast(BF)
        diag = R_A1b.alloc(124 * 128 * 2, "diag").bitcast(BF).rearrange("p (i m) -> p i m", i=124)
        accs = [R_B1b.alloc(8192, "acc%d" % i).bitcast(F32).rearrange("p (c t) -> p c t", c=4) for i in range(2)]
        Ft = [R_B1b.alloc(2048, "F%d" % i).bitcast(F32) for i in range(6)]

        w1 = R_A2.alloc(16384, "w1").bitcast(BF).rearrange("p (l j) -> p l j", l=32)
        w2 = R_A2.alloc(512, "w2").bitcast(BF).rearrange("p (c v d) -> p c v d", c=2, v=2)
        kcTc = [R_A2.alloc(256, "kcTc%d" % g).bitcast(BF)[0:64, 0:127] for g in range(2)]
        VCa = [R_A2.alloc(256, "VCa%d" % g).bitcast(BF)[0:127, 0:97] for g in range(2)]
        PT = [R_B2.alloc(1024, "PT%d" % i).bitcast(BF) for i in range(4)]
        onsa = [R_B2.alloc(2048, "onsa%d" % i).bitcast(F32) for i in range(2)]
        otmp = [R_B2.alloc(1024, "otmp%d" % i).bitcast(F32) for i in range(2)]
        onb = R_B2.alloc(1024, "onb").bitcast(BF)
        cth_off = R_B2.base + R_B2.ptr
        cths = [R_B2.alloc(1024, "cth%d" % i).bitcast(F32)[0:127] for i in range(2)]
        chss = [R_B2.alloc(512, "chs%d" % i).bitcast(BF)[0:127] for i in range(2)]
        chsT2 = R_B2.alloc(1024, "chsT").bitcast(BF).rearrange("p (c n) -> p c n", c=4)
        kcm = R_B2.alloc(256, "kcm").bitcast(F32)[0:127]
        kcn = R_B2.alloc(256, "kcn").bitcast(F32)[0:127]
        kcb = R_B2.alloc(128, "kcb").bitcast(BF)[0:127]
        ct1 = R_B2.alloc(128, "ct1").bitcast(F32)[0:127]
        ct2 = R_B2.alloc(128, "ct2").bitcast(F32)[0:127]
        cjunk = R_B2.alloc(256, "cjunk").bitcast(F32)[0:127]

        hbuf = R_3Z.alloc(65536, "h").bitcast(F32).rearrange("p (t c) -> p t c", t=NT)
        wout = R_3Y.alloc(16384, "wout").bitcast(BF).rearrange("p (k c) -> p k c", k=8)
        hns = [R_3X.alloc(2048, "hn%d" % i).bitcast(BF) for i in range(2)]
        _wgu = [R_3Y.alloc(16384, "wgu0"), R_3X.alloc(16384, "wgu1")]
        wgu = [w_.bitcast(BF).rearrange("p (k u c) -> p k u c", k=8, u=2) for w_ in _wgu]
        _wdn = [R_3Y.alloc(8192, "wdn0"), R_3X.alloc(8192, "wdn1")]
        wdn = [w_.bitcast(BF).rearrange("p (j c) -> p j c", j=4) for w_ in _wdn]
        _act = [R_3Y.alloc(4096, "actT0"), R_3X.alloc(4096, "actT1")]
        actT = [a_.bitcast(BF).rearrange("p (j t) -> p j t", j=4) for a_ in _act]
        fth = [R_3X.alloc(2048, "fth%d" % i).bitcast(F32) for i in range(2)]
        fz = fth
        junk3 = fth[0].bitcast(BF)

        S = Sched(nc)
        toks = {}

        def tk(*key):
            t = toks.get(key)
            if t is None:
                t = toks[key] = Tok(str(key))
            return t

        defer = [None]

        def A(eng, fn, r=(), w=(), dma=False):
            pr = [t for t in r if t.name.startswith("('ps'")]
            if pr:
                r = [t for t in r if not t.name.startswith("('ps'")]
                w = list(w) + [t for t in pr if t not in w]
            if defer[0] is not None:
                defer[0].append((eng, fn, list(r), list(w), dma))
                return None
            return S.add(eng, fn, r=r, w=w, dma=dma)

        conv_q = []

        def drain(n):
            while n > 0 and conv_q:
                eng, fn, r, w, dma = conv_q.pop(0)
                S.add(eng, fn, r=r, w=w, dma=dma)
                n -= 1

        def rsqrt_pool(out, in_, n, scale, eps, tin, tout):
            tmp = in_
            A(POOL, lambda e: e.tensor_scalar(out=out, in0=in_, scalar1=scale, scalar2=eps, op0=ALU.mult, op1=ALU.add),
              r=[tin], w=[tout])
            A(POOL, lambda e: e.tensor_tensor(out=out, in0=out, in1=nh[0:out.shape[0], 0:1].to_broadcast(list(out.shape)), op=ALU.pow),
              r=[tout, tk("nh")], w=[tout])

        A(SP, lambda e: e.dma_start(out=cf, in_=cf_d), w=[tk("cf")], dma=True)
        A(SP, lambda e: e.dma_start(out=cb, in_=cb_d), w=[tk("cb")], dma=True)
        A(SP, lambda e: e.dma_start(out=gbc, in_=gba_d), w=[tk("gbc")], dma=True)
        WGRP = [(0, 512), (512, 1024), (1024, 1304), (1304, 2328)]

        def win_group(gi_):
            c0_, c1_ = WGRP[gi_]
            for k in range(8):
                A(POOL, lambda e, k=k: e.dma_start(out=w_in[:, k, c0_:c1_], in_=win_d[k * 128:(k + 1) * 128, c0_:c1_]),
                  w=[tk("w_in", gi_, k)], dma=True)
        for g in range(2):
            A(SP, lambda e, g=g: e.dma_start(out=KE[g][64:96, :], in_=etab_d), w=[tk("KEe", g)], dma=True)
        A(POOL, lambda e: e.memset(nh, -0.5), w=[tk("nh")])
        A(POOL, lambda e: e.memset(eps_ap, EPS), w=[tk("epsc")])
        A(POOL, lambda e: e.memset(eps4_ap, 4.0 * EPS), w=[tk("epsc")])
        A(POOL, lambda e: e.memset(ones, 1.0), w=[tk("ones")])
        A(POOL, lambda e: e.memset(VA[:, :, :, 64:65], 1.0), w=[tk("VAones")])
        if int(os.environ.get("TESTB", "0")) == 0:
            A(DVE, lambda e: e.memset(hcvb[:, :, 0:30], 0.0), w=[tk("hcvpad")])
        A(POOL, lambda e: e.memset(selpad, 0.0), w=[tk("selpad")])
        A(DVE, lambda e: e.tensor_scalar(out=convh, in0=convc[:, :, 32:34], scalar1=0.5, scalar2=None, op0=ALU.mult),
          r=[tk("cf")], w=[tk("convh")])
        win_group(0)
        w_in_toks = None

        ssq_p = [small(1), small(1)]
        rstd_p = [small(1), small(1)]
        def pbank(t):
            return 0 if t % 2 == 0 else 5

        def P01f(t):
            b0 = pbank(t)
            return ps[:, b0:b0 + 2, :].rearrange("p a b -> p (a b)")

        def p01f(t):
            b0 = pbank(t)
            return [tk("ps", b0), tk("ps", b0 + 1)]

        def stageA1(t):
            xti, txt = xt[t % 2], tk("xt", t % 2)
            sq_, rs_ = ssq_p[t % 2], rstd_p[t % 2]
            A(SP, lambda e: e.dma_start(out=xti, in_=x_d[t * 128:(t + 1) * 128, :]), w=[txt], dma=True)
            A(ACT, lambda e: e.activation(out=junk, in_=xti, func=AF.Square, scale=1.0 / 32.0, accum_out=sq_),
              r=[txt], w=[tk("junk"), tk("ssqp", t % 2)])
            rsqrt_pool(rs_, sq_, 1, 1.0, EPS, tk("ssqp", t % 2), tk("rstdp", t % 2))

        def stageA2a(t):
            xti, txt = xt[t % 2], tk("xt", t % 2)
            rs_ = rstd_p[t % 2]
            A(DVE, lambda e: e.scalar_tensor_tensor(out=xn, in0=xti, scalar=rs_, in1=gbc, op0=ALU.mult, op1=ALU.mult),
              r=[txt, tk("rstdp", t % 2), tk("gbc")], w=[tk("xn")])

        def stageA2b(t):
            tl = t % 4
            for k in range(8):
                A(PE, lambda e, k=k: e.transpose(out=psh(3)[:, k * 128:(k + 1) * 128], in_=xn[:, k * 128:(k + 1) * 128], identity=ident),
                  r=[tk("xn"), tk("cb")], w=[tk("ps", 3)])
            A(ACT, lambda e: e.copy(out=xnT[:, :, tl * 128:(tl + 1) * 128], in_=psh(3).rearrange("p (k t) -> p k t", k=8)),
              r=[tk("ps", 3)], w=[tk("xnT", tl)])

        def stageB(t):
            tl = t % 4
            b0 = pbank(t)
            for gi_, c0, c1 in [(0, 0, 512), (1, 512, 1024), (2, 1024, 1304)]:
                bank = b0 + gi_
                for k in range(8):
                    A(PE, lambda e, k=k, bank=bank, c0=c0, c1=c1: e.matmul(
                        out=ps[:, bank, 0:c1 - c0], lhsT=xnT[:, k, tl * 128:(tl + 1) * 128], rhs=w_in[:, k, c0:c1],
                        start=(k == 0), stop=(k == 7)),
                      r=[tk("xnT", tl), tk("w_in", gi_, k)], w=[tk("ps", bank)])

        def stageC1(t):
            P01, p01 = P01f(t), p01f(t)
            A(ACT, lambda e: e.activation(out=sq, in_=P01[:, 0:768], func=AF.Square), r=p01, w=[tk("sq")])
            A(DVE, lambda e: e.reduce_sum(out=ss12, in_=sq.rearrange("p (h d) -> p h d", d=64), axis=AX.X),
              r=[tk("sq")], w=[tk("ss12")])
            rsqrt_pool(r12, ss12, 12, 1.0 / 64.0, EPS, tk("ss12"), tk("r12"))

        def stageC2a(t):
            P01, p01 = P01f(t), p01f(t)
            b2 = pbank(t) + 2
            qn3 = qn.rearrange("p (h d) -> p h d", d=64)
            A(DVE, lambda e: e.tensor_tensor(out=qn3, in0=P01[:, 0:768].rearrange("p (h d) -> p h d", d=64), in1=bl(r12, 64), op=ALU.mult),
              r=p01 + [tk("r12")], w=[tk("qn")])
            A(ACT, lambda e: e.copy(out=kvst, in_=P01[:, 768:1024]), r=[p01[1]], w=[tk("kvst")])
            A(ACT, lambda e: e.copy(out=VA[:, :, t, 0:64], in_=ps[:, b2, 0:256].rearrange("p (i d) -> p i d", i=4)),
              r=[tk("ps", b2)], w=[tk("VA", t)])
            A(ACT, lambda e: e.activation(out=gtmp, in_=ps[:, b2, 256:280], func=AF.Tanh, scale=0.5), r=[tk("ps", b2)], w=[tk("gtmp")])
            A(DVE, lambda e: e.tensor_scalar(out=gates[:, t, :], in0=gtmp, scalar1=0.5, scalar2=0.5, op0=ALU.mult, op1=ALU.add),
              r=[tk("gtmp")], w=[tk("gates", t)])

        def stageC2b(t):
            qn3 = qn.rearrange("p (h d) -> p h d", d=64)
            qr3 = qr.rearrange("p (h d) -> p h d", d=64)
            A(DVE, lambda e: e.tensor_tensor(out=qn, in0=qn, in1=gain12, op=ALU.mult), r=[tk("qn"), tk("cf")], w=[tk("qn")])
            cb_ = bm(cos_t[:, t, :], 12)
            sb_ = bm(sin_t[:, t, :], 12)
            x1 = qn3[:, :, 0:32]
            x2 = qn3[:, :, 32:64]
            A(POOL, lambda e: e.tensor_tensor(out=rt2, in0=x2, in1=sb_, op=ALU.mult), r=[tk("qn"), tk("cf")], w=[tk("rt2")])
            A(DVE, lambda e: e.tensor_tensor(out=rt1, in0=x1, in1=cb_, op=ALU.mult), r=[tk("qn"), tk("cf")], w=[tk("rt1")])
            A(DVE, lambda e: e.tensor_tensor(out=qr3[:, :, 0:32], in0=rt1, in1=rt2, op=ALU.subtract),
              r=[tk("rt1"), tk("rt2")], w=[tk("qr")])
            A(POOL, lambda e: e.tensor_tensor(out=rt2, in0=x1, in1=sb_, op=ALU.mult), r=[tk("qn"), tk("cf")], w=[tk("rt2")])
            A(DVE, lambda e: e.tensor_tensor(out=rt1, in0=x2, in1=cb_, op=ALU.mult), r=[tk("qn"), tk("cf")], w=[tk("rt1")])
            A(DVE, lambda e: e.tensor_tensor(out=qr3[:, :, 32:64], in0=rt1, in1=rt2, op=ALU.add),
              r=[tk("rt1"), tk("rt2")], w=[tk("qr")])

        def stageC3(t):
            for h in range(8):
                A(PE, lambda e, h=h: e.transpose(out=psh(4)[0:64, h * 128:(h + 1) * 128], in_=qr[:, h * 64:(h + 1) * 64], identity=ident),
                  r=[tk("qr"), tk("cb")], w=[tk("ps", 4)])
            for g in range(2):
                A(ACT, lambda e, g=g: e.copy(out=QB[g][0:64, t, :, :], in_=psh(4)[0:64, g * 512:(g + 1) * 512].rearrange("p (r q) -> p r q", r=4)),
                  r=[tk("ps", 4)], w=[tk("QBq", g, t)])
            for i in range(4):
                A(PE, lambda e, i=i: e.transpose(out=psh(3)[0:64, i * 128:(i + 1) * 128], in_=qr[:, (8 + i) * 64:(9 + i) * 64], identity=ident),
                  r=[tk("qr"), tk("cb")], w=[tk("ps", 3)])
            for g in range(2):
                A(PE, lambda e, g=g: e.transpose(out=psh(3)[:, 512 + g * 128:512 + (g + 1) * 128], in_=kvst[:, g * 128:(g + 1) * 128], identity=ident),
                  r=[tk("kvst"), tk("cb")], w=[tk("ps", 3)])
            for g in range(2):
                A(ACT, lambda e, g=g: e.copy(out=KE[g][0:64, t * 128:(t + 1) * 128], in_=psh(3)[0:64, g * 128:(g + 1) * 128]),
                  r=[tk("ps", 3)], w=[tk("KE", g, t)])
                A(ACT, lambda e, g=g: e.copy(out=KW[g][0:64, t * 128:(t + 1) * 128], in_=psh(3)[0:64, (2 + g) * 128:(3 + g) * 128]),
                  r=[tk("ps", 3)], w=[tk("KW", g, t)])
            for g in range(2):
                A(DVE, lambda e, g=g: e.tensor_copy(out=kvT[g][:, t * 128:(t + 1) * 128], in_=psh(3)[:, 512 + g * 128:512 + (g + 1) * 128]),
                  r=[tk("ps", 3)], w=[tk("kvT", g)])

        def phase1_glu(tb):
            xr = [tk("xnT", i) for i in range(4)]
            for cc in range(4):
                ba, bg = (0, 1) if cc % 2 == 0 else (2, 4)
                for k in range(8):
                    A(PE, lambda e, k=k, cc=cc, ba=ba: e.matmul(out=psb(ba), lhsT=w_in[:, k, C_CA + cc * 128:C_CA + (cc + 1) * 128], rhs=xnT[:, k, :],
                                                                start=(k == 0), stop=(k == 7)), r=xr + [tk("w_in", 3, k)], w=[tk("ps", ba)])
                for k in range(8):
                    A(PE, lambda e, k=k, cc=cc, bg=bg: e.matmul(out=psb(bg), lhsT=w_in[:, k, C_CG + cc * 128:C_CG + (cc + 1) * 128], rhs=xnT[:, k, :],
                                                                start=(k == 0), stop=(k == 7)), r=xr + [tk("w_in", 3, k)], w=[tk("ps", bg)])
                Fi = Fg[cc % 2]
                tFi = tk("Fg", cc % 2)
                A(ACT, lambda e, Fi=Fi, bg=bg: e.activation(out=Fi, in_=psb(bg), func=AF.Tanh, scale=0.5), r=[tk("ps", bg)], w=[tFi])
                A(DVE, lambda e, Fi=Fi: e.tensor_scalar(out=Fi, in0=Fi, scalar1=0.5, scalar2=0.5, op0=ALU.mult, op1=ALU.add), r=[tFi], w=[tFi])
                _o = hcvb[:, cc, 30 + tb * 512:30 + (tb + 1) * 512]
                A(DVE, lambda e, Fi=Fi, cc=cc, _o=_o, ba=ba: e.tensor_tensor(out=_o, in0=psb(ba), in1=Fi, op=ALU.mult),
                  r=[tk("ps", ba), tFi], w=[tk("hcvb", tb)])

        def phase1b_setup():
            for i in range(124):
                cc, k = divmod(i, 31)
                eng = (ACT, DVE)[i % 2]
                if eng == ACT:
                    A(ACT, lambda e, i=i, cc=cc, k=k: e.activation(out=diag[:, i, :], in_=ident, func=AF.Copy, scale=convc[:, cc, k:k + 1]),
                      r=[tk("cb"), tk("cf")], w=[tk("diag", i)])
                else:
                    A(eng, lambda e, i=i, cc=cc, k=k: e.tensor_scalar(out=diag[:, i, :], in0=ident, scalar1=convc[:, cc, k:k + 1], scalar2=None, op0=ALU.mult),
                      r=[tk("cb"), tk("cf")], w=[tk("diag", i)])

        def conv_cc(tb, cc):
            acc = accs[tb % 2]
            hr = [tk("hcvb", tb), tk("hcvpad")] + ([tk("hcvb", tb - 1)] if tb > 0 else [])
            if True:
                bank = cc
                for k in range(31):
                    A(PE, lambda e, cc=cc, k=k, bank=bank: e.matmul(out=psb(bank), lhsT=diag[:, cc * 31 + k, :], rhs=hcvb[:, cc, tb * 512 + k:tb * 512 + k + 512],
                                                                    start=(k == 0), stop=(k == 30)),
                      r=hr + [tk("diag", cc * 31 + k)], w=[tk("ps", bank)])
                A(ACT, lambda e, cc=cc, bank=bank: e.activation(out=acc[:, cc, :], in_=psb(bank), func=AF.Identity, bias=convc[:, cc, 31:32], scale=1.0),
                  r=[tk("ps", bank), tk("cf")], w=[tk("acc", tb % 2, cc, 0), tk("acc", tb % 2, cc, 1)])

        def ln_half(tb, h):
            acc = accs[tb % 2]
            cs = slice(h * 256, (h + 1) * 256)
            tacc = [tk("acc", tb % 2, c, h) for c in range(4)]
            b1, b2 = (6, 7) if h == 0 else (4, 5)
            p1 = ps[:, b1, 0:256]
            p2 = ps[:, b2, 0:256]
            F = lambda i: Ft[i][:, cs]
            tF = lambda i: tk("F", i, h)
            mean, var, r2 = F(2), F(3), F(4)
            for cc in range(4):
                Fi, tFi = F(cc % 2), tF(cc % 2)
                A(ACT, lambda e, Fi=Fi, cc=cc: e.activation(out=Fi, in_=acc[:, cc, cs], func=AF.Square), r=[tacc[cc]], w=[tFi])
                A(PE, lambda e, cc=cc: e.matmul(out=p1, lhsT=ones, rhs=acc[:, cc, cs], start=(cc == 0), stop=(cc == 3)),
                  r=[tacc[cc], tk("ones")], w=[tk("ps", b1)])
                A(PE, lambda e, Fi=Fi, cc=cc: e.matmul(out=p2, lhsT=ones, rhs=Fi, start=(cc == 0), stop=(cc == 3)),
                  r=[tFi, tk("ones")], w=[tk("ps", b2)])
                if cc % 2 == 1:
                    yield
            A(DVE, lambda e: e.tensor_scalar(out=mean, in0=p1, scalar1=1.0 / 512.0, scalar2=None, op0=ALU.mult), r=[tk("ps", b1)], w=[tF(2)])
            A(DVE, lambda e: e.tensor_tensor(out=var, in0=mean, in1=mean, op=ALU.mult), r=[tF(2)], w=[tF(3)])
            A(DVE, lambda e: e.scalar_tensor_tensor(out=var, in0=p2, scalar=1.0 / 512.0, in1=var, op0=ALU.mult, op1=ALU.subtract),
              r=[tk("ps", b2), tF(3)], w=[tF(3)])
            yield
            A(ACT, lambda e: e.activation(out=var, in_=var, func=AF.Sqrt, bias=eps_ap, scale=1.0), r=[tF(3), tk("epsc")], w=[tF(3)])
            yield
            A(DVE, lambda e: e.reciprocal(out=var, in_=var), r=[tF(3)], w=[tF(3)])
            yield
            for cc in range(4):
                th, z = F(5), F(cc % 2)
                tth, tz = tF(5), tF(cc % 2)
                A(DVE, lambda e, cc=cc: e.tensor_tensor(out=acc[:, cc, cs], in0=acc[:, cc, cs], in1=mean, op=ALU.subtract),
                  r=[tacc[cc], tF(2)], w=[tacc[cc]])
                A(DVE, lambda e, cc=cc: e.tensor_tensor(out=acc[:, cc, cs], in0=acc[:, cc, cs], in1=var, op=ALU.mult),
                  r=[tacc[cc], tF(3)], w=[tacc[cc]])
                yield
                A(ACT, lambda e, cc=cc, th=th: e.activation(out=th, in_=acc[:, cc, cs], func=AF.Tanh, scale=convh[:, cc, 0:1], bias=convh[:, cc, 1:2]),
                  r=[tacc[cc], tk("convh")], w=[tth])
                A(DVE, lambda e, cc=cc, z=z: e.tensor_scalar(out=z, in0=acc[:, cc, cs], scalar1=convc[:, cc, 32:33], scalar2=convc[:, cc, 33:34],
                                                             op0=ALU.mult, op1=ALU.add), r=[tacc[cc], tk("cf")], w=[tz])
                yield
                A(DVE, lambda e, cc=cc, z=z, th=th: e.scalar_tensor_tensor(out=acc[:, cc, cs], in0=th, scalar=1.0, in1=z, op0=ALU.add, op1=ALU.mult),
                  r=[tth, tz], w=[tacc[cc]])
                yield
                A(ACT, lambda e, cc=cc, th=th: e.activation(out=th, in_=acc[:, cc, cs], func=AF.Square), r=[tacc[cc]], w=[tth])
                A(PE, lambda e, cc=cc, th=th: e.matmul(out=p1, lhsT=ones, rhs=th, start=(cc == 0), stop=(cc == 3)),
                  r=[tth, tk("ones")], w=[tk("ps", b1)])
                yield
            A(DVE, lambda e: e.tensor_copy(out=r2, in_=p1), r=[tk("ps", b1)], w=[tF(4)])
            yield
            A(ACT, lambda e: e.activation(out=r2, in_=r2, func=AF.Sqrt, bias=eps4_ap, scale=1.0 / 512.0), r=[tF(4), tk("epsc")], w=[tF(4)])
            yield
            A(DVE, lambda e: e.reciprocal(out=r2, in_=r2), r=[tF(4)], w=[tF(4)])
            yield
            t0 = tb * 512 + h * 256
            for cc in range(4):
                A(DVE, lambda e, cc=cc: e.scalar_tensor_tensor(out=mixT[:, cc, t0:t0 + 256], in0=acc[:, cc, cs], scalar=convc[:, cc, 34:35],
                                                               in1=r2, op0=ALU.mult, op1=ALU.mult),
                  r=[tacc[cc], tF(4), tk("cf")], w=[tk("mixT", tb * 4 + h * 2 + i) for i in range(2)])

        def ln_block(tb, fillers):
            gens = [ln_half(tb, 0), ln_half(tb, 1)]
            alive = [True, True]
            step = 0
            while any(alive):
                for i in range(2):
                    if alive[i]:
                        try:
                            next(gens[i])
                        except StopIteration:
                            alive[i] = False
                step += 1
                if fillers and step in (2, 5, 9, 13):
                    fillers.pop(0)()
            while fillers:
                fillers.pop(0)()


        stageA1(0)
        stageA1(1)
        win_group(1)
        win_group(2)
        A(POOL, lambda e: e.dma_start(out=peT, in_=pet_d), w=[tk("peT")], dma=True)
        stageA2a(0)
        stageA2b(0)
        stageB(0)
        stageC1(0)
        for t in range(NT):
            if t % 4 == 3:
                phase1_glu(t // 4)
            if t + 2 < NT:
                stageA1(t + 2)
            if t == 0:
                win_group(3)
            if t + 1 < NT:
                stageA2a(t + 1)
                stageA2b(t + 1)
            stageC2a(t)
            if t + 1 < NT:
                stageB(t + 1)
            stageC2b(t)
            if t + 1 < NT:
                stageC1(t + 1)
            stageC3(t)

        _skip = int(os.environ.get("SKIP1B", "0"))
        S.barrier()
        if _skip != 1:
            phase1b_setup()
        if _skip == 0:
            for cc in range(4):
                conv_cc(0, cc)
            for tb in range(4):
                fillers = [(lambda tb=tb, cc=cc: conv_cc(tb + 1, cc)) for cc in range(4)] if tb + 1 < 4 else []
                ln_block(tb, fillers)
        out_ops = []

        def dump(name, ap, shape, dt, rtoks):
            d = nc.dram_tensor("dbg_" + name, list(shape), dt, kind="ExternalOutput").ap()
            dbg_out[name] = d
            out_ops.append(A(SP, lambda e: e.dma_start(out=d, in_=ap), r=rtoks, dma=True))

        if dbg:
            S.barrier()
            alltoks = list(toks.values())
            for g in range(2):
                dump("QB%d" % g, QB[g][0:64].rearrange("p c r t -> p (c r t)"), [64, 8192], BF, alltoks)
                dump("KE%d" % g, KE[g], [96, 2048], BF, alltoks)
                dump("KW%d" % g, KW[g], [64, 2048], BF, alltoks)
                dump("kvT%d" % g, kvT[g], [128, 2048], BF, alltoks)
            dump("VA", VA.rearrange("p i t d -> p (i t d)"), [128, 4 * NT * 65], BF, alltoks)
            dump("gates", gates.rearrange("p t c -> p (t c)"), [128, NT * 24], F32, alltoks)
            dump("mixc", mixT[:, 0:4, :].rearrange("p k t -> p (k t)"), [128, 4 * 2048], BF, alltoks)

        _st = [0]
        _pt = [0]

        def next_st():
            _st[0] = (_st[0] + 1) % 3
            return _st[0]

        def next_pt():
            _pt[0] = (_pt[0] + 1) % 4
            return _pt[0]

        def phase2_setup():
            A(POOL, lambda e: e.dma_start(out=w1[0:64, :, :], in_=w1k_d.rearrange("(l d) j -> d l j", d=64)), w=[tk("w1", 0)], dma=True)
            A(POOL, lambda e: e.dma_start(out=w1[64:128, :, :], in_=w1v_d.rearrange("(l d) j -> d l j", d=64)), w=[tk("w1", 1)], dma=True)
            A(POOL, lambda e: e.dma_start(out=w2[:, :, 0, :], in_=w2k_d.rearrange("(c p) d -> p c d", p=128)), w=[tk("w2", 0)], dma=True)
            A(POOL, lambda e: e.dma_start(out=w2[:, :, 1, :], in_=w2v_d.rearrange("(c p) d -> p c d", p=128)), w=[tk("w2", 1)], dma=True)
            for g in range(2):
                A(SP, lambda e, g=g: e.dma_start(out=VCa[g][:, 65:97], in_=ovl_d), w=[tk("VCa", g)], dma=True)
                A(POOL, lambda e, g=g: e.memset(VCa[g][:, 64:65], 1.0), w=[tk("VCa", g)])

        def compress(g):
            banks = [7, 5]
            Hs = [ps[0:127, bk, 0:256] for bk in banks]
            KOs = [ps[0:127, bk, 256:320] for bk in banks]
            pts = [tk("ps", bk) for bk in banks]
            rows = [slice(0, 64), slice(64, 128)]
            for l in range(32):
                for kv in range(2):
                    A(PE, lambda e, l=l, kv=kv: e.matmul(out=Hs[kv], lhsT=kvT[g][rows[kv], l:l + 16 * 126 + 1:16], rhs=w1[rows[kv], l, :], start=(l == 0), stop=False),
                      r=[tk("kvT", g), tk("w1", kv)], w=[pts[kv]])
            for l in range(32):
                for kv in range(2):
                    A(PE, lambda e, l=l, kv=kv: e.matmul(out=Hs[kv], lhsT=peT[rows[kv], l:l + 1].to_broadcast([64, 127]), rhs=w1[rows[kv], l, :], start=False, stop=(l == 31)),
                      r=[tk("peT"), tk("w1", kv)], w=[pts[kv]])
            for kv in range(2):
                A(ACT, lambda e, kv=kv: e.activation(out=cths[kv], in_=Hs[kv], func=AF.Tanh, scale=0.5), r=[pts[kv]], w=[tk("cth", kv)])
                A(DVE, lambda e, kv=kv: e.scalar_tensor_tensor(out=chss[kv], in0=cths[kv], scalar=1.0, in1=Hs[kv], op0=ALU.add, op1=ALU.mult),
                  r=[tk("cth", kv), pts[kv]], w=[tk("chs", kv)])
            for kv in range(2):
                for jc in range(2):
                    A(PE, lambda e, jc=jc, kv=kv: e.transpose(out=psh(6)[:, kv * 256 + jc * 128:kv * 256 + jc * 128 + 127], in_=chss[kv][:, jc * 128:(jc + 1) * 128], identity=ident[0:127, 0:127]),
                      r=[tk("chs", kv), tk("cb")], w=[tk("ps", 6)])
            A(ACT, lambda e: e.copy(out=chsT2[:, :, 0:127], in_=psh(6)[:, 0:512].rearrange("p (c n) -> p c n", c=4)[:, :, 0:127]),
              r=[tk("ps", 6)], w=[tk("chsT")])
            for kv in range(2):
                for jc in range(2):
                    A(PE, lambda e, jc=jc, kv=kv: e.matmul(out=KOs[kv], lhsT=chsT2[:, kv * 2 + jc, 0:127], rhs=w2[:, jc, kv, :], start=(jc == 0), stop=(jc == 1)),
                      r=[tk("chsT"), tk("w2", kv)], w=[pts[kv]])
            KO = KOs[0]
            p7 = pts[0]
            A(ACT, lambda e: e.mul(out=VCa[g][:, 0:64], in_=KOs[1], mul=0.5), r=[pts[1]], w=[tk("VCa", g)])
            sc, rc_ = ssqc[0:127], rstdc[0:127]
            A(ACT, lambda e: e.mul(out=kcm, in_=KO, mul=0.5), r=[p7], w=[tk("kcm")])
            A(ACT, lambda e: e.activation(out=cjunk, in_=kcm, func=AF.Square, scale=0.125, accum_out=sc), r=[tk("kcm")], w=[tk("cjunk"), tk("ssqc")])
            rsqrt_pool(rc_, sc, 1, 1.0, EPS, tk("ssqc"), tk("rstdc"))
            A(DVE, lambda e: e.scalar_tensor_tensor(out=kcn, in0=kcm, scalar=rc_, in1=gcmp, op0=ALU.mult, op1=ALU.mult),
              r=[tk("kcm"), tk("rstdc"), tk("cf")], w=[tk("kcn")])
            x1, x2 = kcn[:, 0:32], kcn[:, 32:64]
            A(DVE, lambda e: e.tensor_tensor(out=ct1, in0=x1, in1=cosc, op=ALU.mult), r=[tk("kcn"), tk("cf")], w=[tk("ct1")])
            A(DVE, lambda e: e.tensor_tensor(out=ct2, in0=x2, in1=sinc, op=ALU.mult), r=[tk("kcn"), tk("cf")], w=[tk("ct2")])
            A(DVE, lambda e: e.tensor_tensor(out=kcb[:, 0:32], in0=ct1, in1=ct2, op=ALU.subtract), r=[tk("ct1"), tk("ct2")], w=[tk("kcb")])
            A(DVE, lambda e: e.tensor_tensor(out=ct1, in0=x2, in1=cosc, op=ALU.mult), r=[tk("kcn"), tk("cf")], w=[tk("ct1")])
            A(DVE, lambda e: e.tensor_tensor(out=ct2, in0=x1, in1=sinc, op=ALU.mult), r=[tk("kcn"), tk("cf")], w=[tk("ct2")])
            A(DVE, lambda e: e.tensor_tensor(out=kcb[:, 32:64], in0=ct1, in1=ct2, op=ALU.add), r=[tk("ct1"), tk("ct2")], w=[tk("kcb")])
            A(PE, lambda e: e.transpose(out=psh(6)[0:64, 512:639], in_=kcb, identity=ident[0:127, 0:127]), r=[tk("kcb"), tk("cb")], w=[tk("ps", 6)])
            A(ACT, lambda e: e.copy(out=kcTc[g], in_=psh(6)[0:64, 512:639]), r=[tk("ps", 6)], w=[tk("kcTc", g)])

        PIPE_D = 3
        ST_BANKS = [0, 1, 2, 7]
        pend = []
        _u = [0]
        Oc = ps[:, 3, 0:388].rearrange("p (r d) -> p r d", r=4)
        Os = ps[:, 4, 0:260].rearrange("p (r d) -> p r d", r=4)
        Ow = ps[:, 5, 0:260].rearrange("p (r d) -> p r d", r=4)
        p3, p4, p5 = tk("ps", 3), tk("ps", 4), tk("ps", 5)

        def emit_pv(u):
            pi = u["pi"]
            np_ = u["np"]
            for r_ in range(4):
                A(PE, lambda e, r_=r_, u=u, pi=pi, np_=np_: e.matmul(out=u["out"](r_), lhsT=PT[pi][0:np_, r_ * 128:(r_ + 1) * 128], rhs=u["v"],
                                                                   start=(u["first"] and r_ == 0), stop=u["last"], skip_group_check=True),
                  r=[tk("PT", pi)] + u["vtoks"], w=[u["otok"]])
            if u.get("after"):
                u["after"]()

        def pop_one():
            emit_pv(pend.pop(0))

        delayed = []

        def tick():
            for d in delayed:
                d[0] -= 1
            while delayed and delayed[0][0] <= 0:
                delayed.pop(0)[1]()

        def emit_unit(u):
            tick()
            i = _u[0]
            _u[0] += 1
            sb = ST_BANKS[i % 4]
            pi = i % 4
            u["pi"] = pi
            np_ = u["np"]
            A(PE, lambda e: e.matmul(out=ps[0:np_, sb, :], lhsT=u["k"], rhs=u["q"], start=True, stop=True), r=u["ktoks"], w=[tk("ps", sb)])
            A(ACT, lambda e: e.activation(out=PT[pi][0:np_, :], in_=ps[0:np_, sb, :], func=AF.Exp, scale=SCALE), r=[tk("ps", sb)], w=[tk("PT", pi)])
            if u.get("mask") is not None:
                pv = PT[pi][0:np_, :].rearrange("p (r q) -> p r q", r=4)
                A(POOL, lambda e: e.tensor_tensor(out=pv, in0=pv, in1=bm(u["mask"], 4), op=ALU.mult), r=[tk("PT", pi), tk("cb")], w=[tk("PT", pi)])
            pend.append(u)
            while len(pend) > PIPE_D:
                pop_one()

        def q64(c, g):
            return QB[g][0:64, c, :, :].rearrange("p r q -> p (r q)")

        def q96(c, g):
            return QB[g][0:96, c, :, :].rearrange("p r q -> p (r q)")

        dclA = small(4); rclA = small(4); coefA = small(4)
        den2 = small(8); rd2 = small(8); coef2 = small(8)

        def selection(c, g):
            A(DVE, lambda e: e.tensor_scalar(out=dclA, in0=Oc[:, :, 64], scalar1=1e-30, scalar2=None, op0=ALU.max), r=[p3], w=[tk("dclA")])
            A(DVE, lambda e: e.reciprocal(out=rclA, in_=dclA), r=[tk("dclA")], w=[tk("rclA")])
            A(DVE, lambda e: e.scalar_tensor_tensor(out=imp, in0=Oc[:, 0, 65:97], scalar=rclA[:, 0:1], in1=addc[:, c, :], op0=ALU.mult, op1=ALU.add),
              r=[p3, tk("rclA"), tk("cf")], w=[tk("imp")])
            for r_ in range(1, 4):
                A(DVE, lambda e, r_=r_: e.scalar_tensor_tensor(out=imp, in0=Oc[:, r_, 65:97], scalar=rclA[:, r_:r_ + 1], in1=imp, op0=ALU.mult, op1=ALU.add),
                  r=[p3, tk("rclA"), tk("imp")], w=[tk("imp")])
            A(DVE, lambda e: e.max(out=top8, in_=imp), r=[tk("imp")], w=[tk("top8")])
            A(DVE, lambda e: e.tensor_scalar(out=thr, in0=top8[:, 7:8], scalar1=-1.0, scalar2=None, op0=ALU.max), r=[tk("top8")], w=[tk("thr")])
            A(DVE, lambda e: e.tensor_scalar(out=selt, in0=imp, scalar1=thr, scalar2=None, op0=ALU.is_ge), r=[tk("imp"), tk("thr")], w=[tk("selt")])
            A(DVE, lambda e: e.tensor_scalar(out=selpad[:, 64:96], in0=selt, scalar1=-1.0, scalar2=BIG, op0=ALU.add, op1=ALU.mult),
              r=[tk("selt")], w=[tk("selpad")])
            gv = gates[:, c, g * 12:(g + 1) * 12].rearrange("p (r b) -> p b r", b=3)
            A(DVE, lambda e: e.tensor_tensor(out=coefA, in0=rclA, in1=gv[:, 0, :], op=ALU.mult), r=[tk("rclA"), tk("gates", c)], w=[tk("coefA")])
            on = onsa[c % 2][:, g * 256:(g + 1) * 256].rearrange("p (r d) -> p r d", r=4)
            A(DVE, lambda e: e.tensor_tensor(out=on, in0=Oc[:, :, 0:64], in1=bl(coefA, 64), op=ALU.mult), r=[p3, tk("coefA")], w=[tk("onsa", c % 2, g)])

        def bias_T(c, g):
            A(PE, lambda e: e.transpose(out=psh(6)[0:96, 0:128], in_=selpad, identity=ident), r=[tk("selpad"), tk("cb")], w=[tk("ps", 6)])
            A(DVE, lambda e: e.tensor_copy(out=QB[g][64:96, c, :, :], in_=bm(psh(6)[64:96, 0:128], 4)), r=[tk("ps", 6)], w=[tk("QBb", g, c)])

        def combineB(c, g):
            d2 = den2.rearrange("p (b r) -> p b r", b=2)
            r2_ = rd2.rearrange("p (b r) -> p b r", b=2)
            c2_ = coef2.rearrange("p (b r) -> p b r", b=2)
            A(DVE, lambda e: e.tensor_copy(out=obuf, in_=ps[:, 4:6, 0:260]), r=[p4, p5], w=[tk("obuf")])
            Os_ = obuf[:, 0, :].rearrange("p (r d) -> p r d", r=4)
            Ow_ = obuf[:, 1, :].rearrange("p (r d) -> p r d", r=4)
            A(DVE, lambda e: e.tensor_scalar(out=d2, in0=obuf.rearrange("p b (r d) -> p b r d", r=4)[:, :, :, 64], scalar1=1e-30, scalar2=None, op0=ALU.max),
              r=[tk("obuf")], w=[tk("den2")])
            A(DVE, lambda e: e.reciprocal(out=rd2, in_=den2), r=[tk("den2")], w=[tk("rd2")])
            gv = gates[:, c, g * 12:(g + 1) * 12].rearrange("p (r b) -> p b r", b=3)
            A(DVE, lambda e: e.tensor_tensor(out=c2_, in0=r2_, in1=gv[:, 1:3, :], op=ALU.mult), r=[tk("rd2"), tk("gates", c)], w=[tk("coef2")])
            on = onsa[c % 2][:, g * 256:(g + 1) * 256].rearrange("p (r d) -> p r d", r=4)
            ton = tk("onsa", c % 2, g)
            for bi, O_ in enumerate([Os_, Ow_]):
                ot = otmp[bi].rearrange("p (r d) -> p r d", r=4)
                A(DVE, lambda e, bi=bi, O_=O_, ot=ot: e.tensor_tensor(out=ot, in0=O_[:, :, 0:64], in1=bl(c2_[:, bi, :], 64), op=ALU.mult),
                  r=[tk("obuf"), tk("coef2")], w=[tk("otmp", bi)])
                A(DVE, lambda e, ot=ot: e.tensor_tensor(out=on, in0=on, in1=ot, op=ALU.add), r=[ton, tk("otmp", bi)], w=[ton])

        def cmp_unit(c, g, after):
            vca = [tk("VCa", g), tk("VCa", g), tk("VCa", g)]
            return dict(np=127, k=kcTc[g], q=q64(c, g), ktoks=[tk("kcTc", g), tk("QBq", g, c)], mask=maskc[0:127, c, :],
                        out=lambda r_: ps[:, 3, r_ * 97:(r_ + 1) * 97], v=VCa[g], vtoks=vca, otok=p3, first=True, last=True, after=after)

        def main_units(c, g, after_last):
            us = []
            k0 = max(0, c - 4)
            for kt in range(k0, c + 1):
                mi = 0 if kt == c else (1 if kt == c - 4 else None)
                us.append(dict(np=128, k=KW[g][0:64, kt * 128:(kt + 1) * 128], q=q64(c, g), ktoks=[tk("KW", g, kt), tk("QBq", g, c)],
                               mask=(dmask[:, mi, :] if mi is not None else None),
                               out=lambda r_: ps[:, 5, r_ * 65:(r_ + 1) * 65], v=VA[:, 2 + g, kt, :], vtoks=[tk("VA", kt), tk("VAones")], otok=p5,
                               first=(kt == k0), last=(kt == c)))
            for kt in range(c + 1):
                us.append(dict(np=128, k=KE[g][0:96, kt * 128:(kt + 1) * 128], q=q96(c, g),
                               ktoks=[tk("KE", g, kt), tk("KEe", g), tk("QBq", g, c), tk("QBb", g, c)],
                               mask=(dmask[:, 0, :] if kt == c else None),
                               out=lambda r_: ps[:, 4, r_ * 65:(r_ + 1) * 65], v=VA[:, g, kt, :], vtoks=[tk("VA", kt), tk("VAones")], otok=p4,
                               first=(kt == 0), last=(kt == c)))
            us[-1]["after"] = after_last
            return us

        def attention_all():
            order = [(c, g) for c in range(NT) for g in range(2)]
            state = {}

            def start_cmp(j):
                cj, gj = order[j]
                state[j] = False

                def after():
                    selection(cj, gj)
                    state[j] = True
                emit_unit(cmp_unit(cj, gj, after))

            start_cmp(0)
            while not state[0]:
                pop_one()
            bias_T(*order[0])
            for i, (c, g) in enumerate(order):
                if i + 1 < len(order):
                    start_cmp(i + 1)

                def after_last(c=c, g=g):
                    combineB(c, g)
                    if g == 1:
                        delayed.append([3, lambda c=c: attn_finish_a(c)])
                        delayed.append([8, lambda c=c: attn_finish_b(c)])
                for u in main_units(c, g, after_last):
                    emit_unit(u)
                if i + 1 < len(order):
                    while not state[i + 1]:
                        pop_one()
                    bias_T(*order[i + 1])
            while pend:
                pop_one()
            while delayed:
                delayed.pop(0)[1]()

        def attn_finish_a(c):
            o_ = onsa[c % 2]
            tt_ = [tk("onsa", c % 2, 0), tk("onsa", c % 2, 1)]
            A(ACT, lambda e: e.activation(out=junk2, in_=o_, func=AF.Square, scale=float(1.0 / np.sqrt(512.0)), accum_out=ssq), r=tt_, w=[tk("junk2"), tk("ssq")])
            rsqrt_pool(rstd, ssq, 1, 1.0, EPS, tk("ssq"), tk("rstd"))
            A(DVE, lambda e: e.scalar_tensor_tensor(out=onb, in0=o_, scalar=rstd, in1=gnsa, op0=ALU.mult, op1=ALU.mult), r=tt_ + [tk("rstd"), tk("cf")], w=[tk("onb")])

        def attn_finish_b(c):
            for k in range(4):
                A(PE, lambda e, k=k: e.transpose(out=psh(6)[:, 256 + k * 128:256 + (k + 1) * 128], in_=onb[:, k * 128:(k + 1) * 128], identity=ident),
                  r=[tk("onb"), tk("cb")], w=[tk("ps", 6)])
            A(DVE, lambda e: e.tensor_copy(out=mixT[:, 4:8, c * 128:(c + 1) * 128], in_=psh(6)[:, 256:768].rearrange("p (k t) -> p k t", k=4)),
              r=[tk("ps", 6)], w=[tk("mixT", c)])

        def ffn_weight_dmas(gi):
            j0, j1 = FFN_GROUPS[gi]
            nj = j1 - j0
            b = gi % 2
            for u in range(2):
                A(POOL, lambda e, u=u: e.dma_start(
                    out=wgu[b][:, :, u, 0:nj * 128], in_=wgu_d[:, u * FFN + j0 * 128:u * FFN + j1 * 128].rearrange("(k p) c -> p k c", p=128)),
                  w=[tk("wgu", b, u)], dma=True)
            A(POOL, lambda e: e.dma_start(out=wdn[b][:, 0:nj, :], in_=wdn_d[j0 * 128:j1 * 128, :].rearrange("(j p) c -> p j c", p=128)),
              w=[tk("wdn", b)], dma=True)

        def prefetch3():
            A(SP, lambda e: e.dma_start(out=gbc, in_=gbf_d), w=[tk("gbc")], dma=True)
            for k in range(8):
                A(POOL, lambda e, k=k: e.dma_start(out=wout[:, k, :], in_=wout_d[k * 128:(k + 1) * 128, :]), w=[tk("wout", k)], dma=True)
            ffn_weight_dmas(0)

        def phase3():
            for t in range(NT):
                A(SP, lambda e, t=t: e.dma_start(out=hbuf[:, t, :], in_=x_d[t * 128:(t + 1) * 128, :]), w=[tk("h", t)], dma=True)

            def s1(t):
                b0 = 0 if t % 2 == 0 else 4
                Pv = ps[:, b0:b0 + 2, :].rearrange("p a b -> p (a b)")
                sq_, rs_ = ssq_p[t % 2], rstd_p[t % 2]
                for cc in range(2):
                    for k in range(8):
                        A(PE, lambda e, cc=cc, k=k: e.matmul(out=psb(b0 + cc), lhsT=mixT[:, k, t * 128:(t + 1) * 128], rhs=wout[:, k, cc * 512:(cc + 1) * 512],
                                                             start=(k == 0), stop=(k == 7)), r=[tk("mixT", t), tk("wout", k)], w=[tk("ps", b0 + cc)])
                A(DVE, lambda e: e.tensor_tensor(out=hbuf[:, t, :], in0=hbuf[:, t, :], in1=Pv, op=ALU.add), r=[tk("ps", b0), tk("ps", b0 + 1)], w=[tk("h", t)])
                A(ACT, lambda e: e.activation(out=junk3, in_=hbuf[:, t, :], func=AF.Square, scale=1.0 / 32.0, accum_out=sq_), r=[tk("h", t)], w=[tk("fth", 0), tk("ssqp", t % 2)])
                rsqrt_pool(rs_, sq_, 1, 1.0, EPS, tk("ssqp", t % 2), tk("rstdp", t % 2))

            def s2(t):
                hn = hns[t % 2]
                tb_ = 2 if t % 2 == 0 else 6
                A(DVE, lambda e: e.scalar_tensor_tensor(out=hn, in0=hbuf[:, t, :], scalar=rstd_p[t % 2], in1=gbc, op0=ALU.mult, op1=ALU.mult),
                  r=[tk("h", t), tk("rstdp", t % 2), tk("gbc")], w=[tk("hn", t % 2)])
                for k in range(8):
                    A(PE, lambda e, k=k: e.transpose(out=psh(tb_)[:, k * 128:(k + 1) * 128], in_=hn[:, k * 128:(k + 1) * 128], identity=ident),
                      r=[tk("hn", t % 2), tk("cb")], w=[tk("ps", tb_)])
                A(ACT, lambda e: e.copy(out=mixT[:, :, t * 128:(t + 1) * 128], in_=psh(tb_).rearrange("p (k t) -> p k t", k=8)),
                  r=[tk("ps", tb_)], w=[tk("mixT", t)])

            s1(0)
            for t in range(NT):
                if t + 1 < NT:
                    s1(t + 1)
                s2(t)
            if dbg:
                for t in range(NT):
                    pass
            for gi, (j0, j1) in enumerate(FFN_GROUPS):
                nj = j1 - j0
                b = gi % 2
                if gi > 0:
                    ffn_weight_dmas(gi)
                last = gi == len(FFN_GROUPS) - 1
                for tb in range(4):
                    ab, tab = actT[tb % 2], tk("actT", tb % 2)
                    hT = [tk("mixT", tb * 4 + i) for i in range(4)]
                    for jj in range(nj):
                        gb, ub = (0, 1) if jj % 2 == 0 else (2, 3)
                        f = jj % 2
                        for u, bank in ((0, gb), (1, ub)):
                            for k in range(8):
                                A(PE, lambda e, u=u, bank=bank, k=k, jj=jj, tb=tb, b=b: e.matmul(
                                    out=psb(bank), lhsT=wgu[b][:, k, u, jj * 128:(jj + 1) * 128], rhs=mixT[:, k, tb * 512:(tb + 1) * 512],
                                    start=(k == 0), stop=(k == 7)), r=hT + [tk("wgu", b, u)], w=[tk("ps", bank)])
                        tf = tk("fth", f)
                        A(ACT, lambda e, f=f, gb=gb: e.activation(out=fth[f], in_=psb(gb), func=AF.Tanh, scale=0.5), r=[tk("ps", gb)], w=[tf])
                        A(DVE, lambda e, f=f, gb=gb: e.scalar_tensor_tensor(out=fz[f], in0=fth[f], scalar=1.0, in1=psb(gb), op0=ALU.add, op1=ALU.mult),
                          r=[tf, tk("ps", gb)], w=[tf])
                        A(DVE, lambda e, f=f, ub=ub, jj=jj, ab=ab: e.scalar_tensor_tensor(out=ab[:, jj, :], in0=fz[f], scalar=0.5, in1=psb(ub), op0=ALU.mult, op1=ALU.mult),
                          r=[tf, tk("ps", ub)], w=[tab])
                    for tl in range(4):
                        t = tb * 4 + tl
                        for cc in range(2):
                            yb = 4 + (tl * 2 + cc) % 4
                            for jj in range(nj):
                                A(PE, lambda e, jj=jj, tl=tl, cc=cc, yb=yb, ab=ab, b=b, nj=nj: e.matmul(
                                    out=psb(yb), lhsT=ab[:, jj, tl * 128:(tl + 1) * 128], rhs=wdn[b][:, jj, cc * 512:(cc + 1) * 512],
                                    start=(jj == 0), stop=(jj == nj - 1)), r=[tab, tk("wdn", b)], w=[tk("ps", yb)])
                            hs = hbuf[:, t, cc * 512:(cc + 1) * 512]
                            A(DVE, lambda e, hs=hs, yb=yb: e.tensor_tensor(out=hs, in0=hs, in1=psb(yb), op=ALU.add), r=[tk("h", t), tk("ps", yb)], w=[tk("h", t)])
                        if last:
                            out_ops.append(A(SP, lambda e, t=t: e.dma_start(out=y_d[t * 128:(t + 1) * 128, :], in_=hbuf[:, t, :]), r=[tk("h", t)], dma=True))

        if phases >= 2:
            S.barrier()
            junk2 = Aten[:, cth_off:cth_off + 1024].bitcast(BF)
            obuf = Aten[:, cth_off + 1024:cth_off + 1024 + 2080].bitcast(F32).rearrange("p (b n) -> p b n", b=2)
            phase2_setup()
            if phases >= 3:
                prefetch3()
            for g in range(2):
                compress(g)
            if dbg:
                for g in range(2):
                    dump("kcTc%d" % g, kcTc[g], [64, 127], BF, [tk("kcTc", g)])
                    dump("VCa%d" % g, VCa[g], [127, 97], BF, [tk("VCa", g), tk("VCa", g), tk("VCa", g)])
            attention_all()
            if dbg:
                dump("mixn", mixT[:, 4:8, :].rearrange("p k t -> p (k t)"), [128, 4 * 2048], BF, [tk("mixT", t) for t in range(NT)])
        if phases >= 3:
            S.barrier()
            phase3()

        def live(pa, pb):
            sets = {"all": {0, 1, 2, 3}, "p1a": {0}, "p1b": {1}, "p1": {0, 1}, "p2": {2}, "p12": {0, 1, 2}, "p3": {3}, "p23": {2, 3}}
            return bool(sets[pa] & sets[pb])
        for i in range(len(allocs)):
            for j in range(i + 1, len(allocs)):
                n1, o1, s1, p1 = allocs[i]
                n2, o2, s2, p2 = allocs[j]
                if live(p1, p2) and o1 < o2 + s2 and o2 < o1 + s1:
                    raise AssertionError("arena overlap %s %s" % (allocs[i], allocs[j]))

        S.emit(final_wait_ops=out_ops)
        build.stats = S.stats
    return nc, dbg_out


def _consts():
    half = 32
    inv = 10000.0 ** (-np.arange(half, dtype=np.float64) / half)
    pos = np.arange(S_LEN, dtype=np.float64)
    ang = pos[:, None] * inv[None, :]
    cos = np.cos(ang).astype(np.float32).reshape(NT, 128, 32).transpose(1, 0, 2).reshape(128, 512)
    sin = np.sin(ang).astype(np.float32).reshape(NT, 128, 32).transpose(1, 0, 2).reshape(128, 512)
    cpos = np.arange(127, dtype=np.float64) * 16 + 31
    angc = cpos[:, None] * inv[None, :]
    cosc = np.zeros((128, 32), np.float32); cosc[:127] = np.cos(angc)
    sinc = np.zeros((128, 32), np.float32); sinc[:127] = np.sin(angc)
    ql = np.arange(128)[:, None, None]
    c = np.arange(NT)[None, :, None]
    j = np.arange(32)[None, None, :]
    cur = 2 * c + (ql >= 64)
    valid = j <= cur
    forced = (j == 0) | (j == cur) | (j == cur - 1)
    addc = np.where(valid, np.where(forced, 1e6, 0.0), -2e6).astype(np.float32).reshape(128, 512)
    n = np.arange(128)[:, None, None]
    c2 = np.arange(NT)[None, :, None]
    q2 = np.arange(128)[None, None, :]
    maskc = ((16 * n + 31) <= (128 * c2 + q2)).astype(np.float32)
    maskc[127] = 0
    kl = np.arange(128)[:, None]
    qq = np.arange(128)[None, :]
    dmask = np.stack([(kl <= qq), (kl > qq)], axis=1).astype(np.float32)
    cbb = np.concatenate([np.eye(128, dtype=np.float32), maskc.reshape(128, 2048), dmask.reshape(128, 256)], axis=1).astype(ml_dtypes.bfloat16)
    etab = (np.arange(S_LEN)[None, :] // 64 == np.arange(32)[:, None]).astype(np.float32).astype(ml_dtypes.bfloat16)
    cs = np.arange(127)[:, None] * 16
    ss = np.arange(32)[None, :] * 64
    ov = np.clip(np.minimum(cs + 32, ss + 64) - np.maximum(cs, ss), 0, None) / 32.0
    ovl = ov.astype(np.float32).astype(ml_dtypes.bfloat16)
    return cos, sin, cosc, sinc, addc, cbb, etab, ovl


def _prep(inputs):
    f = lambda a: np.ascontiguousarray(np.asarray(a, dtype=np.float32))
    cos, sin, cosc, sinc, addc, cbb, etab, ovl = _consts()
    w_in = f(inputs["w_in"])[0]
    cols = np.concatenate([
        np.arange(0, 512),
        np.arange(768, 896), np.arange(1024, 1152),
        np.arange(512, 576), np.arange(640, 704), np.arange(576, 640), np.arange(704, 768),
        np.arange(896, 1024), np.arange(1152, 1280),
        np.arange(1280, 1304),
        np.arange(1304, 2328)])
    w_in_p = np.ascontiguousarray(w_in[:, cols])
    w_out = f(inputs["w_out"])[0]
    w_out_p = np.ascontiguousarray(np.concatenate([w_out[512:], w_out[:512]], axis=0))
    rep = lambda v, nrep: np.tile(f(v).reshape(-1), nrep)
    g12 = np.concatenate([rep(inputs["q_norm_g"], 8), rep(inputs["k_norm_slc_g"], 2), rep(inputs["k_norm_win_g"], 2)])
    bc = lambda v: np.ascontiguousarray(np.broadcast_to(np.asarray(v, np.float32).reshape(1, -1), (128, np.asarray(v).size)))
    convc = np.zeros((128, 4, 35), np.float32)
    dw = f(inputs["conv_dw_w"])[0, :, 0, :]
    convc[:, :, 0:31] = dw.T.reshape(4, 128, 31).transpose(1, 0, 2)
    for idx, nm in [(31, "conv_dw_b"), (32, "conv_ln_g"), (33, "conv_ln_b"), (34, "out_norm_conv_g")]:
        convc[:, :, idx] = f(inputs[nm])[0].reshape(4, 128).T
    cfb = np.concatenate([cos, sin, bc(g12), bc(f(inputs["out_norm_nsa_g"])[0]), addc, convc.reshape(128, 140), cosc, sinc,
                          bc(f(inputs["k_norm_cmp_g"])[0])], axis=1)
    assert cfb.shape == (128, CF_END), cfb.shape
    pet = np.concatenate([f(inputs["cmp_pe_k"])[0].T, f(inputs["cmp_pe_v"])[0].T], axis=0)
    shared = {
        "w_in": w_in_p, "w_out": w_out_p, "w_gu": f(inputs["w_gate_up"])[0], "w_dn": f(inputs["w_down"])[0],
        "w1k": f(inputs["cmp_w1_k"])[0], "w1v": f(inputs["cmp_w1_v"])[0], "w2k": f(inputs["cmp_w2_k"])[0], "w2v": f(inputs["cmp_w2_v"])[0],
        "cf": np.ascontiguousarray(cfb), "gbc_attn": bc(f(inputs["attn_norm_g"])[0]), "gbc_ffn": bc(f(inputs["ffn_norm_g"])[0]),
        "cb": np.ascontiguousarray(cbb), "etab": np.ascontiguousarray(etab), "ovl": np.ascontiguousarray(ovl), "pet": np.ascontiguousarray(pet),
    }
    x = f(inputs["x"])
    return [dict(shared, x=np.ascontiguousarray(x[b])) for b in range(8)]


def kernel(**inputs):
    nc, _ = build()
    in_maps = _prep(inputs)
    res = run_bass_kernel_spmd(nc, in_maps, core_ids=list(range(8)))
    return np.stack([np.asarray(r["y"], dtype=np.float32) for r in res.results], axis=0)
```

```python
import contextlib
import os
import numpy as np
import ml_dtypes
import concourse.bass as bass
import concourse.mybir as mybir
from concourse.bass_utils import run_bass_kernel_spmd

F32 = mybir.dt.float32
BF = mybir.dt.bfloat16
U8 = mybir.dt.uint8
AF = mybir.ActivationFunctionType
ALU = mybir.AluOpType
AX = mybir.AxisListType

PE, ACT, DVE, POOL, SP = "pe", "act", "dve", "pool", "sp"
ENGS = [PE, ACT, DVE, POOL, SP]

S_LEN = 2048
D = 1024
NT = 16
EPS = 1e-6
BIG = 29952.0
SCALE = 0.125
FFN = 2816
NJ = 22


class Tok:
    __slots__ = ("name", "w", "r")

    def __init__(self, name):
        self.name = name
        self.w = None
        self.r = []


class Op:
    __slots__ = ("eng", "fn", "deps", "dma", "sem", "val", "signal", "slot_prev")

    def __init__(self, eng, fn, dma):
        self.eng = eng
        self.fn = fn
        self.dma = dma
        self.deps = []
        self.sem = None
        self.val = None
        self.signal = dma
        self.slot_prev = None


class Sched:
    def __init__(self, nc, n_dma_sems=8):
        self.nc = nc
        self.ops = {e: [] for e in ENGS}
        self.n_dma_sems = n_dma_sems
        self.dma_count = {e: 0 for e in ENGS}
        self.dma_last = {}
        self.pending = {e: [] for e in ENGS}

    def add(self, eng, fn, r=(), w=(), dma=False):
        op = Op(eng, fn, dma)
        deps = []
        for t in r:
            if t.w is not None:
                deps.append((t.w, True))
        for t in w:
            if t.w is not None:
                deps.append((t.w, False))
            for rd in t.r:
                deps.append((rd, False))
        for d in self.pending[eng]:
            deps.append((d, True))
        self.pending[eng] = []
        seen = set()
        for d, raw in deps:
            if d is op or id(d) in seen:
                continue
            same = (d.eng == eng) and (not d.dma) and (not dma)
            if same and eng == PE:
                continue
            if same and not raw and int(os.environ.get("TESTC", "0")) == 1:
                continue
            seen.add(id(d))
            op.deps.append(d)
            d.signal = True
        for t in r:
            if not dma:
                t.r = [x for x in t.r if x.dma or x.eng != eng]
            t.r.append(op)
        for t in w:
            t.w = op
            t.r = []
        if dma:
            n = self.dma_count[eng]
            self.dma_count[eng] = n + 1
            key = (eng, n % self.n_dma_sems)
            op.slot_prev = self.dma_last.get(key)
            self.dma_last[key] = op
            op.sem = key
            op.val = 16 * (n // self.n_dma_sems + 1)
        self.ops[eng].append(op)
        return op

    def barrier(self):
        lasts = []
        for e in ENGS:
            for op in reversed(self.ops[e]):
                if not op.dma:
                    lasts.append(op)
                    break
        lasts += list(self.dma_last.values())
        for e in ENGS:
            self.pending[e] = self.pending[e] + lasts

    def emit(self, final_wait_ops=()):
        nc = self.nc
        with contextlib.ExitStack() as st:
            sems = {}
            for e in ENGS:
                sems[e] = st.enter_context(nc.semaphore("s_" + e))
                for k in range(self.n_dma_sems):
                    if self.dma_count[e] > k:
                        sems[(e, k)] = st.enter_context(nc.semaphore("d_%s_%d" % (e, k)))
            for e in ENGS:
                c = 0
                for op in self.ops[e]:
                    if op.dma:
                        continue
                    if op.signal:
                        c += 1
                        op.sem = e
                        op.val = c
            block = st.enter_context(nc.Block())
            engobj = {PE: block.tensor, ACT: block.scalar, DVE: block.vector, POOL: block.gpsimd, SP: block.sync}
            stats = {}
            for e in ENGS:
                ops = self.ops[e]
                if not ops and not (e == SP and final_wait_ops):
                    continue

                def body(eng, ops=ops, e=e):
                    waited = {}
                    nw = 0

                    def wait(semkey, val):
                        nonlocal nw
                        if waited.get(semkey, 0) >= val:
                            return
                        waited[semkey] = val
                        eng.wait_ge(sems[semkey], val)
                        nw += 1

                    for op in ops:
                        for d in op.deps:
                            wait(d.sem, d.val)
                        if op.dma and op.slot_prev is not None:
                            wait(op.slot_prev.sem, op.slot_prev.val)
                        ins = op.fn(eng)
                        if op.dma:
                            ins.then_inc(sems[op.sem], 16)
                        elif op.signal:
                            ins.then_inc(sems[op.sem], 1)
                    if e == SP:
                        for op in final_wait_ops:
                            wait(op.sem, op.val)
                    stats[e] = (len(ops), nw)

                engobj[e](body)
            self.stats = stats


CF_COS, CF_SIN, CF_G12, CF_GNSA, CF_ADDC, CF_CONVC, CF_COSC, CF_SINC, CF_GCMP, CF_END = (
    0, 512, 1024, 1792, 2304, 2816, 2956, 2988, 3020, 3084)
CB_ID, CB_MASKC, CB_DMASK, CB_END = 0, 128, 2176, 2432

C_Q, C_ROPEK, C_KV, C_V, C_GATE, C_CA, C_CG, C_END = 0, 512, 768, 1024, 1280, 1304, 1816, 2328

FFN_GROUPS = [(0, 4), (4, 8), (8, 12), (12, 16), (16, 20), (20, 22)]


def bl(ap, n):
    shp = list(ap.shape)
    return ap.unsqueeze(len(shp)).to_broadcast(shp + [n])


def bm(ap, n):
    shp = list(ap.shape)
    return ap.unsqueeze(1).to_broadcast([shp[0], n] + shp[1:])


def build(dbg=False, phases=3):
    nc = bass.Bass("TRN2", target_bir_lowering=False)

    def din(name, shape, dt=F32):
        return nc.dram_tensor(name, list(shape), dt, kind="ExternalInput").ap()

    x_d = din("x", [S_LEN, D])
    win_d = din("w_in", [D, C_END])
    wout_d = din("w_out", [D, D])
    wgu_d = din("w_gu", [D, 2 * FFN])
    wdn_d = din("w_dn", [FFN, D])
    w1k_d = din("w1k", [2048, 256])
    w1v_d = din("w1v", [2048, 256])
    w2k_d = din("w2k", [256, 64])
    w2v_d = din("w2v", [256, 64])
    cf_d = din("cf", [128, CF_END])
    gba_d = din("gbc_attn", [128, D])
    gbf_d = din("gbc_ffn", [128, D])
    cb_d = din("cb", [128, CB_END], BF)
    etab_d = din("etab", [32, S_LEN], BF)
    ovl_d = din("ovl", [127, 32], BF)
    pet_d = din("pet", [128, 32])
    y_d = nc.dram_tensor("y", [S_LEN, D], F32, kind="ExternalOutput").ap()
    dbg_out = {}

    with contextlib.ExitStack() as st:
        ARENA_BYTES = 206 * 1024
        Aten = st.enter_context(nc.sbuf_tensor("arena", [128, ARENA_BYTES], U8))
        ps = st.enter_context(nc.psum_tensor("ps", [128, 8, 512], F32))

        def psb(b):
            return ps[:, b, :]

        def psh(b):
            return ps[:, b, :].bitcast(BF)

        allocs = []

        class Reg:
            def __init__(self, base, size, phases):
                self.base, self.size, self.phases, self.ptr = base, size, phases, 0

            def alloc(self, nbytes, name=""):
                o = self.base + self.ptr
                self.ptr += (nbytes + 63) // 64 * 64
                assert self.ptr <= self.size, ("arena region overflow", name, self.ptr, self.size)
                allocs.append((name, o, nbytes, self.phases))
                return Aten[:, o:o + nbytes]

        SZ_CONST = 24 * 1024
        SZ_MIXC = 16384
        SZ_A = 37248 + 64
        SZ_B = 61 * 1024
        SZ_C = 67328
        o = 0
        R_const = Reg(o, SZ_CONST, "all"); o += SZ_CONST
        R_mixc = Reg(o, SZ_MIXC, "all"); o += SZ_MIXC
        baseA = o
        R_A1 = Reg(o, SZ_A, "p1a"); R_A1b = Reg(o, SZ_A, "p1b"); R_A2 = Reg(o, SZ_A, "p2"); o += SZ_A
        baseB = o
        SZ_HCVB = (4 * 2078 * 2 + 63) // 64 * 64
        R_Bh = Reg(o, SZ_HCVB, "p1")
        R_B1 = Reg(o + SZ_HCVB, SZ_B - SZ_HCVB, "p1a"); R_B1b = Reg(o + SZ_HCVB, SZ_B - SZ_HCVB, "p1b")
        B2HEAD = 17 * 1024
        R_B2 = Reg(o, B2HEAD, "p2"); o += SZ_B
        baseC = o
        R_C = Reg(o, SZ_C, "p12"); o += SZ_C
        assert o <= ARENA_BYTES, o
        R_3X = Reg(baseA + 16384, (baseB + B2HEAD) - (baseA + 16384), "p3")
        R_3Y = Reg(baseB + B2HEAD, SZ_B - B2HEAD, "p23")
        R_3Z = Reg(baseC, SZ_C, "p3")

        cf = R_const.alloc(CF_END * 4, "cf").bitcast(F32)
        gbc = R_const.alloc(D * 4, "gbc").bitcast(F32)
        cb = R_const.alloc(CB_END * 2, "cb").bitcast(BF)
        ones = R_const.alloc(512, "ones").bitcast(F32)
        peT = R_const.alloc(64, "peT").bitcast(BF)
        convh = R_const.alloc(32, "convh").bitcast(F32).rearrange("p (c k) -> p c k", c=4)
        nh = R_const.alloc(4, "nh").bitcast(F32)
        selpad = R_const.alloc(192, "selpad").bitcast(BF)
        smallf = R_const.alloc(1536, "small").bitcast(F32)

        cos_t = cf[:, CF_COS:CF_COS + 512].rearrange("p (t i) -> p t i", t=NT)
        sin_t = cf[:, CF_SIN:CF_SIN + 512].rearrange("p (t i) -> p t i", t=NT)
        gain12 = cf[:, CF_G12:CF_G12 + 768]
        gnsa = cf[:, CF_GNSA:CF_GNSA + 512]
        addc = cf[:, CF_ADDC:CF_ADDC + 512].rearrange("p (t j) -> p t j", t=NT)
        convc = cf[:, CF_CONVC:CF_CONVC + 140].rearrange("p (c k) -> p c k", c=4)
        cosc = cf[0:127, CF_COSC:CF_COSC + 32]
        sinc = cf[0:127, CF_SINC:CF_SINC + 32]
        gcmp = cf[0:127, CF_GCMP:CF_GCMP + 64]
        ident = cb[:, CB_ID:CB_ID + 128]
        maskc = cb[:, CB_MASKC:CB_MASKC + 2048].rearrange("p (t q) -> p t q", t=NT)
        dmask = cb[:, CB_DMASK:CB_DMASK + 256].rearrange("p (m q) -> p m q", m=2)

        _sp = [0]

        def small(n):
            o_ = _sp[0]
            _sp[0] += n
            assert _sp[0] <= 384
            return smallf[:, o_:o_ + n]

        ssq = small(1); ssq2 = small(1); rstd = small(1)
        ss12 = small(12); ss12b = small(12); r12 = small(12)
        gtmp = small(24)
        den3 = small(12); rd3 = small(12); coef3 = small(12)
        dcl = small(4); rcl = small(4)
        top8 = small(8); thr = small(1)
        imp = small(32); score = small(32); selt = small(32)
        ssqc = small(1); ssqc2 = small(1); rstdc = small(1)
        eps_ap = small(1); eps4_ap = small(1)

        mix_raw = Aten[:, R_mixc.base:R_mixc.base + 32768].bitcast(BF).rearrange("p (k t) -> p k t", k=8)
        R_mixc.alloc(16384, "mixT_conv")
        R_A2.alloc(16384, "mixT_nsa")
        mixT = mix_raw

        QB = [R_C.alloc(16384, "QB%d" % g).bitcast(BF)[0:96].rearrange("p (c r t) -> p c r t", c=NT, r=4) for g in range(2)]
        KE = [R_C.alloc(4096, "KE%d" % g).bitcast(BF)[0:96] for g in range(2)]
        KW = [R_C.alloc(4096, "KW%d" % g).bitcast(BF)[0:64] for g in range(2)]
        VA = R_C.alloc(4 * NT * 65 * 2, "VA").bitcast(BF).rearrange("p (i t d) -> p i t d", i=4, t=NT)
        kvT = [R_C.alloc(4096, "kvT%d" % g).bitcast(BF) for g in range(2)]
        gates = R_C.alloc(NT * 24 * 4, "gates").bitcast(F32).rearrange("p (t c) -> p t c", t=NT)

        w_in = R_A1.alloc(8 * C_END * 2, "w_in").bitcast(BF).rearrange("p (k c) -> p k c", k=8)
        xnT = R_B1.alloc(8192, "xnT").bitcast(BF).rearrange("p (k t) -> p k t", k=8)
        xt = [R_B1.alloc(4096, "xt%d" % i).bitcast(F32) for i in range(2)]
        xn = R_B1.alloc(2048, "xn").bitcast(BF)
        sq = R_B1.alloc(3072, "sq").bitcast(F32)
        qn = R_B1.alloc(3072, "qn").bitcast(F32)
        qr = R_B1.alloc(1536, "qr").bitcast(BF)
        rt1 = R_B1.alloc(1536, "rt1").bitcast(F32).rearrange("p (h i) -> p h i", h=12)
        rt2 = R_B1.alloc(1536, "rt2").bitcast(F32).rearrange("p (h i) -> p h i", h=12)
        kvst = R_B1.alloc(512, "kvst").bitcast(BF)
        hcvb = R_Bh.alloc(4 * 2078 * 2, "hcvb").bitcast(BF).rearrange("p (c t) -> p c t", c=4)
        Fg = [R_B1.alloc(2048, "Fg%d" % i).bitcast(F32) for i in range(2)]
        junk = R_B1.alloc(2048, "junk").bitcast(BF)
        diag = R_A1b.alloc(124 * 128 * 2, "diag").bitcast(BF).rearrange("p (i m) -> p i m", i=124)
        accs = [R_B1b.alloc(8192, "acc%d" % i).bitcast(F32).rearrange("p (c t) -> p c t", c=4) for i in range(2)]
        Ft = [R_B1b.alloc(2048, "F%d" % i).bitcast(F32) for i in range(6)]

        w1 = R_A2.alloc(16384, "w1").bitcast(BF).rearrange("p (l j) -> p l j", l=32)
        w2 = R_A2.alloc(512, "w2").bitcast(BF).rearrange("p (c v d) -> p c v d", c=2, v=2)
        kcTc = [R_A2.alloc(256, "kcTc%d" % g).bitcast(BF)[0:64, 0:127] for g in range(2)]
        VCa = [R_A2.alloc(256, "VCa%d" % g).bitcast(BF)[0:127, 0:97] for g in range(2)]
        PT = [R_B2.alloc(1024, "PT%d" % i).bitcast(BF) for i in range(4)]
        onsa = [R_B2.alloc(2048, "onsa%d" % i).bitcast(F32) for i in range(2)]
        otmp = [R_B2.alloc(1024, "otmp%d" % i).bitcast(F32) for i in range(2)]
        onb = R_B2.alloc(1024, "onb").bitcast(BF)
        cth_off = R_B2.base + R_B2.ptr
        cths = [R_B2.alloc(1024, "cth%d" % i).bitcast(F32)[0:127] for i in range(2)]
        chss = [R_B2.alloc(512, "chs%d" % i).bitcast(BF)[0:127] for i in range(2)]
        chsT2 = R_B2.alloc(1024, "chsT").bitcast(BF).rearrange("p (c n) -> p c n", c=4)
        kcm = R_B2.alloc(256, "kcm").bitcast(F32)[0:127]
        kcn = R_B2.alloc(256, "kcn").bitcast(F32)[0:127]
        kcb = R_B2.alloc(128, "kcb").bitcast(BF)[0:127]
        ct1 = R_B2.alloc(128, "ct1").bitcast(F32)[0:127]
        ct2 = R_B2.alloc(128, "ct2").bitcast(F32)[0:127]
        cjunk = R_B2.alloc(256, "cjunk").bitcast(F32)[0:127]

        hbuf = R_3Z.alloc(65536, "h").bitcast(F32).rearrange("p (t c) -> p t c", t=NT)
        wout = R_3Y.alloc(16384, "wout").bitcast(BF).rearrange("p (k c) -> p k c", k=8)
        hns = [R_3X.alloc(2048, "hn%d" % i).bitcast(BF) for i in range(2)]
        _wgu = [R_3Y.alloc(16384, "wgu0"), R_3X.alloc(16384, "wgu1")]
        wgu = [w_.bitcast(BF).rearrange("p (k u c) -> p k u c", k=8, u=2) for w_ in _wgu]
        _wdn = [R_3Y.alloc(8192, "wdn0"), R_3X.alloc(8192, "wdn1")]
        wdn = [w_.bitcast(BF).rearrange("p (j c) -> p j c", j=4) for w_ in _wdn]
        _act = [R_3Y.alloc(4096, "actT0"), R_3X.alloc(4096, "actT1")]
        actT = [a_.bitcast(BF).rearrange("p (j t) -> p j t", j=4) for a_ in _act]
        fth = [R_3X.alloc(2048, "fth%d" % i).bitcast(F32) for i in range(2)]
        fz = fth
        junk3 = fth[0].bitcast(BF)

        S = Sched(nc)
        toks = {}

        def tk(*key):
            t = toks.get(key)
            if t is None:
                t = toks[key] = Tok(str(key))
            return t

        defer = [None]

        def A(eng, fn, r=(), w=(), dma=False):
            pr = [t for t in r if t.name.startswith("('ps'")]
            if pr:
                r = [t for t in r if not t.name.startswith("('ps'")]
                w = list(w) + [t for t in pr if t not in w]
            if defer[0] is not None:
                defer[0].append((eng, fn, list(r), list(w), dma))
                return None
            return S.add(eng, fn, r=r, w=w, dma=dma)

        conv_q = []

        def drain(n):
            while n > 0 and conv_q:
                eng, fn, r, w, dma = conv_q.pop(0)
                S.add(eng, fn, r=r, w=w, dma=dma)
                n -= 1

        def rsqrt_pool(out, in_, n, scale, eps, tin, tout):
            tmp = in_
            A(POOL, lambda e: e.tensor_scalar(out=out, in0=in_, scalar1=scale, scalar2=eps, op0=ALU.mult, op1=ALU.add),
              r=[tin], w=[tout])
            A(POOL, lambda e: e.tensor_tensor(out=out, in0=out, in1=nh[0:out.shape[0], 0:1].to_broadcast(list(out.shape)), op=ALU.pow),
              r=[tout, tk("nh")], w=[tout])

        A(SP, lambda e: e.dma_start(out=cf, in_=cf_d), w=[tk("cf")], dma=True)
        A(SP, lambda e: e.dma_start(out=cb, in_=cb_d), w=[tk("cb")], dma=True)
        A(SP, lambda e: e.dma_start(out=gbc, in_=gba_d), w=[tk("gbc")], dma=True)
        WGRP = [(0, 512), (512, 1024), (1024, 1304), (1304, 2328)]

        def win_group(gi_):
            c0_, c1_ = WGRP[gi_]
            for k in range(8):
                A(POOL, lambda e, k=k: e.dma_start(out=w_in[:, k, c0_:c1_], in_=win_d[k * 128:(k + 1) * 128, c0_:c1_]),
                  w=[tk("w_in", gi_, k)], dma=True)
        for g in range(2):
            A(SP, lambda e, g=g: e.dma_start(out=KE[g][64:96, :], in_=etab_d), w=[tk("KEe", g)], dma=True)
        A(POOL, lambda e: e.memset(nh, -0.5), w=[tk("nh")])
        A(POOL, lambda e: e.memset(eps_ap, EPS), w=[tk("epsc")])
        A(POOL, lambda e: e.memset(eps4_ap, 4.0 * EPS), w=[tk("epsc")])
        A(POOL, lambda e: e.memset(ones, 1.0), w=[tk("ones")])
        A(POOL, lambda e: e.memset(VA[:, :, :, 64:65], 1.0), w=[tk("VAones")])
        if int(os.environ.get("TESTB", "0")) == 0:
            A(DVE, lambda e: e.memset(hcvb[:, :, 0:30], 0.0), w=[tk("hcvpad")])
        A(POOL, lambda e: e.memset(selpad, 0.0), w=[tk("selpad")])
        A(DVE, lambda e: e.tensor_scalar(out=convh, in0=convc[:, :, 32:34], scalar1=0.5, scalar2=None, op0=ALU.mult),
          r=[tk("cf")], w=[tk("convh")])
        win_group(0)
        w_in_toks = None

        ssq_p = [small(1), small(1)]
        rstd_p = [small(1), small(1)]
        def pbank(t):
            return 0 if t % 2 == 0 else 5

        def P01f(t):
            b0 = pbank(t)
            return ps[:, b0:b0 + 2, :].rearrange("p a b -> p (a b)")

        def p01f(t):
            b0 = pbank(t)
            return [tk("ps", b0), tk("ps", b0 + 1)]

        def stageA1(t):
            xti, txt = xt[t % 2], tk("xt", t % 2)
            sq_, rs_ = ssq_p[t % 2], rstd_p[t % 2]
            A(SP, lambda e: e.dma_start(out=xti, in_=x_d[t * 128:(t + 1) * 128, :]), w=[txt], dma=True)
            A(ACT, lambda e: e.activation(out=junk, in_=xti, func=AF.Square, scale=1.0 / 32.0, accum_out=sq_),
              r=[txt], w=[tk("junk"), tk("ssqp", t % 2)])
            rsqrt_pool(rs_, sq_, 1, 1.0, EPS, tk("ssqp", t % 2), tk("rstdp", t % 2))

        def stageA2a(t):
            xti, txt = xt[t % 2], tk("xt", t % 2)
            rs_ = rstd_p[t % 2]
            A(DVE, lambda e: e.scalar_tensor_tensor(out=xn, in0=xti, scalar=rs_, in1=gbc, op0=ALU.mult, op1=ALU.mult),
              r=[txt, tk("rstdp", t % 2), tk("gbc")], w=[tk("xn")])

        def stageA2b(t):
            tl = t % 4
            for k in range(8):
                A(PE, lambda e, k=k: e.transpose(out=psh(3)[:, k * 128:(k + 1) * 128], in_=xn[:, k * 128:(k + 1) * 128], identity=ident),
                  r=[tk("xn"), tk("cb")], w=[tk("ps", 3)])
            A(ACT, lambda e: e.copy(out=xnT[:, :, tl * 128:(tl + 1) * 128], in_=psh(3).rearrange("p (k t) -> p k t", k=8)),
              r=[tk("ps", 3)], w=[tk("xnT", tl)])

        def stageB(t):
            tl = t % 4
            b0 = pbank(t)
            for gi_, c0, c1 in [(0, 0, 512), (1, 512, 1024), (2, 1024, 1304)]:
                bank = b0 + gi_
                for k in range(8):
                    A(PE, lambda e, k=k, bank=bank, c0=c0, c1=c1: e.matmul(
                        out=ps[:, bank, 0:c1 - c0], lhsT=xnT[:, k, tl * 128:(tl + 1) * 128], rhs=w_in[:, k, c0:c1],
                        start=(k == 0), stop=(k == 7)),
                      r=[tk("xnT", tl), tk("w_in", gi_, k)], w=[tk("ps", bank)])

        def stageC1(t):
            P01, p01 = P01f(t), p01f(t)
            A(ACT, lambda e: e.activation(out=sq, in_=P01[:, 0:768], func=AF.Square), r=p01, w=[tk("sq")])
            A(DVE, lambda e: e.reduce_sum(out=ss12, in_=sq.rearrange("p (h d) -> p h d", d=64), axis=AX.X),
              r=[tk("sq")], w=[tk("ss12")])
            rsqrt_pool(r12, ss12, 12, 1.0 / 64.0, EPS, tk("ss12"), tk("r12"))

        def stageC2a(t):
            P01, p01 = P01f(t), p01f(t)
            b2 = pbank(t) + 2
            qn3 = qn.rearrange("p (h d) -> p h d", d=64)
            A(DVE, lambda e: e.tensor_tensor(out=qn3, in0=P01[:, 0:768].rearrange("p (h d) -> p h d", d=64), in1=bl(r12, 64), op=ALU.mult),
              r=p01 + [tk("r12")], w=[tk("qn")])
            A(ACT, lambda e: e.copy(out=kvst, in_=P01[:, 768:1024]), r=[p01[1]], w=[tk("kvst")])
            A(ACT, lambda e: e.copy(out=VA[:, :, t, 0:64], in_=ps[:, b2, 0:256].rearrange("p (i d) -> p i d", i=4)),
              r=[tk("ps", b2)], w=[tk("VA", t)])
            A(ACT, lambda e: e.activation(out=gtmp, in_=ps[:, b2, 256:280], func=AF.Tanh, scale=0.5), r=[tk("ps", b2)], w=[tk("gtmp")])
            A(DVE, lambda e: e.tensor_scalar(out=gates[:, t, :], in0=gtmp, scalar1=0.5, scalar2=0.5, op0=ALU.mult, op1=ALU.add),
              r=[tk("gtmp")], w=[tk("gates", t)])

        def stageC2b(t):
            qn3 = qn.rearrange("p (h d) -> p h d", d=64)
            qr3 = qr.rearrange("p (h d) -> p h d", d=64)
            A(DVE, lambda e: e.tensor_tensor(out=qn, in0=qn, in1=gain12, op=ALU.mult), r=[tk("qn"), tk("cf")], w=[tk("qn")])
            cb_ = bm(cos_t[:, t, :], 12)
            sb_ = bm(sin_t[:, t, :], 12)
            x1 = qn3[:, :, 0:32]
            x2 = qn3[:, :, 32:64]
            A(DVE, lambda e: e.tensor_tensor(out=rt2, in0=x2, in1=sb_, op=ALU.mult), r=[tk("qn"), tk("cf")], w=[tk("rt2")])
            A(DVE, lambda e: e.tensor_tensor(out=rt1, in0=x1, in1=cb_, op=ALU.mult), r=[tk("qn"), tk("cf")], w=[tk("rt1")])
            A(DVE, lambda e: e.tensor_tensor(out=qr3[:, :, 0:32], in0=rt1, in1=rt2, op=ALU.subtract),
              r=[tk("rt1"), tk("rt2")], w=[tk("qr")])
            A(DVE, lambda e: e.tensor_tensor(out=rt2, in0=x1, in1=sb_, op=ALU.mult), r=[tk("qn"), tk("cf")], w=[tk("rt2")])
            A(DVE, lambda e: e.tensor_tensor(out=rt1, in0=x2, in1=cb_, op=ALU.mult), r=[tk("qn"), tk("cf")], w=[tk("rt1")])
            A(DVE, lambda e: e.tensor_tensor(out=qr3[:, :, 32:64], in0=rt1, in1=rt2, op=ALU.add),
              r=[tk("rt1"), tk("rt2")], w=[tk("qr")])

        def stageC3(t):
            for h in range(8):
                A(PE, lambda e, h=h: e.transpose(out=psh(4)[0:64, h * 128:(h + 1) * 128], in_=qr[:, h * 64:(h + 1) * 64], identity=ident),
                  r=[tk("qr"), tk("cb")], w=[tk("ps", 4)])
            for g in range(2):
                A(ACT, lambda e, g=g: e.copy(out=QB[g][0:64, t, :, :], in_=psh(4)[0:64, g * 512:(g + 1) * 512].rearrange("p (r q) -> p r q", r=4)),
                  r=[tk("ps", 4)], w=[tk("QBq", g, t)])
            for i in range(4):
                A(PE, lambda e, i=i: e.transpose(out=psh(3)[0:64, i * 128:(i + 1) * 128], in_=qr[:, (8 + i) * 64:(9 + i) * 64], identity=ident),
                  r=[tk("qr"), tk("cb")], w=[tk("ps", 3)])
            for g in range(2):
                A(PE, lambda e, g=g: e.transpose(out=psh(3)[:, 512 + g * 128:512 + (g + 1) * 128], in_=kvst[:, g * 128:(g + 1) * 128], identity=ident),
                  r=[tk("kvst"), tk("cb")], w=[tk("ps", 3)])
            for g in range(2):
                A(ACT, lambda e, g=g: e.copy(out=KE[g][0:64, t * 128:(t + 1) * 128], in_=psh(3)[0:64, g * 128:(g + 1) * 128]),
                  r=[tk("ps", 3)], w=[tk("KE", g, t)])
                A(ACT, lambda e, g=g: e.copy(out=KW[g][0:64, t * 128:(t + 1) * 128], in_=psh(3)[0:64, (2 + g) * 128:(3 + g) * 128]),
                  r=[tk("ps", 3)], w=[tk("KW", g, t)])
            for g in range(2):
                A(DVE, lambda e, g=g: e.tensor_copy(out=kvT[g][:, t * 128:(t + 1) * 128], in_=psh(3)[:, 512 + g * 128:512 + (g + 1) * 128]),
                  r=[tk("ps", 3)], w=[tk("kvT", g)])

        def phase1_glu(tb):
            xr = [tk("xnT", i) for i in range(4)]
            for cc in range(4):
                ba, bg = (0, 1) if cc % 2 == 0 else (2, 4)
                for k in range(8):
                    A(PE, lambda e, k=k, cc=cc, ba=ba: e.matmul(out=psb(ba), lhsT=w_in[:, k, C_CA + cc * 128:C_CA + (cc + 1) * 128], rhs=xnT[:, k, :],
                                                                start=(k == 0), stop=(k == 7)), r=xr + [tk("w_in", 3, k)], w=[tk("ps", ba)])
                for k in range(8):
                    A(PE, lambda e, k=k, cc=cc, bg=bg: e.matmul(out=psb(bg), lhsT=w_in[:, k, C_CG + cc * 128:C_CG + (cc + 1) * 128], rhs=xnT[:, k, :],
                                                                start=(k == 0), stop=(k == 7)), r=xr + [tk("w_in", 3, k)], w=[tk("ps", bg)])
                Fi = Fg[cc % 2]
                tFi = tk("Fg", cc % 2)
                A(ACT, lambda e, Fi=Fi, bg=bg: e.activation(out=Fi, in_=psb(bg), func=AF.Tanh, scale=0.5), r=[tk("ps", bg)], w=[tFi])
                A(DVE, lambda e, Fi=Fi: e.tensor_scalar(out=Fi, in0=Fi, scalar1=0.5, scalar2=0.5, op0=ALU.mult, op1=ALU.add), r=[tFi], w=[tFi])
                _o = hcvb[:, cc, 30 + tb * 512:30 + (tb + 1) * 512]
                A(DVE, lambda e, Fi=Fi, cc=cc, _o=_o, ba=ba: e.tensor_tensor(out=_o, in0=psb(ba), in1=Fi, op=ALU.mult),
                  r=[tk("ps", ba), tFi], w=[tk("hcvb", tb)])

        def phase1b_setup():
            for i in range(124):
                cc, k = divmod(i, 31)
                eng = (ACT, DVE)[i % 2]
                if eng == ACT:
                    A(ACT, lambda e, i=i, cc=cc, k=k: e.activation(out=diag[:, i, :], in_=ident, func=AF.Copy, scale=convc[:, cc, k:k + 1]),
                      r=[tk("cb"), tk("cf")], w=[tk("diag", i)])
                else:
                    A(eng, lambda e, i=i, cc=cc, k=k: e.tensor_scalar(out=diag[:, i, :], in0=ident, scalar1=convc[:, cc, k:k + 1], scalar2=None, op0=ALU.mult),
                      r=[tk("cb"), tk("cf")], w=[tk("diag", i)])

        def conv_cc(tb, cc):
            acc = accs[tb % 2]
            hr = [tk("hcvb", tb), tk("hcvpad")] + ([tk("hcvb", tb - 1)] if tb > 0 else [])
            if True:
                bank = cc
                for k in range(31):
                    A(PE, lambda e, cc=cc, k=k, bank=bank: e.matmul(out=psb(bank), lhsT=diag[:, cc * 31 + k, :], rhs=hcvb[:, cc, tb * 512 + k:tb * 512 + k + 512],
                                                                    start=(k == 0), stop=(k == 30)),
                      r=hr + [tk("diag", cc * 31 + k)], w=[tk("ps", bank)])
                A(ACT, lambda e, cc=cc, bank=bank: e.activation(out=acc[:, cc, :], in_=psb(bank), func=AF.Identity, bias=convc[:, cc, 31:32], scale=1.0),
                  r=[tk("ps", bank), tk("cf")], w=[tk("acc", tb % 2, cc, 0), tk("acc", tb % 2, cc, 1)])

        def ln_half(tb, h):
            acc = accs[tb % 2]
            cs = slice(h * 256, (h + 1) * 256)
            tacc = [tk("acc", tb % 2, c, h) for c in range(4)]
            b1, b2 = (6, 7) if h == 0 else (4, 5)
            p1 = ps[:, b1, 0:256]
            p2 = ps[:, b2, 0:256]
            F = lambda i: Ft[i][:, cs]
            tF = lambda i: tk("F", i, h)
            mean, var, r2 = F(2), F(3), F(4)
            for cc in range(4):
                Fi, tFi = F(cc % 2), tF(cc % 2)
                A(ACT, lambda e, Fi=Fi, cc=cc: e.activation(out=Fi, in_=acc[:, cc, cs], func=AF.Square), r=[tacc[cc]], w=[tFi])
                A(PE, lambda e, cc=cc: e.matmul(out=p1, lhsT=ones, rhs=acc[:, cc, cs], start=(cc == 0), stop=(cc == 3)),
                  r=[tacc[cc], tk("ones")], w=[tk("ps", b1)])
                A(PE, lambda e, Fi=Fi, cc=cc: e.matmul(out=p2, lhsT=ones, rhs=Fi, start=(cc == 0), stop=(cc == 3)),
                  r=[tFi, tk("ones")], w=[tk("ps", b2)])
                if cc % 2 == 1:
                    yield
            A(DVE, lambda e: e.tensor_scalar(out=mean, in0=p1, scalar1=1.0 / 512.0, scalar2=None, op0=ALU.mult), r=[tk("ps", b1)], w=[tF(2)])
            A(DVE, lambda e: e.tensor_tensor(out=var, in0=mean, in1=mean, op=ALU.mult), r=[tF(2)], w=[tF(3)])
            A(DVE, lambda e: e.scalar_tensor_tensor(out=var, in0=p2, scalar=1.0 / 512.0, in1=var, op0=ALU.mult, op1=ALU.subtract),
              r=[tk("ps", b2), tF(3)], w=[tF(3)])
            yield
            A(ACT, lambda e: e.activation(out=var, in_=var, func=AF.Sqrt, bias=eps_ap, scale=1.0), r=[tF(3), tk("epsc")], w=[tF(3)])
            yield
            A(DVE, lambda e: e.reciprocal(out=var, in_=var), r=[tF(3)], w=[tF(3)])
            yield
            for cc in range(4):
                th, z = F(5), F(cc % 2)
                tth, tz = tF(5), tF(cc % 2)
                A(DVE, lambda e, cc=cc: e.tensor_tensor(out=acc[:, cc, cs], in0=acc[:, cc, cs], in1=mean, op=ALU.subtract),
                  r=[tacc[cc], tF(2)], w=[tacc[cc]])
                A(DVE, lambda e, cc=cc: e.tensor_tensor(out=acc[:, cc, cs], in0=acc[:, cc, cs], in1=var, op=ALU.mult),
                  r=[tacc[cc], tF(3)], w=[tacc[cc]])
                yield
                A(ACT, lambda e, cc=cc, th=th: e.activation(out=th, in_=acc[:, cc, cs], func=AF.Tanh, scale=convh[:, cc, 0:1], bias=convh[:, cc, 1:2]),
                  r=[tacc[cc], tk("convh")], w=[tth])
                A(DVE, lambda e, cc=cc, z=z: e.tensor_scalar(out=z, in0=acc[:, cc, cs], scalar1=convc[:, cc, 32:33], scalar2=convc[:, cc, 33:34],
                                                             op0=ALU.mult, op1=ALU.add), r=[tacc[cc], tk("cf")], w=[tz])
                yield
                A(DVE, lambda e, cc=cc, z=z, th=th: e.scalar_tensor_tensor(out=acc[:, cc, cs], in0=th, scalar=1.0, in1=z, op0=ALU.add, op1=ALU.mult),
                  r=[tth, tz], w=[tacc[cc]])
                yield
                A(ACT, lambda e, cc=cc, th=th: e.activation(out=th, in_=acc[:, cc, cs], func=AF.Square), r=[tacc[cc]], w=[tth])
                A(PE, lambda e, cc=cc, th=th: e.matmul(out=p1, lhsT=ones, rhs=th, start=(cc == 0), stop=(cc == 3)),
                  r=[tth, tk("ones")], w=[tk("ps", b1)])
                yield
            A(DVE, lambda e: e.tensor_copy(out=r2, in_=p1), r=[tk("ps", b1)], w=[tF(4)])
            yield
            A(ACT, lambda e: e.activation(out=r2, in_=r2, func=AF.Sqrt, bias=eps4_ap, scale=1.0 / 512.0), r=[tF(4), tk("epsc")], w=[tF(4)])
            yield
            A(DVE, lambda e: e.reciprocal(out=r2, in_=r2), r=[tF(4)], w=[tF(4)])
            yield
            t0 = tb * 512 + h * 256
            for cc in range(4):
                A(DVE, lambda e, cc=cc: e.scalar_tensor_tensor(out=mixT[:, cc, t0:t0 + 256], in0=acc[:, cc, cs], scalar=convc[:, cc, 34:35],
                                                               in1=r2, op0=ALU.mult, op1=ALU.mult),
                  r=[tacc[cc], tF(4), tk("cf")], w=[tk("mixT", tb * 4 + h * 2 + i) for i in range(2)])

        def ln_block(tb, fillers):
            gens = [ln_half(tb, 0), ln_half(tb, 1)]
            alive = [True, True]
            step = 0
            while any(alive):
                for i in range(2):
                    if alive[i]:
                        try:
                            next(gens[i])
                        except StopIteration:
                            alive[i] = False
                step += 1
                if fillers and step in (2, 5, 9, 13):
                    fillers.pop(0)()
            while fillers:
                fillers.pop(0)()


        stageA1(0)
        stageA1(1)
        win_group(1)
        win_group(2)
        A(POOL, lambda e: e.dma_start(out=peT, in_=pet_d), w=[tk("peT")], dma=True)
        stageA2a(0)
        stageA2b(0)
        stageB(0)
        stageC1(0)
        for t in range(NT):
            if t % 4 == 3:
                phase1_glu(t // 4)
            if t + 2 < NT:
                stageA1(t + 2)
            if t == 0:
                win_group(3)
            if t + 1 < NT:
                stageA2a(t + 1)
                stageA2b(t + 1)
            stageC2a(t)
            if t + 1 < NT:
                stageB(t + 1)
            stageC2b(t)
            if t + 1 < NT:
                stageC1(t + 1)
            stageC3(t)

        _skip = int(os.environ.get("SKIP1B", "0"))
        S.barrier()
        if _skip != 1:
            phase1b_setup()
        if _skip == 0:
            for cc in range(4):
                conv_cc(0, cc)
            for tb in range(4):
                fillers = [(lambda tb=tb, cc=cc: conv_cc(tb + 1, cc)) for cc in range(4)] if tb + 1 < 4 else []
                ln_block(tb, fillers)
        out_ops = []

        def dump(name, ap, shape, dt, rtoks):
            d = nc.dram_tensor("dbg_" + name, list(shape), dt, kind="ExternalOutput").ap()
            dbg_out[name] = d
            out_ops.append(A(SP, lambda e: e.dma_start(out=d, in_=ap), r=rtoks, dma=True))

        if dbg:
            S.barrier()
            alltoks = list(toks.values())
            for g in range(2):
                dump("QB%d" % g, QB[g][0:64].rearrange("p c r t -> p (c r t)"), [64, 8192], BF, alltoks)
                dump("KE%d" % g, KE[g], [96, 2048], BF, alltoks)
                dump("KW%d" % g, KW[g], [64, 2048], BF, alltoks)
                dump("kvT%d" % g, kvT[g], [128, 2048], BF, alltoks)
            dump("VA", VA.rearrange("p i t d -> p (i t d)"), [128, 4 * NT * 65], BF, alltoks)
            dump("gates", gates.rearrange("p t c -> p (t c)"), [128, NT * 24], F32, alltoks)
            dump("mixc", mixT[:, 0:4, :].rearrange("p k t -> p (k t)"), [128, 4 * 2048], BF, alltoks)

        _st = [0]
        _pt = [0]

        def next_st():
            _st[0] = (_st[0] + 1) % 3
            return _st[0]

        def next_pt():
            _pt[0] = (_pt[0] + 1) % 4
            return _pt[0]

        def phase2_setup():
            A(POOL, lambda e: e.dma_start(out=w1[0:64, :, :], in_=w1k_d.rearrange("(l d) j -> d l j", d=64)), w=[tk("w1", 0)], dma=True)
            A(POOL, lambda e: e.dma_start(out=w1[64:128, :, :], in_=w1v_d.rearrange("(l d) j -> d l j", d=64)), w=[tk("w1", 1)], dma=True)
            A(POOL, lambda e: e.dma_start(out=w2[:, :, 0, :], in_=w2k_d.rearrange("(c p) d -> p c d", p=128)), w=[tk("w2", 0)], dma=True)
            A(POOL, lambda e: e.dma_start(out=w2[:, :, 1, :], in_=w2v_d.rearrange("(c p) d -> p c d", p=128)), w=[tk("w2", 1)], dma=True)
            for g in range(2):
                A(SP, lambda e, g=g: e.dma_start(out=VCa[g][:, 65:97], in_=ovl_d), w=[tk("VCa", g)], dma=True)
                A(POOL, lambda e, g=g: e.memset(VCa[g][:, 64:65], 1.0), w=[tk("VCa", g)])

        def compress(g):
            banks = [7, 5]
            Hs = [ps[0:127, bk, 0:256] for bk in banks]
            KOs = [ps[0:127, bk, 256:320] for bk in banks]
            pts = [tk("ps", bk) for bk in banks]
            rows = [slice(0, 64), slice(64, 128)]
            for l in range(32):
                for kv in range(2):
                    A(PE, lambda e, l=l, kv=kv: e.matmul(out=Hs[kv], lhsT=kvT[g][rows[kv], l:l + 16 * 126 + 1:16], rhs=w1[rows[kv], l, :], start=(l == 0), stop=False),
                      r=[tk("kvT", g), tk("w1", kv)], w=[pts[kv]])
            for l in range(32):
                for kv in range(2):
                    A(PE, lambda e, l=l, kv=kv: e.matmul(out=Hs[kv], lhsT=peT[rows[kv], l:l + 1].to_broadcast([64, 127]), rhs=w1[rows[kv], l, :], start=False, stop=(l == 31)),
                      r=[tk("peT"), tk("w1", kv)], w=[pts[kv]])
            for kv in range(2):
                A(ACT, lambda e, kv=kv: e.activation(out=cths[kv], in_=Hs[kv], func=AF.Tanh, scale=0.5), r=[pts[kv]], w=[tk("cth", kv)])
                A(DVE, lambda e, kv=kv: e.scalar_tensor_tensor(out=chss[kv], in0=cths[kv], scalar=1.0, in1=Hs[kv], op0=ALU.add, op1=ALU.mult),
                  r=[tk("cth", kv), pts[kv]], w=[tk("chs", kv)])
            for kv in range(2):
                for jc in range(2):
                    A(PE, lambda e, jc=jc, kv=kv: e.transpose(out=psh(6)[:, kv * 256 + jc * 128:kv * 256 + jc * 128 + 127], in_=chss[kv][:, jc * 128:(jc + 1) * 128], identity=ident[0:127, 0:127]),
                      r=[tk("chs", kv), tk("cb")], w=[tk("ps", 6)])
            A(ACT, lambda e: e.copy(out=chsT2[:, :, 0:127], in_=psh(6)[:, 0:512].rearrange("p (c n) -> p c n", c=4)[:, :, 0:127]),
              r=[tk("ps", 6)], w=[tk("chsT")])
            for kv in range(2):
                for jc in range(2):
                    A(PE, lambda e, jc=jc, kv=kv: e.matmul(out=KOs[kv], lhsT=chsT2[:, kv * 2 + jc, 0:127], rhs=w2[:, jc, kv, :], start=(jc == 0), stop=(jc == 1)),
                      r=[tk("chsT"), tk("w2", kv)], w=[pts[kv]])
            KO = KOs[0]
            p7 = pts[0]
            A(ACT, lambda e: e.mul(out=VCa[g][:, 0:64], in_=KOs[1], mul=0.5), r=[pts[1]], w=[tk("VCa", g)])
            sc, rc_ = ssqc[0:127], rstdc[0:127]
            A(ACT, lambda e: e.mul(out=kcm, in_=KO, mul=0.5), r=[p7], w=[tk("kcm")])
            A(ACT, lambda e: e.activation(out=cjunk, in_=kcm, func=AF.Square, scale=0.125, accum_out=sc), r=[tk("kcm")], w=[tk("cjunk"), tk("ssqc")])
            rsqrt_pool(rc_, sc, 1, 1.0, EPS, tk("ssqc"), tk("rstdc"))
            A(DVE, lambda e: e.scalar_tensor_tensor(out=kcn, in0=kcm, scalar=rc_, in1=gcmp, op0=ALU.mult, op1=ALU.mult),
              r=[tk("kcm"), tk("rstdc"), tk("cf")], w=[tk("kcn")])
            x1, x2 = kcn[:, 0:32], kcn[:, 32:64]
            A(DVE, lambda e: e.tensor_tensor(out=ct1, in0=x1, in1=cosc, op=ALU.mult), r=[tk("kcn"), tk("cf")], w=[tk("ct1")])
            A(DVE, lambda e: e.tensor_tensor(out=ct2, in0=x2, in1=sinc, op=ALU.mult), r=[tk("kcn"), tk("cf")], w=[tk("ct2")])
            A(DVE, lambda e: e.tensor_tensor(out=kcb[:, 0:32], in0=ct1, in1=ct2, op=ALU.subtract), r=[tk("ct1"), tk("ct2")], w=[tk("kcb")])
            A(DVE, lambda e: e.tensor_tensor(out=ct1, in0=x2, in1=cosc, op=ALU.mult), r=[tk("kcn"), tk("cf")], w=[tk("ct1")])
            A(DVE, lambda e: e.tensor_tensor(out=ct2, in0=x1, in1=sinc, op=ALU.mult), r=[tk("kcn"), tk("cf")], w=[tk("ct2")])
            A(DVE, lambda e: e.tensor_tensor(out=kcb[:, 32:64], in0=ct1, in1=ct2, op=ALU.add), r=[tk("ct1"), tk("ct2")], w=[tk("kcb")])
            A(PE, lambda e: e.transpose(out=psh(6)[0:64, 512:639], in_=kcb, identity=ident[0:127, 0:127]), r=[tk("kcb"), tk("cb")], w=[tk("ps", 6)])
            A(ACT, lambda e: e.copy(out=kcTc[g], in_=psh(6)[0:64, 512:639]), r=[tk("ps", 6)], w=[tk("kcTc", g)])

        PIPE_D = 3
        ST_BANKS = [0, 1, 2, 7]
        pend = []
        _u = [0]
        Oc = ps[:, 3, 0:388].rearrange("p (r d) -> p r d", r=4)
        Os = ps[:, 4, 0:260].rearrange("p (r d) -> p r d", r=4)
        Ow = ps[:, 5, 0:260].rearrange("p (r d) -> p r d", r=4)
        p3, p4, p5 = tk("ps", 3), tk("ps", 4), tk("ps", 5)

        def emit_pv(u):
            pi = u["pi"]
            np_ = u["np"]
            for r_ in range(4):
                A(PE, lambda e, r_=r_, u=u, pi=pi, np_=np_: e.matmul(out=u["out"](r_), lhsT=PT[pi][0:np_, r_ * 128:(r_ + 1) * 128], rhs=u["v"],
                                                                   start=(u["first"] and r_ == 0), stop=u["last"], skip_group_check=True),
                  r=[tk("PT", pi)] + u["vtoks"], w=[u["otok"]])
            if u.get("after"):
                u["after"]()

        def pop_one():
            emit_pv(pend.pop(0))

        delayed = []

        def tick():
            for d in delayed:
                d[0] -= 1
            while delayed and delayed[0][0] <= 0:
                delayed.pop(0)[1]()

        def emit_unit(u):
            tick()
            i = _u[0]
            _u[0] += 1
            sb = ST_BANKS[i % 4]
            pi = i % 4
            u["pi"] = pi
            np_ = u["np"]
            A(PE, lambda e: e.matmul(out=ps[0:np_, sb, :], lhsT=u["k"], rhs=u["q"], start=True, stop=True), r=u["ktoks"], w=[tk("ps", sb)])
            A(ACT, lambda e: e.activation(out=PT[pi][0:np_, :], in_=ps[0:np_, sb, :], func=AF.Exp, scale=SCALE), r=[tk("ps", sb)], w=[tk("PT", pi)])
            if u.get("mask") is not None:
                pv = PT[pi][0:np_, :].rearrange("p (r q) -> p r q", r=4)
                A(POOL, lambda e: e.tensor_tensor(out=pv, in0=pv, in1=bm(u["mask"], 4), op=ALU.mult), r=[tk("PT", pi), tk("cb")], w=[tk("PT", pi)])
            pend.append(u)
            while len(pend) > PIPE_D:
                pop_one()

        def q64(c, g):
            return QB[g][0:64, c, :, :].rearrange("p r q -> p (r q)")

        def q96(c, g):
            return QB[g][0:96, c, :, :].rearrange("p r q -> p (r q)")

        dclA = small(4); rclA = small(4); coefA = small(4)
        den2 = small(8); rd2 = small(8); coef2 = small(8)

        def selection(c, g):
            A(DVE, lambda e: e.tensor_scalar(out=dclA, in0=Oc[:, :, 64], scalar1=1e-30, scalar2=None, op0=ALU.max), r=[p3], w=[tk("dclA")])
            A(DVE, lambda e: e.reciprocal(out=rclA, in_=dclA), r=[tk("dclA")], w=[tk("rclA")])
            A(DVE, lambda e: e.scalar_tensor_tensor(out=imp, in0=Oc[:, 0, 65:97], scalar=rclA[:, 0:1], in1=addc[:, c, :], op0=ALU.mult, op1=ALU.add),
              r=[p3, tk("rclA"), tk("cf")], w=[tk("imp")])
            for r_ in range(1, 4):
                A(DVE, lambda e, r_=r_: e.scalar_tensor_tensor(out=imp, in0=Oc[:, r_, 65:97], scalar=rclA[:, r_:r_ + 1], in1=imp, op0=ALU.mult, op1=ALU.add),
                  r=[p3, tk("rclA"), tk("imp")], w=[tk("imp")])
            A(DVE, lambda e: e.max(out=top8, in_=imp), r=[tk("imp")], w=[tk("top8")])
            A(DVE, lambda e: e.tensor_scalar(out=thr, in0=top8[:, 7:8], scalar1=-1.0, scalar2=None, op0=ALU.max), r=[tk("top8")], w=[tk("thr")])
            A(DVE, lambda e: e.tensor_scalar(out=selt, in0=imp, scalar1=thr, scalar2=None, op0=ALU.is_ge), r=[tk("imp"), tk("thr")], w=[tk("selt")])
            A(DVE, lambda e: e.tensor_scalar(out=selpad[:, 64:96], in0=selt, scalar1=-1.0, scalar2=BIG, op0=ALU.add, op1=ALU.mult),
              r=[tk("selt")], w=[tk("selpad")])
            gv = gates[:, c, g * 12:(g + 1) * 12].rearrange("p (r b) -> p b r", b=3)
            A(DVE, lambda e: e.tensor_tensor(out=coefA, in0=rclA, in1=gv[:, 0, :], op=ALU.mult), r=[tk("rclA"), tk("gates", c)], w=[tk("coefA")])
            on = onsa[c % 2][:, g * 256:(g + 1) * 256].rearrange("p (r d) -> p r d", r=4)
            A(DVE, lambda e: e.tensor_tensor(out=on, in0=Oc[:, :, 0:64], in1=bl(coefA, 64), op=ALU.mult), r=[p3, tk("coefA")], w=[tk("onsa", c % 2, g)])

        def bias_T(c, g):
            A(PE, lambda e: e.transpose(out=psh(6)[0:96, 0:128], in_=selpad, identity=ident), r=[tk("selpad"), tk("cb")], w=[tk("ps", 6)])
            A(DVE, lambda e: e.tensor_copy(out=QB[g][64:96, c, :, :], in_=bm(psh(6)[64:96, 0:128], 4)), r=[tk("ps", 6)], w=[tk("QBb", g, c)])

        def combineB(c, g):
            d2 = den2.rearrange("p (b r) -> p b r", b=2)
            r2_ = rd2.rearrange("p (b r) -> p b r", b=2)
            c2_ = coef2.rearrange("p (b r) -> p b r", b=2)
            A(DVE, lambda e: e.tensor_copy(out=obuf, in_=ps[:, 4:6, 0:260]), r=[p4, p5], w=[tk("obuf")])
            Os_ = obuf[:, 0, :].rearrange("p (r d) -> p r d", r=4)
            Ow_ = obuf[:, 1, :].rearrange("p (r d) -> p r d", r=4)
            A(DVE, lambda e: e.tensor_scalar(out=d2, in0=obuf.rearrange("p b (r d) -> p b r d", r=4)[:, :, :, 64], scalar1=1e-30, scalar2=None, op0=ALU.max),
              r=[tk("obuf")], w=[tk("den2")])
            A(DVE, lambda e: e.reciprocal(out=rd2, in_=den2), r=[tk("den2")], w=[tk("rd2")])
            gv = gates[:, c, g * 12:(g + 1) * 12].rearrange("p (r b) -> p b r", b=3)
            A(DVE, lambda e: e.tensor_tensor(out=c2_, in0=r2_, in1=gv[:, 1:3, :], op=ALU.mult), r=[tk("rd2"), tk("gates", c)], w=[tk("coef2")])
            on = onsa[c % 2][:, g * 256:(g + 1) * 256].rearrange("p (r d) -> p r d", r=4)
            ton = tk("onsa", c % 2, g)
            for bi, O_ in enumerate([Os_, Ow_]):
                ot = otmp[bi].rearrange("p (r d) -> p r d", r=4)
                A(DVE, lambda e, bi=bi, O_=O_, ot=ot: e.tensor_tensor(out=ot, in0=O_[:, :, 0:64], in1=bl(c2_[:, bi, :], 64), op=ALU.mult),
                  r=[tk("obuf"), tk("coef2")], w=[tk("otmp", bi)])
                A(DVE, lambda e, ot=ot: e.tensor_tensor(out=on, in0=on, in1=ot, op=ALU.add), r=[ton, tk("otmp", bi)], w=[ton])

        def cmp_unit(c, g, after):
            vca = [tk("VCa", g), tk("VCa", g), tk("VCa", g)]
            return dict(np=127, k=kcTc[g], q=q64(c, g), ktoks=[tk("kcTc", g), tk("QBq", g, c)], mask=maskc[0:127, c, :],
                        out=lambda r_: ps[:, 3, r_ * 97:(r_ + 1) * 97], v=VCa[g], vtoks=vca, otok=p3, first=True, last=True, after=after)

        def main_units(c, g, after_last):
            us = []
            k0 = max(0, c - 4)
            for kt in range(k0, c + 1):
                mi = 0 if kt == c else (1 if kt == c - 4 else None)
                us.append(dict(np=128, k=KW[g][0:64, kt * 128:(kt + 1) * 128], q=q64(c, g), ktoks=[tk("KW", g, kt), tk("QBq", g, c)],
                               mask=(dmask[:, mi, :] if mi is not None else None),
                               out=lambda r_: ps[:, 5, r_ * 65:(r_ + 1) * 65], v=VA[:, 2 + g, kt, :], vtoks=[tk("VA", kt), tk("VAones")], otok=p5,
                               first=(kt == k0), last=(kt == c)))
            for kt in range(c + 1):
                us.append(dict(np=128, k=KE[g][0:96, kt * 128:(kt + 1) * 128], q=q96(c, g),
                               ktoks=[tk("KE", g, kt), tk("KEe", g), tk("QBq", g, c), tk("QBb", g, c)],
                               mask=(dmask[:, 0, :] if kt == c else None),
                               out=lambda r_: ps[:, 4, r_ * 65:(r_ + 1) * 65], v=VA[:, g, kt, :], vtoks=[tk("VA", kt), tk("VAones")], otok=p4,
                               first=(kt == 0), last=(kt == c)))
            us[-1]["after"] = after_last
            return us

        def attention_all():
            order = [(c, g) for c in range(NT) for g in range(2)]
            state = {}

            def start_cmp(j):
                cj, gj = order[j]
                state[j] = False

                def after():
                    selection(cj, gj)
                    state[j] = True
                emit_unit(cmp_unit(cj, gj, after))

            start_cmp(0)
            while not state[0]:
                pop_one()
            bias_T(*order[0])
            for i, (c, g) in enumerate(order):
                if i + 1 < len(order):
                    start_cmp(i + 1)

                def after_last(c=c, g=g):
                    combineB(c, g)
                    if g == 1:
                        delayed.append([3, lambda c=c: attn_finish_a(c)])
                        delayed.append([8, lambda c=c: attn_finish_b(c)])
                for u in main_units(c, g, after_last):
                    emit_unit(u)
                if i + 1 < len(order):
                    while not state[i + 1]:
                        pop_one()
                    bias_T(*order[i + 1])
            while pend:
                pop_one()
            while delayed:
                delayed.pop(0)[1]()

        def attn_finish_a(c):
            o_ = onsa[c % 2]
            tt_ = [tk("onsa", c % 2, 0), tk("onsa", c % 2, 1)]
            A(ACT, lambda e: e.activation(out=junk2, in_=o_, func=AF.Square, scale=float(1.0 / np.sqrt(512.0)), accum_out=ssq), r=tt_, w=[tk("junk2"), tk("ssq")])
            rsqrt_pool(rstd, ssq, 1, 1.0, EPS, tk("ssq"), tk("rstd"))
            A(DVE, lambda e: e.scalar_tensor_tensor(out=onb, in0=o_, scalar=rstd, in1=gnsa, op0=ALU.mult, op1=ALU.mult), r=tt_ + [tk("rstd"), tk("cf")], w=[tk("onb")])

        def attn_finish_b(c):
            for k in range(4):
                A(PE, lambda e, k=k: e.transpose(out=psh(6)[:, 256 + k * 128:256 + (k + 1) * 128], in_=onb[:, k * 128:(k + 1) * 128], identity=ident),
                  r=[tk("onb"), tk("cb")], w=[tk("ps", 6)])
            A(DVE, lambda e: e.tensor_copy(out=mixT[:, 4:8, c * 128:(c + 1) * 128], in_=psh(6)[:, 256:768].rearrange("p (k t) -> p k t", k=4)),
              r=[tk("ps", 6)], w=[tk("mixT", c)])

        def ffn_weight_dmas(gi):
            j0, j1 = FFN_GROUPS[gi]
            nj = j1 - j0
            b = gi % 2
            for u in range(2):
                A(POOL, lambda e, u=u: e.dma_start(
                    out=wgu[b][:, :, u, 0:nj * 128], in_=wgu_d[:, u * FFN + j0 * 128:u * FFN + j1 * 128].rearrange("(k p) c -> p k c", p=128)),
                  w=[tk("wgu", b, u)], dma=True)
            A(POOL, lambda e: e.dma_start(out=wdn[b][:, 0:nj, :], in_=wdn_d[j0 * 128:j1 * 128, :].rearrange("(j p) c -> p j c", p=128)),
              w=[tk("wdn", b)], dma=True)

        def prefetch3():
            A(SP, lambda e: e.dma_start(out=gbc, in_=gbf_d), w=[tk("gbc")], dma=True)
            for k in range(8):
                A(POOL, lambda e, k=k: e.dma_start(out=wout[:, k, :], in_=wout_d[k * 128:(k + 1) * 128, :]), w=[tk("wout", k)], dma=True)
            ffn_weight_dmas(0)

        def phase3():
            for t in range(NT):
                A(SP, lambda e, t=t: e.dma_start(out=hbuf[:, t, :], in_=x_d[t * 128:(t + 1) * 128, :]), w=[tk("h", t)], dma=True)

            def s1(t):
                b0 = 0 if t % 2 == 0 else 4
                Pv = ps[:, b0:b0 + 2, :].rearrange("p a b -> p (a b)")
                sq_, rs_ = ssq_p[t % 2], rstd_p[t % 2]
                for cc in range(2):
                    for k in range(8):
                        A(PE, lambda e, cc=cc, k=k: e.matmul(out=psb(b0 + cc), lhsT=mixT[:, k, t * 128:(t + 1) * 128], rhs=wout[:, k, cc * 512:(cc + 1) * 512],
                                                             start=(k == 0), stop=(k == 7)), r=[tk("mixT", t), tk("wout", k)], w=[tk("ps", b0 + cc)])
                A(DVE, lambda e: e.tensor_tensor(out=hbuf[:, t, :], in0=hbuf[:, t, :], in1=Pv, op=ALU.add), r=[tk("ps", b0), tk("ps", b0 + 1)], w=[tk("h", t)])
                A(ACT, lambda e: e.activation(out=junk3, in_=hbuf[:, t, :], func=AF.Square, scale=1.0 / 32.0, accum_out=sq_), r=[tk("h", t)], w=[tk("fth", 0), tk("ssqp", t % 2)])
                rsqrt_pool(rs_, sq_, 1, 1.0, EPS, tk("ssqp", t % 2), tk("rstdp", t % 2))

            def s2(t):
                hn = hns[t % 2]
                tb_ = 2 if t % 2 == 0 else 6
                A(DVE, lambda e: e.scalar_tensor_tensor(out=hn, in0=hbuf[:, t, :], scalar=rstd_p[t % 2], in1=gbc, op0=ALU.mult, op1=ALU.mult),
                  r=[tk("h", t), tk("rstdp", t % 2), tk("gbc")], w=[tk("hn", t % 2)])
                for k in range(8):
                    A(PE, lambda e, k=k: e.transpose(out=psh(tb_)[:, k * 128:(k + 1) * 128], in_=hn[:, k * 128:(k + 1) * 128], identity=ident),
                      r=[tk("hn", t % 2), tk("cb")], w=[tk("ps", tb_)])
                A(ACT, lambda e: e.copy(out=mixT[:, :, t * 128:(t + 1) * 128], in_=psh(tb_).rearrange("p (k t) -> p k t", k=8)),
                  r=[tk("ps", tb_)], w=[tk("mixT", t)])

            s1(0)
            for t in range(NT):
                if t + 1 < NT:
                    s1(t + 1)
                s2(t)
            if dbg:
                for t in range(NT):
                    pass
            for gi, (j0, j1) in enumerate(FFN_GROUPS):
                nj = j1 - j0
                b = gi % 2
                if gi > 0:
                    ffn_weight_dmas(gi)
                last = gi == len(FFN_GROUPS) - 1
                for tb in range(4):
                    ab, tab = actT[tb % 2], tk("actT", tb % 2)
                    hT = [tk("mixT", tb * 4 + i) for i in range(4)]
                    for jj in range(nj):
                        gb, ub = (0, 1) if jj % 2 == 0 else (2, 3)
                        f = jj % 2
                        for u, bank in ((0, gb), (1, ub)):
                            for k in range(8):
                                A(PE, lambda e, u=u, bank=bank, k=k, jj=jj, tb=tb, b=b: e.matmul(
                                    out=psb(bank), lhsT=wgu[b][:, k, u, jj * 128:(jj + 1) * 128], rhs=mixT[:, k, tb * 512:(tb + 1) * 512],
                                    start=(k == 0), stop=(k == 7)), r=hT + [tk("wgu", b, u)], w=[tk("ps", bank)])
                        tf = tk("fth", f)
                        A(ACT, lambda e, f=f, gb=gb: e.activation(out=fth[f], in_=psb(gb), func=AF.Tanh, scale=0.5), r=[tk("ps", gb)], w=[tf])
                        A(DVE, lambda e, f=f, gb=gb: e.scalar_tensor_tensor(out=fz[f], in0=fth[f], scalar=1.0, in1=psb(gb), op0=ALU.add, op1=ALU.mult),
                          r=[tf, tk("ps", gb)], w=[tf])
                        A(DVE, lambda e, f=f, ub=ub, jj=jj, ab=ab: e.scalar_tensor_tensor(out=ab[:, jj, :], in0=fz[f], scalar=0.5, in1=psb(ub), op0=ALU.mult, op1=ALU.mult),
                          r=[tf, tk("ps", ub)], w=[tab])
                    for tl in range(4):
                        t = tb * 4 + tl
                        for cc in range(2):
                            yb = 4 + (tl * 2 + cc) % 4
                            for jj in range(nj):
                                A(PE, lambda e, jj=jj, tl=tl, cc=cc, yb=yb, ab=ab, b=b, nj=nj: e.matmul(
                                    out=psb(yb), lhsT=ab[:, jj, tl * 128:(tl + 1) * 128], rhs=wdn[b][:, jj, cc * 512:(cc + 1) * 512],
                                    start=(jj == 0), stop=(jj == nj - 1)), r=[tab, tk("wdn", b)], w=[tk("ps", yb)])
                            hs = hbuf[:, t, cc * 512:(cc + 1) * 512]
                            A(DVE, lambda e, hs=hs, yb=yb: e.tensor_tensor(out=hs, in0=hs, in1=psb(yb), op=ALU.add), r=[tk("h", t), tk("ps", yb)], w=[tk("h", t)])
                        if last:
                            out_ops.append(A(SP, lambda e, t=t: e.dma_start(out=y_d[t * 128:(t + 1) * 128, :], in_=hbuf[:, t, :]), r=[tk("h", t)], dma=True))

        if phases >= 2:
            S.barrier()
            junk2 = Aten[:, cth_off:cth_off + 1024].bitcast(BF)
            obuf = Aten[:, cth_off + 1024:cth_off + 1024 + 2080].bitcast(F32).rearrange("p (b n) -> p b n", b=2)
            phase2_setup()
            if phases >= 3:
                prefetch3()
            for g in range(2):
                compress(g)
            if dbg:
                for g in range(2):
                    dump("kcTc%d" % g, kcTc[g], [64, 127], BF, [tk("kcTc", g)])
                    dump("VCa%d" % g, VCa[g], [127, 97], BF, [tk("VCa", g), tk("VCa", g), tk("VCa", g)])
            attention_all()
            if dbg:
                dump("mixn", mixT[:, 4:8, :].rearrange("p k t -> p (k t)"), [128, 4 * 2048], BF, [tk("mixT", t) for t in range(NT)])
        if phases >= 3:
            S.barrier()
            phase3()

        def live(pa, pb):
            sets = {"all": {0, 1, 2, 3}, "p1a": {0}, "p1b": {1}, "p1": {0, 1}, "p2": {2}, "p12": {0, 1, 2}, "p3": {3}, "p23": {2, 3}}
            return bool(sets[pa] & sets[pb])
        for i in range(len(allocs)):
            for j in range(i + 1, len(allocs)):
                n1, o1, s1, p1 = allocs[i]
                n2, o2, s2, p2 = allocs[j]
                if live(p1, p2) and o1 < o2 + s2 and o2 < o1 + s1:
                    raise AssertionError("arena overlap %s %s" % (allocs[i], allocs[j]))

        S.emit(final_wait_ops=out_ops)
        build.stats = S.stats
    return nc, dbg_out


def _consts():
    half = 32
    inv = 10000.0 ** (-np.arange(half, dtype=np.float64) / half)
    pos = np.arange(S_LEN, dtype=np.float64)
    ang = pos[:, None] * inv[None, :]
    cos = np.cos(ang).astype(np.float32).reshape(NT, 128, 32).transpose(1, 0, 2).reshape(128, 512)
    sin = np.sin(ang).astype(np.float32).reshape(NT, 128, 32).transpose(1, 0, 2).reshape(128, 512)
    cpos = np.arange(127, dtype=np.float64) * 16 + 31
    angc = cpos[:, None] * inv[None, :]
    cosc = np.zeros((128, 32), np.float32); cosc[:127] = np.cos(angc)
    sinc = np.zeros((128, 32), np.float32); sinc[:127] = np.sin(angc)
    ql = np.arange(128)[:, None, None]
    c = np.arange(NT)[None, :, None]
    j = np.arange(32)[None, None, :]
    cur = 2 * c + (ql >= 64)
    valid = j <= cur
    forced = (j == 0) | (j == cur) | (j == cur - 1)
    addc = np.where(valid, np.where(forced, 1e6, 0.0), -2e6).astype(np.float32).reshape(128, 512)
    n = np.arange(128)[:, None, None]
    c2 = np.arange(NT)[None, :, None]
    q2 = np.arange(128)[None, None, :]
    maskc = ((16 * n + 31) <= (128 * c2 + q2)).astype(np.float32)
    maskc[127] = 0
    kl = np.arange(128)[:, None]
    qq = np.arange(128)[None, :]
    dmask = np.stack([(kl <= qq), (kl > qq)], axis=1).astype(np.float32)
    cbb = np.concatenate([np.eye(128, dtype=np.float32), maskc.reshape(128, 2048), dmask.reshape(128, 256)], axis=1).astype(ml_dtypes.bfloat16)
    etab = (np.arange(S_LEN)[None, :] // 64 == np.arange(32)[:, None]).astype(np.float32).astype(ml_dtypes.bfloat16)
    cs = np.arange(127)[:, None] * 16
    ss = np.arange(32)[None, :] * 64
    ov = np.clip(np.minimum(cs + 32, ss + 64) - np.maximum(cs, ss), 0, None) / 32.0
    ovl = ov.astype(np.float32).astype(ml_dtypes.bfloat16)
    return cos, sin, cosc, sinc, addc, cbb, etab, ovl


def _prep(inputs):
    f = lambda a: np.ascontiguousarray(np.asarray(a, dtype=np.float32))
    cos, sin, cosc, sinc, addc, cbb, etab, ovl = _consts()
    w_in = f(inputs["w_in"])[0]
    cols = np.concatenate([
        np.arange(0, 512),
        np.arange(768, 896), np.arange(1024, 1152),
        np.arange(512, 576), np.arange(640, 704), np.arange(576, 640), np.arange(704, 768),
        np.arange(896, 1024), np.arange(1152, 1280),
        np.arange(1280, 1304),
        np.arange(1304, 2328)])
    w_in_p = np.ascontiguousarray(w_in[:, cols])
    w_out = f(inputs["w_out"])[0]
    w_out_p = np.ascontiguousarray(np.concatenate([w_out[512:], w_out[:512]], axis=0))
    rep = lambda v, nrep: np.tile(f(v).reshape(-1), nrep)
    g12 = np.concatenate([rep(inputs["q_norm_g"], 8), rep(inputs["k_norm_slc_g"], 2), rep(inputs["k_norm_win_g"], 2)])
    bc = lambda v: np.ascontiguousarray(np.broadcast_to(np.asarray(v, np.float32).reshape(1, -1), (128, np.asarray(v).size)))
    convc = np.zeros((128, 4, 35), np.float32)
    dw = f(inputs["conv_dw_w"])[0, :, 0, :]
    convc[:, :, 0:31] = dw.T.reshape(4, 128, 31).transpose(1, 0, 2)
    for idx, nm in [(31, "conv_dw_b"), (32, "conv_ln_g"), (33, "conv_ln_b"), (34, "out_norm_conv_g")]:
        convc[:, :, idx] = f(inputs[nm])[0].reshape(4, 128).T
    cfb = np.concatenate([cos, sin, bc(g12), bc(f(inputs["out_norm_nsa_g"])[0]), addc, convc.reshape(128, 140), cosc, sinc,
                          bc(f(inputs["k_norm_cmp_g"])[0])], axis=1)
    assert cfb.shape == (128, CF_END), cfb.shape
    pet = np.concatenate([f(inputs["cmp_pe_k"])[0].T, f(inputs["cmp_pe_v"])[0].T], axis=0)
    shared = {
        "w_in": w_in_p, "w_out": w_out_p, "w_gu": f(inputs["w_gate_up"])[0], "w_dn": f(inputs["w_down"])[0],
        "w1k": f(inputs["cmp_w1_k"])[0], "w1v": f(inputs["cmp_w1_v"])[0], "w2k": f(inputs["cmp_w2_k"])[0], "w2v": f(inputs["cmp_w2_v"])[0],
        "cf": np.ascontiguousarray(cfb), "gbc_attn": bc(f(inputs["attn_norm_g"])[0]), "gbc_ffn": bc(f(inputs["ffn_norm_g"])[0]),
        "cb": np.ascontiguousarray(cbb), "etab": np.ascontiguousarray(etab), "ovl": np.ascontiguousarray(ovl), "pet": np.ascontiguousarray(pet),
    }
    x = f(inputs["x"])
    return [dict(shared, x=np.ascontiguousarray(x[b])) for b in range(8)]


def kernel(**inputs):
    nc, _ = build()
    in_maps = _prep(inputs)
    res = run_bass_kernel_spmd(nc, in_maps, core_ids=list(range(8)))
    return np.stack([np.asarray(r["y"], dtype=np.float32) for r in res.results], axis=0)
```
